# Optimizing a Trainium2 kernel written in Bass

```python
import jax
import jax.numpy as jnp
from jax import lax
import numpy as np

D_MODEL = 1024
BATCH = 32
SEQ = 2048
DEPTH = 2
DEC_BATCH = 4
DEC_SEQ = 8192
PAST_LEN = 128

F32 = jnp.float32
EPS = 1e-6
N_EVEN = (DEPTH + 1) // 2
N_ODD = DEPTH // 2

A_WIDTH = D_MODEL // 2
A_GROUPS = 4
A_GROUP_DIM = A_WIDTH // A_GROUPS
CHUNK = 128
B_WIDTH = D_MODEL // 2
CONV_W = 31
EVEN_IN = 2 * A_WIDTH + 2 * B_WIDTH
EVEN_OUT = A_WIDTH + B_WIDTH
C_HEADS = 4
C_HEAD_DIM = D_MODEL // C_HEADS
C_WIDTH = C_HEADS * C_HEAD_DIM
C_CHUNK = 128
N_GATES = 4 * C_HEADS
ODD_IN = 4 * C_WIDTH + N_GATES
FORGET_BIAS = 3.0
X_HEADS = 4
X_HEAD_DIM = D_MODEL // X_HEADS
MEM_LEN = 256
N_EXPERTS = 16
EXPERT_FF = 2048
CAPACITY_FACTOR = 2

kernel_name = "hybrid_bidir_gmlp_conformer_mlstm_ecmoe"


def rmsnorm(x, g):
    xf = x.astype(F32)
    y = xf * lax.rsqrt(jnp.mean(xf * xf, axis=-1, keepdims=True) + EPS)
    return (y * g.astype(F32)).astype(x.dtype)


def layernorm(x, g, b):
    xf = x.astype(F32)
    mu = jnp.mean(xf, axis=-1, keepdims=True)
    var = jnp.mean(jnp.square(xf - mu), axis=-1, keepdims=True)
    y = (xf - mu) * lax.rsqrt(var + EPS)
    return (y * g.astype(F32) + b.astype(F32)).astype(x.dtype)


def spatial_gating(u, v, ws, bs, ln_g, ln_b):
    bn, s, _ = v.shape
    v = layernorm(v, ln_g, ln_b)
    vc = v.reshape(bn, s // CHUNK, CHUNK, A_GROUPS, A_GROUP_DIM)
    mixed = jnp.einsum('gpq,bnqgc->bnpgc', ws, vc) + bs.T[None, None, :, :, None]
    return u * mixed.reshape(bn, s, A_WIDTH)


def even_mixer(h, w_in, ws, bs, a_ln_g, a_ln_b, conv_w, conv_b, b_ln_g, b_ln_b, w_out):
    z = h @ w_in
    a_u, a_v, b_val, b_gate = jnp.split(z, [A_WIDTH, 2 * A_WIDTH, 2 * A_WIDTH + B_WIDTH], axis=-1)
    a_out = spatial_gating(jax.nn.gelu(a_u), jax.nn.gelu(a_v), ws, bs, a_ln_g, a_ln_b)
    g = b_val * jax.nn.sigmoid(b_gate)
    c = lax.conv_general_dilated(
        g, conv_w[:, None, :].astype(g.dtype), window_strides=(1,),
        padding=[(CONV_W // 2, CONV_W // 2)],
        dimension_numbers=('NWC', 'WIO', 'NWC'),
        feature_group_count=B_WIDTH) + conv_b
    b_out = jax.nn.silu(layernorm(c, b_ln_g, b_ln_b))
    return jnp.concatenate([a_out, b_out], axis=-1) @ w_out


def mlstm_scan(q, k, v, i_pre, f_pre):
    bn, s, nh, dh = q.shape
    nc = s // C_CHUNK
    L = C_CHUNK

    def chunks4(t):
        return t.astype(F32).reshape(bn, nc, L, nh, dh).transpose(1, 0, 3, 2, 4)

    def chunks3(t):
        return t.astype(F32).reshape(bn, nc, L, nh).transpose(1, 0, 3, 2)

    tril = jnp.tril(jnp.ones((L, L), dtype=bool))

    def step(carry, xs):
        cmat, nvec, m = carry
        qc, kc, vc, ic, fc = xs
        bcum = jnp.cumsum(jax.nn.log_sigmoid(fc), axis=-1)
        dmat = bcum[..., :, None] - bcum[..., None, :] + ic[..., None, :]
        dmat = jnp.where(tril, dmat, -jnp.inf)
        inter = bcum + m[..., None]
        m_row = jnp.maximum(jnp.max(dmat, axis=-1), inter)
        smat = jnp.einsum('bhld,bhjd->bhlj', qc, kc) * jnp.exp(dmat - m_row[..., None])
        w_inter = jnp.exp(inter - m_row)
        num = jnp.einsum('bhlj,bhjd->bhld', smat, vc) + w_inter[..., None] * jnp.einsum('bhde,bhle->bhld', cmat, qc)
        den = jnp.sum(smat, axis=-1) + w_inter * jnp.einsum('bhd,bhld->bhl', nvec, qc)
        hc = num / jnp.maximum(jnp.abs(den), jnp.exp(-m_row))[..., None]
        b_last = bcum[..., -1]
        g = b_last[..., None] - bcum + ic
        m_new = jnp.maximum(b_last + m, jnp.max(g, axis=-1))
        wg = jnp.exp(g - m_new[..., None])
        decay = jnp.exp(b_last + m - m_new)
        c_new = decay[..., None, None] * cmat + jnp.einsum('bhl,bhld,bhle->bhde', wg, vc, kc)
        n_new = decay[..., None] * nvec + jnp.einsum('bhl,bhld->bhd', wg, kc)
        return (c_new, n_new, m_new), hc

    init = (jnp.zeros((bn, nh, dh, dh), F32), jnp.zeros((bn, nh, dh), F32), jnp.zeros((bn, nh), F32))
    _, hs = lax.scan(step, init, (chunks4(q), chunks4(k), chunks4(v), chunks3(i_pre), chunks3(f_pre)))
    return hs.transpose(1, 0, 3, 2, 4).reshape(bn, s, nh, dh)


def odd_mixer(h, w_in, b_gate, hnorm_g, w_out):
    bn, s, _ = h.shape
    z = h @ w_in
    q, k, v, o, gates = jnp.split(z, [C_WIDTH, 2 * C_WIDTH, 3 * C_WIDTH, 4 * C_WIDTH], axis=-1)
    shp = (bn, s, C_HEADS, C_HEAD_DIM)
    q = q.reshape(shp)
    k = k.reshape(shp) * (C_HEAD_DIM ** -0.5)
    v = v.reshape(shp)
    gates = (gates.astype(F32) + b_gate.astype(F32)).reshape(bn, s, 4, C_HEADS)
    h_fwd = mlstm_scan(q, k, v, gates[:, :, 0], gates[:, :, 1])
    rev = lambda t: jnp.flip(t, axis=1)
    h_bwd = rev(mlstm_scan(rev(q), rev(k), rev(v), rev(gates[:, :, 2]), rev(gates[:, :, 3])))
    hs = rmsnorm(h_fwd + h_bwd, hnorm_g).astype(h.dtype)
    out = jax.nn.sigmoid(o) * hs.reshape(bn, s, C_WIDTH)
    return out @ w_out


def cross_attend(h, mem, wq, wkv, wo):
    bn, s, _ = h.shape
    m_len = mem.shape[1]
    q = (h @ wq).reshape(bn, s, X_HEADS, X_HEAD_DIM)
    k, v = jnp.split(mem @ wkv, 2, axis=-1)
    k = k.reshape(bn, m_len, X_HEADS, X_HEAD_DIM)
    v = v.reshape(bn, m_len, X_HEADS, X_HEAD_DIM)
    scores = jnp.einsum('bshd,bmhd->bhsm', q, k).astype(F32) * (X_HEAD_DIM ** -0.5)
    p = jax.nn.softmax(scores, axis=-1).astype(v.dtype)
    o = jnp.einsum('bhsm,bmhd->bshd', p, v).reshape(bn, s, D_MODEL)
    return o @ wo


def expert_choice_ffn(h, w_router, w1, w3, w2):
    bn, s, d = h.shape
    t = bn * s
    xt = h.reshape(t, d)
    aff = jax.nn.softmax((xt @ w_router).astype(F32), axis=-1)
    cap = CAPACITY_FACTOR * t // N_EXPERTS
    gate, idx = lax.top_k(aff.T, cap)
    xe = xt[idx]
    hid = jax.nn.silu(jnp.einsum('ecd,edf->ecf', xe, w1)) * jnp.einsum('ecd,edf->ecf', xe, w3)
    ye = jnp.einsum('ecf,efd->ecd', hid, w2) * gate[..., None].astype(h.dtype)
    out = jnp.zeros_like(xt).at[idx.reshape(-1)].add(ye.reshape(-1, d))
    return out.reshape(bn, s, d)


def trunk(x, mem, norm_mix, norm_xattn, norm_ffn, even_w_in, a_ws, a_bs, a_ln_g, a_ln_b,
          b_conv_w, b_conv_b, b_ln_g, b_ln_b, even_w_out, odd_w_in, odd_b_gate, odd_hnorm_g,
          odd_w_out, xa_wq, xa_wkv, xa_wo, moe_router, moe_w1, moe_w3, moe_w2, norm_final):
    for l in range(DEPTH):
        j = l // 2
        hmix = rmsnorm(x, norm_mix[l])
        if l % 2 == 0:
            x = x + even_mixer(hmix, even_w_in[j], a_ws[j], a_bs[j], a_ln_g[j], a_ln_b[j],
                               b_conv_w[j], b_conv_b[j], b_ln_g[j], b_ln_b[j], even_w_out[j])
        else:
            x = x + odd_mixer(hmix, odd_w_in[j], odd_b_gate[j], odd_hnorm_g[j], odd_w_out[j])
        x = x + cross_attend(rmsnorm(x, norm_xattn[l]), mem, xa_wq[l], xa_wkv[l], xa_wo[l])
        x = x + expert_choice_ffn(rmsnorm(x, norm_ffn[l]), moe_router[l], moe_w1[l], moe_w3[l], moe_w2[l])
    return rmsnorm(x, norm_final)


def _normal(key, shape, scale):
    return jax.random.normal(key, shape, jnp.float32) * scale


def setup_inputs(seed: int = 0) -> dict:
    key = jax.random.key(seed)
    ks = jax.random.split(key, 32)
    D = D_MODEL
    gate_base = jnp.concatenate([jnp.zeros((C_HEADS,), F32), jnp.full((C_HEADS,), FORGET_BIAS, F32),
                                 jnp.zeros((C_HEADS,), F32), jnp.full((C_HEADS,), FORGET_BIAS, F32)])
    return {
        'x_prompt': _normal(ks[0], (BATCH, SEQ, D), 1.0),
        'x_sample': _normal(ks[1], (DEC_BATCH, DEC_SEQ, D), 1.0),
        'mem_prompt': _normal(ks[2], (BATCH, MEM_LEN, D), 1.0),
        'mem_sample': _normal(ks[3], (DEC_BATCH, MEM_LEN, D), 1.0),
        'norm_mix': 1.0 + _normal(ks[4], (DEPTH, D), 0.02),
        'norm_xattn': 1.0 + _normal(ks[5], (DEPTH, D), 0.02),
        'norm_ffn': 1.0 + _normal(ks[6], (DEPTH, D), 0.02),
        'even_w_in': _normal(ks[7], (N_EVEN, D, EVEN_IN), D ** -0.5),
        'a_ws': _normal(ks[8], (N_EVEN, A_GROUPS, CHUNK, CHUNK), 0.5 * CHUNK ** -0.5),
        'a_bs': 1.0 + _normal(ks[9], (N_EVEN, A_GROUPS, CHUNK), 0.1),
        'a_ln_g': 1.0 + _normal(ks[10], (N_EVEN, A_WIDTH), 0.02),
        'a_ln_b': _normal(ks[11], (N_EVEN, A_WIDTH), 0.02),
        'b_conv_w': _normal(ks[12], (N_EVEN, CONV_W, B_WIDTH), CONV_W ** -0.5),
        'b_conv_b': _normal(ks[13], (N_EVEN, B_WIDTH), 0.02),
        'b_ln_g': 1.0 + _normal(ks[14], (N_EVEN, B_WIDTH), 0.02),
        'b_ln_b': _normal(ks[15], (N_EVEN, B_WIDTH), 0.02),
        'even_w_out': _normal(ks[16], (N_EVEN, EVEN_OUT, D), EVEN_OUT ** -0.5),
        'odd_w_in': _normal(ks[17], (N_ODD, D, ODD_IN), D ** -0.5),
        'odd_b_gate': gate_base + _normal(ks[18], (N_ODD, N_GATES), 0.1),
        'odd_hnorm_g': 1.0 + _normal(ks[19], (N_ODD, C_HEADS, C_HEAD_DIM), 0.02),
        'odd_w_out': _normal(ks[20], (N_ODD, C_WIDTH, D), C_WIDTH ** -0.5),
        'xa_wq': _normal(ks[21], (DEPTH, D, D), D ** -0.5),
        'xa_wkv': _normal(ks[22], (DEPTH, D, 2 * D), D ** -0.5),
        'xa_wo': _normal(ks[23], (DEPTH, D, D), D ** -0.5),
        'moe_router': _normal(ks[24], (DEPTH, D, N_EXPERTS), D ** -0.5),
        'moe_w1': _normal(ks[25], (DEPTH, N_EXPERTS, D, EXPERT_FF), D ** -0.5),
        'moe_w3': _normal(ks[26], (DEPTH, N_EXPERTS, D, EXPERT_FF), D ** -0.5),
        'moe_w2': _normal(ks[27], (DEPTH, N_EXPERTS, EXPERT_FF, D), EXPERT_FF ** -0.5),
        'norm_final': 1.0 + _normal(ks[28], (D,), 0.02),
    }


def reference(x_prompt, x_sample, mem_prompt, mem_sample, norm_mix, norm_xattn, norm_ffn,
              even_w_in, a_ws, a_bs, a_ln_g, a_ln_b, b_conv_w, b_conv_b, b_ln_g, b_ln_b,
              even_w_out, odd_w_in, odd_b_gate, odd_hnorm_g, odd_w_out, xa_wq, xa_wkv, xa_wo,
              moe_router, moe_w1, moe_w3, moe_w2, norm_final):
    y_prompt = trunk(x_prompt, mem_prompt, norm_mix, norm_xattn, norm_ffn, even_w_in, a_ws, a_bs,
                     a_ln_g, a_ln_b, b_conv_w, b_conv_b, b_ln_g, b_ln_b, even_w_out, odd_w_in,
                     odd_b_gate, odd_hnorm_g, odd_w_out, xa_wq, xa_wkv, xa_wo, moe_router,
                     moe_w1, moe_w3, moe_w2, norm_final)
    y_sample = trunk(x_sample, mem_sample, norm_mix, norm_xattn, norm_ffn, even_w_in, a_ws, a_bs,
                     a_ln_g, a_ln_b, b_conv_w, b_conv_b, b_ln_g, b_ln_b, even_w_out, odd_w_in,
                     odd_b_gate, odd_hnorm_g, odd_w_out, xa_wq, xa_wkv, xa_wo, moe_router,
                     moe_w1, moe_w3, moe_w2, norm_final)
    return (y_prompt, y_sample)
```

```python
import numpy as np
import concourse.bass as bass
import concourse.mybir as mybir
from concourse.bass_utils import run_bass_kernel_spmd
from contextlib import ExitStack

ENGS = ("pe", "act", "dve", "pool", "sp")
SEM_WINDOW = 20000
DMA_SEM_MAX = 3900


class Buf:
    __slots__ = ("name", "t", "last_w", "reads", "dsem", "dcnt")

    def __init__(self, name, t=None):
        self.name = name
        self.t = t
        self.last_w = None
        self.reads = []
        self.dsem = None
        self.dcnt = 0

    def __getitem__(self, k):
        return self.t[k]


class Sched:
    def __init__(self, nc, stack, same_engine_sync=True):
        self.nc = nc
        self.stack = stack
        self.ops = {e: [] for e in ENGS}
        self.signal = {e: set() for e in ENGS}
        self.known = {e: {} for e in ENGS}
        self.same_engine_sync = same_engine_sync
        self.free_dma_sems = []
        self.all_dma = []
        self.nsem = 0
        self.dma_bufs = []
        self.bufs = []
        self.emitted = {e: 0 for e in ENGS}
        self.semval = {}
        self.semstate = {e: [None, 0] for e in ENGS}

    def buf(self, name, t=None):
        b = Buf(name, t)
        self.bufs.append(b)
        return b

    def end_phase(self):
        self.barrier()
        self.emit()
        for b in self.bufs:
            b.last_w = None
            b.reads = []
        for b in self.dma_bufs:
            if b.dsem is not None:
                self.free_dma_sems.append((b.dsem, b.dcnt))
                b.dsem = None
        self.dma_bufs = []
        self.bufs = [b for b in self.bufs if b.t is None or getattr(b, "keep", False)] if False else self.bufs

    def new_sem(self, name):
        self.nsem += 1
        return self.stack.enter_context(self.nc.semaphore(f"{name}_{self.nsem}"))

    def _dma_sem(self, buf):
        if buf.dsem is None or buf.dcnt >= DMA_SEM_MAX:
            got = False
            if buf.dsem is None:
                while self.free_dma_sems:
                    sem, cnt = self.free_dma_sems.pop()
                    if cnt < DMA_SEM_MAX - 200:
                        buf.dsem, buf.dcnt = sem, cnt
                        got = True
                        break
            if not got:
                buf.dsem, buf.dcnt = self.new_sem("d"), 0
            if buf not in self.dma_bufs:
                self.dma_bufs.append(buf)
        return buf.dsem

    def release(self, *bufs):
        for b in bufs:
            if b.dsem is not None:
                self.free_dma_sems.append((b.dsem, b.dcnt))
                b.dsem = None
            if b in self.dma_bufs:
                self.dma_bufs.remove(b)

    def _need(self, eng, ev, waits, raw=False):
        if ev is None:
            return
        if ev[0] == "c":
            _, e2, idx = ev
            if e2 == eng and not (raw and eng != "pe" and self.same_engine_sync):
                return
            k = ("c", e2)
            if self.known[eng].get(k, -1) >= idx:
                return
            self.known[eng][k] = idx
            self.signal[e2].add(idx)
            waits.append(ev)
        else:
            _, sem, val = ev
            k = ("d", id(sem))
            if self.known[eng].get(k, -1) >= val:
                return
            self.known[eng][k] = val
            waits.append(ev)

    def op(self, eng, fn, reads=(), writes=(), dma_key=None):
        waits = []
        for r in reads:
            self._need(eng, r.last_w, waits, raw=True)
        for w in writes:
            self._need(eng, w.last_w, waits)
            for ev in w.reads:
                self._need(eng, ev, waits)
        idx = len(self.ops[eng])
        if dma_key is not None:
            sem = self._dma_sem(dma_key)
            dma_key.dcnt += 1
            ev = ("d", sem, 16 * dma_key.dcnt)
        else:
            ev = ("c", eng, idx)
        self.ops[eng].append([waits, fn, ev])
        for r in reads:
            r.reads.append(ev)
        for w in writes:
            w.last_w = ev
            w.reads = []
        return ev

    def dma(self, eng, out_ap, in_ap, reads=(), writes=(), key=None, **kw):
        key = key if key is not None else (writes[0] if writes else reads[0])
        return self.op(eng, lambda e: e.dma_start(out=out_ap, in_=in_ap, **kw),
                       reads=reads, writes=writes, dma_key=key)

    def barrier(self):
        last = {e: len(self.ops[e]) - 1 for e in ENGS}
        for e in ENGS:
            waits = []
            for e2 in ENGS:
                if e2 != e and last[e2] >= self.emitted[e2] and self.ops[e2][last[e2]][2][0] == "c":
                    self._need(e, ("c", e2, last[e2]), waits)
                elif e2 != e and last[e2] >= self.emitted[e2]:
                    for j in range(last[e2], self.emitted[e2] - 1, -1):
                        if self.ops[e2][j][2][0] == "c":
                            self._need(e, ("c", e2, j), waits)
                            break
            for b in list(self.dma_bufs):
                if b.dsem is not None and b.dcnt > 0:
                    self._need(e, ("d", b.dsem, 16 * b.dcnt), waits)
            if waits:
                self.ops[e].append([waits, None, ("c", e, len(self.ops[e]))])

    def emit(self):
        nc = self.nc
        semval = self.semval
        for e in ENGS:
            sem, n = self.semstate[e]
            for idx in sorted(i for i in self.signal[e] if i >= self.emitted[e]):
                if sem is None or n >= SEM_WINDOW:
                    sem = self.new_sem("p" + e)
                    n = 0
                n += 1
                semval[(e, idx)] = (sem, n)
            self.semstate[e] = [sem, n]
        engobj = {"pe": "tensor", "act": "scalar", "dve": "vector", "pool": "gpsimd", "sp": "sync"}

        def run(ename, eng):
            start = self.emitted[ename]
            for idx in range(start, len(self.ops[ename])):
                waits, fn, ev = self.ops[ename][idx]
                for w in waits:
                    if w[0] == "c":
                        s, v = semval[(w[1], w[2])]
                        eng.wait_ge(s, v)
                    else:
                        eng.wait_ge(w[1], w[2])
                if fn is None:
                    if (ename, idx) in semval:
                        s, v = semval[(ename, idx)]
                        eng.sem_inc(s, 1)
                    continue
                ins = fn(eng)
                if ev[0] == "d":
                    ins.then_inc(ev[1], 16)
                elif (ename, idx) in semval:
                    ins.then_inc(semval[(ename, idx)][0], 1)

        with nc.Block() as block:
            @block.tensor
            def _(e):
                run("pe", e)

            @block.scalar
            def _(e):
                run("act", e)

            @block.vector
            def _(e):
                run("dve", e)

            @block.gpsimd
            def _(e):
                run("pool", e)

            @block.sync
            def _(e):
                run("sp", e)
        for e in ENGS:
            self.emitted[e] = len(self.ops[e])

F32 = mybir.dt.float32
BF16 = mybir.dt.bfloat16
I32 = mybir.dt.int32
AF = mybir.ActivationFunctionType
ALU = mybir.AluOpType
AX = mybir.AxisListType

D = 1024
NE = 16
MEM = 256
EPS = 1e-6
NCORES = 8


class Cfg:
    def __init__(self, seg=2048, ff=2048, cmax=None):
        self.SEG = seg
        self.NSEG = 6
        self.TOK = self.NSEG * seg
        self.NT = self.TOK // 512
        self.NB = self.TOK // 128
        self.FF = ff
        self.NF = ff // 128
        self.NBALL = NCORES * self.NB
        self.CMAX = cmax if cmax is not None else ((self.TOK // 8) * 3 // 2 + 255) // 256 * 256
        self.CAP_P = 2 * (32 * seg) // NE
        self.CAP_S = 2 * (4 * 4 * seg) // NE


class Prog:
    def __init__(self, cfg):
        self.cfg = cfg
        self.nc = bass.Bass("TRN2", target_bir_lowering=False)
        self.st = ExitStack()
        self.S = Sched(self.nc, self.st)
        self.psn = 0
        self.ps = [self.S.buf(f"ps{i}", self.st.enter_context(self.nc.psum_tensor(f"ps{i}", [128, 512], F32)))
                   for i in range(8)]
        self.ins = {}
        self.outs = {}
        self.cn = 0
        self.consts_done = False

    def din(self, name, shape, dt=F32):
        h = self.nc.dram_tensor(name, list(shape), dt, kind="ExternalInput")
        self.ins[name] = h
        return h

    def dout(self, name, shape, dt=F32):
        h = self.nc.dram_tensor(name, list(shape), dt, kind="ExternalOutput")
        self.outs[name] = h
        return h

    def dint(self, name, shape, dt=F32):
        return self.nc.dram_tensor(name, list(shape), dt, kind="Internal")

    def sb(self, stack, name, shape, dt=F32):
        self.cn += 1
        return self.S.buf(name, stack.enter_context(self.nc.sbuf_tensor(f"{name}_{self.cn}", list(shape), dt)))

    def dbg(self, name, buf, ap, shape, dt=F32):
        if not getattr(self, "debug", False) or name in self.outs:
            return
        h = self.dout("dbg_" + name, shape, dt)
        self.S.dma("sp", h.ap(), ap, reads=[buf], writes=[self.S.buf("dbg_" + name)])

    def nps(self):
        b = self.ps[self.psn % 7]
        self.psn += 1
        return b

    @property
    def psx(self):
        return self.ps[7]

    def breg2(self, e):
        if getattr(self, "_bc2", None) is None:
            self._bc2 = e.to_reg(self.cfg.TOK - 1)
        return self._bc2

    def breg(self, e):
        if getattr(self, "_bc", None) is None:
            self._bc = e.to_reg(self.cfg.CMAX - 1)
        return self._bc

    def consts(self):
        S, nc, st = self.S, self.nc, self.st
        self.ones_f = self.sb(st, "ones_f", [128, 128], F32)
        self.ones_b = self.sb(st, "ones_b", [128, 128], BF16)
        self.id_f = self.sb(st, "id_f", [128, 128], F32)
        self.id_b = self.sb(st, "id_b", [128, 128], BF16)
        self.tri_b = self.sb(st, "tri_b", [128, 128], BF16)
        self.tri_f = self.sb(st, "tri_f", [128, 128], F32)
        of, ob, idf, idb, trb, trf = self.ones_f, self.ones_b, self.id_f, self.id_b, self.tri_b, self.tri_f
        S.op("pool", lambda e: e.memset(of[:], 1.0), writes=[of])
        S.op("pool", lambda e: e.memset(ob[:], 1.0), writes=[ob])
        S.op("pool", lambda e: e.memset(idf[:], 0.0), writes=[idf])
        S.op("pool", lambda e: e.memset(trf[:], 0.0), writes=[trf])
        S.op("pool", lambda e: e.affine_select(out=idf[:], in_=of[:], pattern=[[-1, 128]], compare_op=ALU.is_equal,
                                               fill=0.0, base=0, channel_multiplier=1), reads=[of], writes=[idf])
        S.op("pool", lambda e: e.affine_select(out=trf[:], in_=of[:], pattern=[[1, 128]], compare_op=ALU.is_ge,
                                               fill=0.0, base=0, channel_multiplier=-1), reads=[of], writes=[trf])
        self.tril_f = self.sb(st, "tril_f", [128, 128], F32)
        self.ones512 = self.sb(st, "ones512", [128, 512], F32)
        tl, o5 = self.tril_f, self.ones512
        S.op("pool", lambda e: e.memset(tl[:], 0.0), writes=[tl])
        S.op("pool", lambda e: e.memset(o5[:], 1.0), writes=[o5])
        S.op("pool", lambda e: e.affine_select(out=tl[:], in_=of[:], pattern=[[-1, 128]], compare_op=ALU.is_ge,
                                               fill=0.0, base=0, channel_multiplier=1), reads=[of], writes=[tl])
        S.op("pool", lambda e: e.tensor_copy(out=idb[:], in_=idf[:]), reads=[idf], writes=[idb])
        S.op("pool", lambda e: e.tensor_copy(out=trb[:], in_=trf[:]), reads=[trf], writes=[trb])

    def load_w(self, wsb, wdram_ap, nk, ndma=4, dbuf=None):
        src = wdram_ap.rearrange("(k p) n -> p k n", p=128)
        step = max(1, nk // ndma)
        for k0 in range(0, nk, step):
            k1 = min(nk, k0 + step)
            self.S.dma("pool", wsb[:, k0:k1, :], src[:, k0:k1, :], reads=[dbuf] if dbuf else [], writes=[wsb])

    def transpose_blocks(self, xn_b, nb, xnT, rows=128, evac=("act", "dve")):
        S = self.S
        for b in range(nb):
            ps = self.nps()
            psb = ps.t.bitcast(BF16)
            for k in range(8):
                S.op("pe", lambda e, b=b, k=k, psb=psb: e.transpose(psb[:, k * rows:(k + 1) * rows],
                                                                   xn_b[0:rows, b, k * 128:(k + 1) * 128],
                                                                   self.id_b[0:rows, 0:rows]),
                     reads=[xn_b, self.id_b], writes=[ps])
            eng = evac[b % len(evac)]
            src = psb[:, 0:8 * rows].rearrange("p (k t) -> p k t", k=8)
            if eng == "act":
                S.op("act", lambda e, b=b, src=src: e.copy(out=xnT[:, :, b * rows:(b + 1) * rows], in_=src),
                     reads=[ps], writes=[xnT])
            else:
                S.op("dve", lambda e, b=b, src=src: e.tensor_copy(out=xnT[:, :, b * rows:(b + 1) * rows], in_=src),
                     reads=[ps], writes=[xnT])

    def proj_fm(self, ps, w, c0, xT, n=512, nk=8, cw=128):
        for k in range(nk):
            self.S.op("pe", lambda e, k=k: e.matmul(ps[0:cw, 0:n], w[:, k, c0:c0 + cw], xT[:, k, 0:n],
                                                    start=(k == 0), stop=(k == nk - 1)),
                      reads=[w, xT], writes=[ps])

    def proj_tm(self, ps, xT, t0, w, n0, n=512, nk=8):
        for k in range(nk):
            self.S.op("pe", lambda e, k=k: e.matmul(ps[:, 0:n], xT[:, k, t0:t0 + 128], w[:, k, n0:n0 + n],
                                                    start=(k == 0), stop=(k == nk - 1)),
                      reads=[w, xT], writes=[ps])

    def even_mixer(self, x_in, x_out, W, par, flags):
        cfg, S, nc = self.cfg, self.S, self.nc
        TOK, NT = cfg.TOK, cfg.NT
        with ExitStack() as ph:
            sb = lambda n, s, d=F32: self.sb(ph, n, s, d)
            w_in = sb("w_in", [128, 8, 2048], BF16)
            w_out = sb("w_out", [128, 8, D], BF16)
            wsT = sb("wsT", [128, 4, 128], BF16)
            P = sb("par", [128, EP_N])
            fl = sb("fl", [128, 2 * NT])
            self.load_w(w_in, W["w_in"].ap(), 8)
            self.load_w(w_out, W["w_out"].ap(), 8)
            S.dma("pool", wsT[:], W["wsT"].ap(), writes=[wsT])
            S.dma("sp", P[:], par.ap(), writes=[P])
            S.dma("sp", fl[:], flags.ap(), writes=[fl])
            x_ts = [sb(f"x_t{i}", [128, 4, D]) for i in range(2)]
            xhs = [sb(f"xh{i}", [32, 1, D]) for i in range(2)]
            xn_b = sb("xn_b", [128, 4, D], BF16)
            xnh_b = sb("xnh_b", [32, 1, D], BF16)
            xnTs = [sb(f"xnT{i}", [128, 8, 512], BF16) for i in range(2)]
            xnhT = sb("xnhT", [128, 8, 32], BF16)
            junk = sb("junk", [128, D], BF16)
            ss = sb("ss", [128, 8]); rstd = sb("rstd", [128, 8])
            ssh = sb("ssh", [32, 8]); rstdh = sb("rstdh", [32, 8])
            uT = sb("uT", [128, 4, 512])
            gv = sb("gv", [128, 512])
            bst = sb("bst", [128, 1, 6]); mv = sb("mv", [128, 2]); vr = sb("vr", [128, 1])
            v_b = sb("v_b", [128, 4, 512], BF16)
            sig = sb("sig", [128, 512])
            gh = sb("gh", [128, 4, 32]); sigh = sb("sigh", [128, 4, 32])
            gT = sb("gT", [128, 4, 544])
            cT = sb("cT", [128, 4, 512])
            cT3 = sb("cT3", [128, 512])
            ctmp = [sb(f"ctmp{j}", [128, 512]) for j in range(3)]
            csq = sb("csq", [128, 4, 512])
            mean = sb("mean", [128, 512]); var = sb("var", [128, 512]); msq = sb("msq", [128, 512])
            tmp4 = sb("tmp4", [128, 4, 128])
            actT = sb("actT", [128, 8, 512], BF16)
            d_in = S.buf("d_xin"); d_out = S.buf("d_xout")
            for i in range(NT):
                x_t = x_ts[i % 2]; xh = xhs[i % 2]; xnT = xnTs[i % 2]
                t0 = i * 512
                lo0 = max(t0 - 16, 0); hi0 = min(t0 + 512, TOK - 16)
                S.dma("sp", x_t[:], x_in.ap()[t0:t0 + 512, :].rearrange("(b p) d -> p b d", p=128), reads=[d_in], writes=[x_t])
                S.dma("sp", xh[0:16, 0, :], x_in.ap()[lo0:lo0 + 16, :], reads=[d_in], writes=[xh])
                S.dma("sp", xh[16:32, 0, :], x_in.ap()[hi0:hi0 + 16, :], reads=[d_in], writes=[xh])
                self._rms(x_t, 4, P, EP["gamma"], xn_b, ss, rstd, junk, 128)
                self._rms(xh, 1, P, EP["gamma"], xnh_b, ssh, rstdh, junk, 32)
                self.transpose_blocks(xn_b, 4, xnT)
                self.transpose_blocks(xnh_b, 1, xnhT, rows=32)
                if i == 1:
                    self.dbg("e_xn", xn_b, xn_b[:], [128, 4, D], BF16)
                    self.dbg("e_xnT", xnT, xnT[:], [128, 8, 512], BF16)
                    self.dbg("e_xnhT", xnhT, xnhT[:], [128, 8, 32], BF16)
                psh = self.psx
                for c in range(4):
                    pv = self.nps(); pg = self.nps()
                    self.proj_fm(pv, w_in, 1024 + c * 128, xnT)
                    self.proj_fm(pg, w_in, 1536 + c * 128, xnT)
                    S.op("act", lambda e, pg=pg: e.activation(out=sig[:, :], in_=pg[:, :], func=AF.Sigmoid), reads=[pg], writes=[sig])
                    S.op("dve", lambda e, pv=pv, c=c: e.tensor_tensor(out=gT[:, c, 16:528], in0=pv[:, :], in1=sig[:, :], op=ALU.mult),
                         reads=[pv, sig], writes=[gT])
                    for k in range(8):
                        S.op("pe", lambda e, k=k, c=c: e.matmul(psh[:, c * 64:c * 64 + 32], w_in[:, k, 1024 + c * 128:1024 + (c + 1) * 128],
                                                                xnhT[:, k, :], start=(k == 0), stop=(k == 7)), reads=[w_in, xnhT], writes=[psh])
                    for k in range(8):
                        S.op("pe", lambda e, k=k, c=c: e.matmul(psh[:, c * 64 + 32:c * 64 + 64], w_in[:, k, 1536 + c * 128:1536 + (c + 1) * 128],
                                                                xnhT[:, k, :], start=(k == 0), stop=(k == 7)), reads=[w_in, xnhT], writes=[psh])
                psh3 = psh.t[:, 0:256].rearrange("p (c t) -> p c t", c=4)
                S.op("act", lambda e, psh3=psh3: e.activation(out=sigh[:], in_=psh3[:, :, 32:64], func=AF.Sigmoid), reads=[psh], writes=[sigh])
                S.op("dve", lambda e, psh3=psh3: e.tensor_tensor(out=gh[:], in0=psh3[:, :, 0:32], in1=sigh[:], op=ALU.mult), reads=[psh, sigh], writes=[gh])
                S.op("dve", lambda e, i=i: e.tensor_scalar(out=gT[:, :, 0:16], in0=gh[:, :, 0:16], scalar1=fl[:, 2 * i:2 * i + 1], scalar2=None, op0=ALU.mult),
                     reads=[gh, fl], writes=[gT])
                S.op("dve", lambda e, i=i: e.tensor_scalar(out=gT[:, :, 528:544], in0=gh[:, :, 16:32], scalar1=fl[:, 2 * i + 1:2 * i + 2], scalar2=None, op0=ALU.mult),
                     reads=[gh, fl], writes=[gT])
                if i == 1:
                    self.dbg("e_gT", gT, gT[:], [128, 4, 544])
                cw0 = EP["cw"]; cb0 = EP["cb"]
                for c in range(3):
                    S.op("dve", lambda e, c=c: e.tensor_scalar(out=cT[:, c, :], in0=gT[:, c, 1:513], scalar1=P[:, cw0 + c * 31:cw0 + c * 31 + 1],
                                                               scalar2=P[:, cb0 + c:cb0 + c + 1], op0=ALU.mult, op1=ALU.add),
                         reads=[gT, P], writes=[cT])
                    for k in range(1, 31):
                        S.op("dve", lambda e, c=c, k=k: e.scalar_tensor_tensor(out=cT[:, c, :], in0=gT[:, c, k + 1:k + 513],
                                                                                scalar=P[:, cw0 + c * 31 + k:cw0 + c * 31 + k + 1],
                                                                                in1=cT[:, c, :], op0=ALU.mult, op1=ALU.add),
                             reads=[gT, P, cT], writes=[cT])
                c = 3
                S.op("pool", lambda e: e.tensor_scalar(out=cT3[:, :], in0=gT[:, 3, 1:513], scalar1=P[:, cw0 + 93:cw0 + 94],
                                                       scalar2=P[:, cb0 + 3:cb0 + 4], op0=ALU.mult, op1=ALU.add), reads=[gT, P], writes=[cT3])
                for k in range(1, 31):
                    tk = ctmp[k % 3]
                    S.op("act", lambda e, k=k, tk=tk: e.activation(out=tk[:, :], in_=gT[:, 3, k + 1:k + 513], func=AF.Copy,
                                                                   scale=P[:, cw0 + 93 + k:cw0 + 94 + k]), reads=[gT, P], writes=[tk])
                    S.op("pool", lambda e, tk=tk: e.tensor_tensor(out=cT3[:, :], in0=cT3[:, :], in1=tk[:, :], op=ALU.add), reads=[cT3, tk], writes=[cT3])
                S.op("pool", lambda e: e.tensor_copy(out=cT[:, 3, :], in_=cT3[:, :]), reads=[cT3], writes=[cT])
                if i == 1:
                    self.dbg("e_cT", cT, cT[:], [128, 4, 512])
                S.op("act", lambda e: e.activation(out=csq[:].rearrange("p c t -> p (c t)"), in_=cT[:].rearrange("p c t -> p (c t)"), func=AF.Square),
                     reads=[cT], writes=[csq])
                p1 = self.nps(); p2 = self.nps()
                for c in range(4):
                    S.op("pe", lambda e, c=c, p1=p1: e.matmul(p1[:, :], self.ones_f[:, :], cT[:, c, :], start=(c == 0), stop=(c == 3)),
                         reads=[self.ones_f, cT], writes=[p1])
                for c in range(4):
                    S.op("pe", lambda e, c=c, p2=p2: e.matmul(p2[:, :], self.ones_f[:, :], csq[:, c, :], start=(c == 0), stop=(c == 3)),
                         reads=[self.ones_f, csq], writes=[p2])
                S.op("act", lambda e, p1=p1: e.activation(out=mean[:, :], in_=p1[:, :], func=AF.Copy, scale=1.0 / 512), reads=[p1], writes=[mean])
                S.op("dve", lambda e: e.tensor_tensor(out=msq[:, :], in0=mean[:, :], in1=mean[:, :], op=ALU.mult), reads=[mean], writes=[msq])
                S.op("dve", lambda e, p2=p2: e.scalar_tensor_tensor(out=var[:, :], in0=p2[:, :], scalar=1.0 / 512, in1=msq[:, :], op0=ALU.mult, op1=ALU.subtract),
                     reads=[p2, msq], writes=[var])
                S.op("act", lambda e: e.activation(out=var[:, :], in_=var[:, :], func=AF.Ln, bias=EPS, scale=1.0), reads=[var], writes=[var])
                S.op("act", lambda e: e.activation(out=var[:, :], in_=var[:, :], func=AF.Exp, scale=-0.5), reads=[var], writes=[var])
                mb = mean.t[:, :].rearrange("p (o t) -> p o t", o=1).to_broadcast([128, 4, 512])
                vb = var.t[:, :].rearrange("p (o t) -> p o t", o=1).to_broadcast([128, 4, 512])
                S.op("pool", lambda e, mb=mb: e.tensor_tensor(out=cT[:], in0=cT[:], in1=mb, op=ALU.subtract), reads=[cT, mean], writes=[cT])
                S.op("dve", lambda e, vb=vb: e.tensor_tensor(out=cT[:], in0=cT[:], in1=vb, op=ALU.mult), reads=[cT, var], writes=[cT])
                for c in range(4):
                    S.op("act", lambda e, c=c: e.activation(out=actT[:, 4 + c, :], in_=cT[:, c, :], func=AF.Silu,
                                                            bias=P[:, EP["blb"] + c:EP["blb"] + c + 1], scale=P[:, EP["blg"] + c:EP["blg"] + c + 1]),
                         reads=[cT, P], writes=[actT])
                for c in range(4):
                    pu = self.nps()
                    self.proj_fm(pu, w_in, c * 128, xnT)
                    S.op("act", lambda e, pu=pu, c=c: e.activation(out=uT[:, c, :], in_=pu[:, :], func=AF.Gelu_apprx_tanh), reads=[pu], writes=[uT])
                for b in range(4):
                    pa = self.nps()
                    self.proj_tm(pa, xnT, b * 128, w_in, 512)
                    S.op("act", lambda e, pa=pa: e.activation(out=gv[:, :], in_=pa[:, :], func=AF.Gelu_apprx_tanh), reads=[pa], writes=[gv])
                    S.op("dve", lambda e: e.bn_stats(out=bst[:, 0, :], in_=gv[:, :]), reads=[gv], writes=[bst])
                    S.op("dve", lambda e: e.bn_aggr(out=mv[:, :], in_=bst[:, :, :]), reads=[bst], writes=[mv])
                    S.op("act", lambda e: e.activation(out=vr[:, :], in_=mv[:, 1:2], func=AF.Ln, bias=EPS, scale=1.0), reads=[mv], writes=[vr])
                    S.op("act", lambda e: e.activation(out=vr[:, :], in_=vr[:, :], func=AF.Exp, scale=-0.5), reads=[vr], writes=[vr])
                    S.op("dve", lambda e: e.tensor_scalar(out=gv[:, :], in0=gv[:, :], scalar1=mv[:, 0:1], scalar2=vr[:, 0:1], op0=ALU.subtract, op1=ALU.mult),
                         reads=[gv, mv, vr], writes=[gv])
                    S.op("dve", lambda e: e.tensor_tensor(out=gv[:, :], in0=gv[:, :], in1=P[:, EP["alg"]:EP["alg"] + 512], op=ALU.mult), reads=[gv, P], writes=[gv])
                    S.op("dve", lambda e, b=b: e.tensor_tensor(out=v_b[:, b, :], in0=gv[:, :], in1=P[:, EP["alb"]:EP["alb"] + 512], op=ALU.add), reads=[gv, P], writes=[v_b])
                    pm = self.nps()
                    for g in range(4):
                        S.op("pe", lambda e, g=g, b=b, pm=pm: e.matmul(pm[:, g * 128:(g + 1) * 128], v_b[:, b, g * 128:(g + 1) * 128], wsT[:, g, :], start=True, stop=True),
                             reads=[v_b, wsT], writes=[pm])
                    pm3 = pm.t[:, :].rearrange("p (g t) -> p g t", g=4)
                    bs3 = P.t[:, EP["bs"]:EP["bs"] + 512].rearrange("p (g t) -> p g t", g=4)
                    S.op("dve", lambda e, pm3=pm3, bs3=bs3: e.tensor_tensor(out=tmp4[:], in0=pm3, in1=bs3, op=ALU.add), reads=[pm, P], writes=[tmp4])
                    S.op("dve", lambda e, b=b: e.tensor_tensor(out=actT[:, 0:4, b * 128:(b + 1) * 128], in0=tmp4[:], in1=uT[:, :, b * 128:(b + 1) * 128], op=ALU.mult),
                         reads=[tmp4, uT], writes=[actT])
                if i == 1:
                    self.dbg("e_actT", actT, actT[:], [128, 8, 512], BF16)
                    self.dbg("e_uT", uT, uT[:], [128, 4, 512])
                    self.dbg("e_vb", v_b, v_b[:], [128, 4, 512], BF16)
                for b in range(4):
                    for h in range(2):
                        po = self.nps()
                        self.proj_tm(po, actT, b * 128, w_out, h * 512)
                        S.op("dve", lambda e, po=po, b=b, h=h, x_t=x_t: e.tensor_tensor(out=x_t[:, b, h * 512:(h + 1) * 512], in0=x_t[:, b, h * 512:(h + 1) * 512],
                                                                                        in1=po[:, :], op=ALU.add), reads=[x_t, po], writes=[x_t])
                S.dma("sp", x_out.ap()[t0:t0 + 512, :].rearrange("(b p) d -> p b d", p=128), x_t[:], reads=[x_t], writes=[d_out])
            S.end_phase()

    def _rms(self, x_t, nb, P, goff, out, ss, rstd, junk, rows):
        S = self.S
        S.op("pool", lambda e: e.memset(ss[0:rows, 0:nb], 0.0), writes=[ss])
        for b in range(nb):
            S.op("act", lambda e, b=b: e.activation(out=junk[0:rows, :], in_=x_t[0:rows, b, :], func=AF.Square,
                                                    accum_out=ss[0:rows, b:b + 1]), reads=[x_t, ss], writes=[junk, ss])
        S.op("act", lambda e: e.activation(out=rstd[0:rows, 0:nb], in_=ss[0:rows, 0:nb], func=AF.Ln, bias=EPS, scale=1.0 / D),
             reads=[ss], writes=[rstd])
        S.op("act", lambda e: e.activation(out=rstd[0:rows, 0:nb], in_=rstd[0:rows, 0:nb], func=AF.Exp, scale=-0.5),
             reads=[rstd], writes=[rstd])
        if out is None:
            return
        for b in range(nb):
            S.op("dve", lambda e, b=b: e.scalar_tensor_tensor(out=out[0:rows, b, :], in0=x_t[0:rows, b, :], scalar=rstd[0:rows, b:b + 1],
                                                              in1=P[0:rows, goff:goff + D], op0=ALU.mult, op1=ALU.mult),
                 reads=[x_t, rstd, P], writes=[out])


def _mk_layout(items):
    off, d = 0, {}
    for k, w in items:
        d[k] = off
        off += w
    return d, off


EP, EP_N = _mk_layout([("gamma", D), ("alg", 512), ("alb", 512), ("bs", 512), ("cw", 124), ("cb", 4), ("blg", 4), ("blb", 4)])

XP, XP_N = _mk_layout([("gx", D), ("gf", D)])


def _xattn_router(self, x_in, x_out, aff_out, W, par, mem):
    cfg, S, nc = self.cfg, self.S, self.nc
    TOK, NT, NSEG, SEG = cfg.TOK, cfg.NT, cfg.NSEG, cfg.SEG
    TPS = SEG // 512
    with ExitStack() as ph:
        sb = lambda n, s, d=F32: self.sb(ph, n, s, d)
        wq = sb("wq", [128, 8, D], BF16)
        wkv = sb("wkv", [128, 8, 2 * D], BF16)
        wo = sb("wo", [128, 8, D], BF16)
        wr = sb("wr", [128, 8, NE])
        P = sb("xpar", [128, XP_N])
        self.load_w(wq, W["wq"].ap(), 8)
        self.load_w(wkv, W["wkv"].ap(), 8)
        self.load_w(wo, W["wo"].ap(), 8)
        S.dma("sp", wr[:], W["wr"].ap().rearrange("(k p) n -> p k n", p=128), writes=[wr])
        S.dma("sp", P[:], par.ap(), writes=[P])
        mem_b = sb("mem_b", [128, 2, D], BF16)
        memT = sb("memT", [128, 8, MEM], BF16)
        KT = sb("KT", [128, 8, MEM], BF16)
        V_b = sb("V_b", [128, 2, D], BF16)
        x_ts = [sb(f"xa_t{i}", [128, 4, D]) for i in range(1)]
        xn_b = sb("xa_nb", [128, 4, D], BF16)
        xnT = sb("xa_nT", [128, 8, 512], BF16)
        qT = sb("qT", [128, 8, 512], BF16)
        ET = sb("ET", [128, 2, 512], BF16)
        rec = sb("rec", [128, 512])
        oT = sb("oT", [128, 8, 512], BF16)
        junk = sb("xa_junk", [128, D], BF16)
        ss = sb("xa_ss", [128, 8]); rstd = sb("xa_rstd", [128, 8])
        xn2 = sb("xn2", [128, 1, D])
        xn2T = sb("xn2T", [128, 8, 128])
        mx = sb("mx", [128, 1]); sm = sb("sm", [128, 1]); ex = sb("ex", [128, NE])
        aff = sb("aff", [128, cfg.NB, NE])
        d_in = S.buf("d_xin"); d_out = S.buf("d_xout"); d_mem = S.buf("d_mem")
        for i in range(NT):
            seg = i // TPS
            t0 = i * 512
            x_t = x_ts[0]
            S.dma("sp", x_t[:], x_in.ap()[t0:t0 + 512, :].rearrange("(b p) d -> p b d", p=128), reads=[d_in], writes=[x_t])
            if i % TPS == 0:
                S.dma("pool", mem_b[:], mem.ap()[seg].rearrange("(b p) d -> p b d", p=128), reads=[d_mem], writes=[mem_b])
                self.transpose_blocks(mem_b, 2, memT)
                for n in range(8):
                    pk = self.nps()
                    self.proj_fm(pk, wkv, n * 128, memT, n=MEM)
                    if n % 2 == 0:
                        S.op("act", lambda e, pk=pk, n=n: e.copy(out=KT[:, n, :], in_=pk[:, 0:MEM]), reads=[pk], writes=[KT])
                    else:
                        S.op("dve", lambda e, pk=pk, n=n: e.tensor_copy(out=KT[:, n, :], in_=pk[:, 0:MEM]), reads=[pk], writes=[KT])
                for mb in range(2):
                    for h in range(2):
                        pv = self.nps()
                        self.proj_tm(pv, memT, mb * 128, wkv, D + h * 512)
                        if h == 0:
                            S.op("act", lambda e, pv=pv, mb=mb, h=h: e.copy(out=V_b[:, mb, h * 512:(h + 1) * 512], in_=pv[:, :]), reads=[pv], writes=[V_b])
                        else:
                            S.op("dve", lambda e, pv=pv, mb=mb, h=h: e.tensor_copy(out=V_b[:, mb, h * 512:(h + 1) * 512], in_=pv[:, :]), reads=[pv], writes=[V_b])
            self._rms(x_t, 4, P, XP["gx"], xn_b, ss, rstd, junk, 128)
            self.transpose_blocks(xn_b, 4, xnT)
            for n in range(8):
                pq = self.nps()
                self.proj_fm(pq, wq, n * 128, xnT)
                if n % 2 == 0:
                    S.op("act", lambda e, pq=pq, n=n: e.copy(out=qT[:, n, :], in_=pq[:, :]), reads=[pq], writes=[qT])
                else:
                    S.op("dve", lambda e, pq=pq, n=n: e.tensor_copy(out=qT[:, n, :], in_=pq[:, :]), reads=[pq], writes=[qT])
            for h in range(4):
                for mc in range(2):
                    psc = self.nps()
                    for dc in range(2):
                        S.op("pe", lambda e, psc=psc, h=h, mc=mc, dc=dc: e.matmul(psc[:, :], KT[:, 2 * h + dc, mc * 128:(mc + 1) * 128], qT[:, 2 * h + dc, :],
                                                                                  start=(dc == 0), stop=(dc == 1)), reads=[KT, qT], writes=[psc])
                    S.op("act", lambda e, psc=psc, mc=mc: e.activation(out=ET[:, mc, :], in_=psc[:, :], func=AF.Exp, scale=1.0 / 16.0), reads=[psc], writes=[ET])
                pd = self.nps()
                for mc in range(2):
                    S.op("pe", lambda e, pd=pd, mc=mc: e.matmul(pd[:, :], self.ones_b[:, :], ET[:, mc, :], start=(mc == 0), stop=(mc == 1)),
                         reads=[self.ones_b, ET], writes=[pd])
                S.op("dve", lambda e, pd=pd: e.reciprocal(out=rec[:, :], in_=pd[:, :]), reads=[pd], writes=[rec])
                for dc in range(2):
                    po = self.nps()
                    for mc in range(2):
                        S.op("pe", lambda e, po=po, h=h, mc=mc, dc=dc: e.matmul(po[:, :], V_b[:, mc, h * 256 + dc * 128:h * 256 + (dc + 1) * 128], ET[:, mc, :],
                                                                                start=(mc == 0), stop=(mc == 1)), reads=[V_b, ET], writes=[po])
                    S.op("dve", lambda e, po=po, h=h, dc=dc: e.tensor_tensor(out=oT[:, 2 * h + dc, :], in0=po[:, :], in1=rec[:, :], op=ALU.mult),
                         reads=[po, rec], writes=[oT])
            if i == 0:
                self.dbg("x_qT", qT, qT[:], [128, 8, 512], BF16)
                self.dbg("x_KT", KT, KT[:], [128, 8, MEM], BF16)
                self.dbg("x_V", V_b, V_b[:], [128, 2, D], BF16)
                self.dbg("x_oT", oT, oT[:], [128, 8, 512], BF16)
            for b in range(4):
                for hh in range(2):
                    pp = self.nps()
                    self.proj_tm(pp, oT, b * 128, wo, hh * 512)
                    S.op("dve", lambda e, pp=pp, b=b, hh=hh, x_t=x_t: e.tensor_tensor(out=x_t[:, b, hh * 512:(hh + 1) * 512], in0=x_t[:, b, hh * 512:(hh + 1) * 512],
                                                                                      in1=pp[:, :], op=ALU.add), reads=[x_t, pp], writes=[x_t])
            S.dma("sp", x_out.ap()[t0:t0 + 512, :].rearrange("(b p) d -> p b d", p=128), x_t[:], reads=[x_t], writes=[d_out])
            self._rms(x_t, 4, P, XP["gf"], None, ss, rstd, junk, 128)
            for b in range(4):
                S.op("dve", lambda e, b=b, x_t=x_t: e.scalar_tensor_tensor(out=xn2[:, 0, :], in0=x_t[:, b, :], scalar=rstd[:, b:b + 1],
                                                                          in1=P[:, XP["gf"]:XP["gf"] + D], op0=ALU.mult, op1=ALU.mult),
                     reads=[x_t, rstd, P], writes=[xn2])
                pa, pb = self.nps(), self.nps()
                for k in range(8):
                    pt = pa if k < 4 else pb
                    S.op("pe", lambda e, pt=pt, k=k, b=b: e.transpose(pt[:, (k % 4) * 128:(k % 4 + 1) * 128], xn2[:, 0, k * 128:(k + 1) * 128], self.id_f[:, :]),
                         reads=[xn2, self.id_f], writes=[pt])
                S.op("act", lambda e, pa=pa: e.copy(out=xn2T[:, 0:4, :], in_=pa[:, :].rearrange("p (k t) -> p k t", k=4)), reads=[pa], writes=[xn2T])
                S.op("dve", lambda e, pb=pb: e.tensor_copy(out=xn2T[:, 4:8, :], in_=pb[:, :].rearrange("p (k t) -> p k t", k=4)), reads=[pb], writes=[xn2T])
                if i == 0 and b == 0:
                    self.dbg("x_xn2T", xn2T, xn2T[:], [128, 8, 128])
                pr = self.nps()
                for k in range(8):
                    S.op("pe", lambda e, pr=pr, k=k: e.matmul(pr[:, 0:NE], xn2T[:, k, :], wr[:, k, :], start=(k == 0), stop=(k == 7)), reads=[xn2T, wr], writes=[pr])
                S.op("dve", lambda e, pr=pr: e.tensor_reduce(out=mx[:, :], in_=pr[:, 0:NE], axis=AX.X, op=ALU.max), reads=[pr], writes=[mx])
                S.op("dve", lambda e: e.tensor_scalar(out=mx[:, :], in0=mx[:, :], scalar1=-1.0, scalar2=None, op0=ALU.mult), reads=[mx], writes=[mx])
                S.op("pool", lambda e: e.memset(sm[:, :], 0.0), writes=[sm])
                S.op("act", lambda e, pr=pr: e.activation(out=ex[:, :], in_=pr[:, 0:NE], func=AF.Exp, bias=mx[:, 0:1], scale=1.0, accum_out=sm[:, 0:1]),
                     reads=[pr, mx, sm], writes=[ex, sm])
                S.op("dve", lambda e: e.reciprocal(out=sm[:, :], in_=sm[:, :]), reads=[sm], writes=[sm])
                S.op("dve", lambda e, i=i, b=b: e.tensor_scalar(out=aff[:, i * 4 + b, :], in0=ex[:, :], scalar1=sm[:, 0:1], scalar2=None, op0=ALU.mult),
                     reads=[ex, sm], writes=[aff])
        d_aff = S.buf("d_aff")
        S.dma("sp", aff_out.ap(), aff[:].rearrange("p b e -> p (b e)"), reads=[aff], writes=[d_aff])
        S.end_phase()


Prog.xattn_router = _xattn_router


def _final_norm(self, x_in, y_out, par):
    cfg, S = self.cfg, self.S
    with ExitStack() as ph:
        sb = lambda n, s, d=F32: self.sb(ph, n, s, d)
        P = sb("fpar", [128, D])
        S.dma("sp", P[:], par.ap(), writes=[P])
        x_ts = [sb(f"f_t{i}", [128, 4, D]) for i in range(2)]
        y_ts = [sb(f"f_y{i}", [128, 4, D]) for i in range(2)]
        junk = sb("f_junk", [128, D], BF16)
        ss = sb("f_ss", [128, 8]); rstd = sb("f_rstd", [128, 8])
        d_in = S.buf("d_xin"); d_out = S.buf("d_yout")
        for i in range(cfg.NT):
            t0 = i * 512
            x_t, y_t = x_ts[i % 2], y_ts[i % 2]
            S.dma("sp", x_t[:], x_in.ap()[t0:t0 + 512, :].rearrange("(b p) d -> p b d", p=128), reads=[d_in], writes=[x_t])
            self._rms(x_t, 4, P, 0, y_t, ss, rstd, junk, 128)
            S.dma("sp", y_out.ap()[t0:t0 + 512, :].rearrange("(b p) d -> p b d", p=128), y_t[:], reads=[y_t], writes=[d_out])
        S.end_phase()


Prog.final_norm = _final_norm


def core_segments():
    out = []
    for c in range(NCORES):
        if c < 4:
            out.append([("s", c, j) for j in range(4)] + [("p", 2 * c, 0), ("p", 2 * c + 1, 0)])
        else:
            out.append([("p", 8 + 6 * (c - 4) + j, 0) for j in range(6)])
    return out


def shard_tokens(cfg, xp, xs):
    SEG = cfg.SEG
    res = []
    for segs in core_segments():
        parts = []
        for g, q, j in segs:
            parts.append(xp[q] if g == "p" else xs[q, j * SEG:(j + 1) * SEG])
        res.append(np.ascontiguousarray(np.concatenate(parts, 0)))
    return res


def unshard_tokens(cfg, per_core, n_p=32, n_s=4):
    SEG = cfg.SEG
    yp = np.zeros((n_p, SEG, D), np.float32)
    ys = np.zeros((n_s, 4 * SEG, D), np.float32)
    for c, segs in enumerate(core_segments()):
        for si, (g, q, j) in enumerate(segs):
            blk = per_core[c][si * SEG:(si + 1) * SEG]
            if g == "p":
                yp[q] = blk
            else:
                ys[q, j * SEG:(j + 1) * SEG] = blk
    return yp, ys


def shard_mem(cfg, mp, ms):
    res = []
    for segs in core_segments():
        res.append(np.ascontiguousarray(np.stack([mp[q] if g == "p" else ms[q] for g, q, j in segs], 0)))
    return res


def tile_flags(cfg):
    SEG, NT, NSEG = cfg.SEG, cfg.NT, cfg.NSEG
    res = []
    for segs in core_segments():
        cont = [False] + [segs[s][0] == segs[s - 1][0] and segs[s][1] == segs[s - 1][1] and segs[s][2] == segs[s - 1][2] + 1
                          for s in range(1, NSEG)] + [False]
        f = np.zeros((2 * NT,), np.float32)
        for i in range(NT):
            t0 = i * 512
            s = t0 // SEG
            f[2 * i] = 1.0 if (t0 % SEG != 0 or cont[s]) else 0.0
            e = t0 + 512
            f[2 * i + 1] = 1.0 if (e % SEG != 0 or cont[s + 1]) else 0.0
        res.append(np.ascontiguousarray(np.broadcast_to(f[None, :], (128, 2 * NT))))
    return res


def bc(v, n=128):
    v = np.asarray(v, np.float32).reshape(1, -1)
    return np.broadcast_to(v, (n, v.shape[1]))


def pack_even_params(norm_mix_l, a_ln_g, a_ln_b, a_bs, conv_w, conv_b, b_ln_g, b_ln_b):
    P = np.zeros((128, EP_N), np.float32)
    P[:, EP["gamma"]:EP["gamma"] + D] = bc(norm_mix_l)
    P[:, EP["alg"]:EP["alg"] + 512] = bc(a_ln_g)
    P[:, EP["alb"]:EP["alb"] + 512] = bc(a_ln_b)
    P[:, EP["bs"]:EP["bs"] + 512] = bc(a_bs.reshape(-1))
    P[:, EP["cw"]:EP["cw"] + 124] = conv_w.T.reshape(4, 128, 31).transpose(1, 0, 2).reshape(128, 124)
    P[:, EP["cb"]:EP["cb"] + 4] = conv_b.reshape(4, 128).T
    P[:, EP["blg"]:EP["blg"] + 4] = b_ln_g.reshape(4, 128).T
    P[:, EP["blb"]:EP["blb"] + 4] = b_ln_b.reshape(4, 128).T
    return P


def pack_xattn_params(norm_x_l, norm_f_l):
    P = np.zeros((128, XP_N), np.float32)
    P[:, XP["gx"]:XP["gx"] + D] = bc(norm_x_l)
    P[:, XP["gf"]:XP["gf"] + D] = bc(norm_f_l)
    return P


def _moe(self, x_in, x_out, aff_all, aff_loc, gm_loc, W, par, cnt_out):
    cfg, S, nc = self.cfg, self.S, self.nc
    TOK, NT, NB, SEG, FF, NF, CMAX = cfg.TOK, cfg.NT, cfg.NB, cfg.SEG, cfg.FF, cfg.NF, cfg.CMAX
    NBS_C = 4 * SEG // 128
    NFS = 4 * NBS_C
    NFP = NCORES * NB - NFS
    xes = [self.dint(f"xe{e}_{self.cn}", [CMAX, D], BF16) for e in range(NE)]
    d_xe = [S.buf(f"d_xe{e}") for e in range(NE)]
    with ExitStack() as mo:
        posI = self.sb(mo, "posI", [128, NB, NE], I32)
        gate = self.sb(mo, "gate", [128, NB, NE])
        thr = self.sb(mo, "thr", [128, 2 * NE])
        with ExitStack() as ph:
            sb = lambda n, s, d=F32: self.sb(ph, n, s, d)
            A_P = sb("A_P", [128, NFP, NE]); A_S = sb("A_S", [128, NFS, NE])
            cmp_ = sb("cmp", [128, NFP, NE], BF16)
            lo = sb("lo", [128, 2 * NE]); hi = sb("hi", [128, 2 * NE]); mid = sb("mid", [128, 2 * NE])
            cnt = sb("cnt", [128, 2 * NE]); cond = sb("cond", [128, 2 * NE]); t1 = sb("t1", [128, 2 * NE])
            capm = sb("capm", [128, 2 * NE])
            src = aff_all.ap().rearrange("p (f e) -> p f e", e=NE)
            d_aff = S.buf("d_affall")
            po, so = 0, 0
            for c in range(NCORES):
                f0 = c * NB
                if c < 4:
                    S.dma("sp", A_S[:, so:so + NBS_C, :], src[:, f0:f0 + NBS_C, :], reads=[d_aff], writes=[A_S]); so += NBS_C
                    n = NB - NBS_C
                    S.dma("sp", A_P[:, po:po + n, :], src[:, f0 + NBS_C:f0 + NB, :], reads=[d_aff], writes=[A_P]); po += n
                else:
                    S.dma("sp", A_P[:, po:po + NB, :], src[:, f0:f0 + NB, :], reads=[d_aff], writes=[A_P]); po += NB
            S.op("pool", lambda e: e.memset(lo[:, :], 0.0), writes=[lo])
            S.op("pool", lambda e: e.memset(hi[:, :], 1.0), writes=[hi])
            S.op("pool", lambda e: e.memset(mid[:, :], 0.5), writes=[mid])
            S.op("pool", lambda e: e.memset(capm[:, 0:NE], cfg.CAP_P + 0.5), writes=[capm])
            S.op("pool", lambda e: e.memset(capm[:, NE:2 * NE], cfg.CAP_S + 0.5), writes=[capm])
            for it in range(32):
                for (A, nf, c0) in ((A_P, NFP, 0), (A_S, NFS, NE)):
                    mb = mid.t[:, c0:c0 + NE].rearrange("p (o e) -> p o e", o=1).to_broadcast([128, nf, NE])
                    S.op("dve", lambda e, A=A, nf=nf, mb=mb: e.tensor_tensor(out=cmp_[:, 0:nf, :], in0=A[:, :, :], in1=mb, op=ALU.is_gt),
                         reads=[A, mid], writes=[cmp_])
                    S.op("dve", lambda e, nf=nf, c0=c0: e.tensor_reduce(out=cnt[:, c0:c0 + NE], in_=cmp_[:, 0:nf, :].rearrange("p f e -> p e f"), axis=AX.X, op=ALU.add),
                         reads=[cmp_], writes=[cnt])
                pt = self.nps()
                S.op("pe", lambda e, pt=pt: e.matmul(pt[:, 0:2 * NE], self.ones_f[:, :], cnt[:, :], start=True, stop=True), reads=[self.ones_f, cnt], writes=[pt])
                S.op("dve", lambda e, pt=pt: e.tensor_tensor(out=cond[:, :], in0=pt[:, 0:2 * NE], in1=capm[:, :], op=ALU.is_gt), reads=[pt, capm], writes=[cond])
                S.op("dve", lambda e: e.tensor_tensor(out=t1[:, :], in0=cond[:, :], in1=mid[:, :], op=ALU.mult), reads=[cond, mid], writes=[t1])
                S.op("dve", lambda e: e.tensor_tensor(out=lo[:, :], in0=lo[:, :], in1=t1[:, :], op=ALU.max), reads=[lo, t1], writes=[lo])
                S.op("dve", lambda e: e.scalar_tensor_tensor(out=t1[:, :], in0=cond[:, :], scalar=4.0, in1=mid[:, :], op0=ALU.mult, op1=ALU.add), reads=[cond, mid], writes=[t1])
                S.op("dve", lambda e: e.tensor_tensor(out=hi[:, :], in0=hi[:, :], in1=t1[:, :], op=ALU.min), reads=[hi, t1], writes=[hi])
                S.op("dve", lambda e: e.tensor_tensor(out=t1[:, :], in0=lo[:, :], in1=hi[:, :], op=ALU.add), reads=[lo, hi], writes=[t1])
                S.op("dve", lambda e: e.tensor_scalar(out=mid[:, :], in0=t1[:, :], scalar1=0.5, scalar2=None, op0=ALU.mult), reads=[t1], writes=[mid])
            S.op("dve", lambda e: e.tensor_copy(out=thr[:, :], in_=hi[:, :]), reads=[hi], writes=[thr])
            S.end_phase()
        with ExitStack() as ph:
            sb = lambda n, s, d=F32: self.sb(ph, n, s, d)
            NC_ = NB * NE
            AL = sb("AL", [128, NB, NE]); gm = sb("gm", [128, NB])
            tt = sb("tt", [128, NB, NE]); sel = sb("sel", [128, NB, NE]); sel_b = sb("sel_b", [128, NB, NE], BF16)
            incl = sb("incl", [128, NC_]); ca = sb("ca", [128, NC_]); cb = sb("cb", [128, NC_]); cn0 = sb("cn0", [128, NC_])
            dthr = sb("dthr", [128, NE])
            d_al = S.buf("d_al")
            S.dma("sp", AL[:], aff_loc.ap().rearrange("p (f e) -> p f e", e=NE), reads=[d_al], writes=[AL])
            S.dma("sp", gm[:], gm_loc.ap(), reads=[d_al], writes=[gm])
            S.op("dve", lambda e: e.tensor_tensor(out=dthr[:, :], in0=thr[:, 0:NE], in1=thr[:, NE:2 * NE], op=ALU.subtract), reads=[thr], writes=[dthr])
            gmb = gm.t[:, :].rearrange("p (f o) -> p f o", o=1).to_broadcast([128, NB, NE])
            dtb = dthr.t[:, :].rearrange("p (o e) -> p o e", o=1).to_broadcast([128, NB, NE])
            tsb = thr.t[:, NE:2 * NE].rearrange("p (o e) -> p o e", o=1).to_broadcast([128, NB, NE])
            S.op("dve", lambda e: e.tensor_tensor(out=tt[:], in0=gmb, in1=dtb, op=ALU.mult), reads=[gm, dthr], writes=[tt])
            S.op("dve", lambda e: e.tensor_tensor(out=tt[:], in0=tt[:], in1=tsb, op=ALU.add), reads=[tt, thr], writes=[tt])
            S.op("dve", lambda e: e.tensor_tensor(out=sel[:], in0=AL[:], in1=tt[:], op=ALU.is_gt), reads=[AL, tt], writes=[sel])
            S.op("dve", lambda e: e.tensor_copy(out=sel_b[:], in_=sel[:]), reads=[sel], writes=[sel_b])
            S.op("dve", lambda e: e.tensor_tensor(out=gate[:], in0=AL[:], in1=sel[:], op=ALU.mult), reads=[AL, sel], writes=[gate])
            selb2 = sel_b.t[:].rearrange("p f e -> p (f e)")
            for c0 in range(0, NC_, 512):
                n = min(512, NC_ - c0)
                pi = self.nps(); pc = self.nps()
                S.op("pe", lambda e, pi=pi, c0=c0, n=n: e.matmul(pi[:, 0:n], self.tri_b[:, :], selb2[:, c0:c0 + n], start=True, stop=True), reads=[self.tri_b, sel_b], writes=[pi])
                S.op("pe", lambda e, pc=pc, c0=c0, n=n: e.matmul(pc[:, 0:n], self.ones_b[:, :], selb2[:, c0:c0 + n], start=True, stop=True), reads=[self.ones_b, sel_b], writes=[pc])
                S.op("act", lambda e, pi=pi, c0=c0, n=n: e.copy(out=incl[:, c0:c0 + n], in_=pi[:, 0:n]), reads=[pi], writes=[incl])
                S.op("dve", lambda e, pc=pc, c0=c0, n=n: e.tensor_copy(out=cn0[:, c0:c0 + n], in_=pc[:, 0:n]), reads=[pc], writes=[cn0])
            cur = cn0
            sft = 1
            k = 0
            while sft < NB:
                nxt = ca if k % 2 == 0 else cb
                w = sft * NE
                S.op("dve", lambda e, cur=cur, nxt=nxt, w=w: e.tensor_copy(out=nxt[:, 0:w], in_=cur[:, 0:w]), reads=[cur], writes=[nxt])
                S.op("dve", lambda e, cur=cur, nxt=nxt, w=w: e.tensor_tensor(out=nxt[:, w:NC_], in0=cur[:, w:NC_], in1=cur[:, 0:NC_ - w], op=ALU.add), reads=[cur], writes=[nxt])
                cur = nxt
                sft *= 2
                k += 1
            d_cnt = S.buf("d_cnt")
            S.dma("sp", cnt_out.ap(), cur[:, NC_ - NE:NC_], reads=[cur], writes=[d_cnt])
            S.op("dve", lambda e, cur=cur: e.tensor_tensor(out=incl[:, :], in0=incl[:, :], in1=cur[:, :], op=ALU.add), reads=[incl, cur], writes=[incl])
            S.op("dve", lambda e: e.tensor_tensor(out=incl[:, :], in0=incl[:, :], in1=cn0[:, :], op=ALU.subtract), reads=[incl, cn0], writes=[incl])
            sel2 = sel.t[:].rearrange("p f e -> p (f e)")
            S.op("dve", lambda e: e.scalar_tensor_tensor(out=incl[:, :], in0=sel2, scalar=-1.0e6, in1=incl[:, :], op0=ALU.mult, op1=ALU.add), reads=[sel, incl], writes=[incl])
            S.op("dve", lambda e: e.tensor_scalar(out=incl[:, :], in0=incl[:, :], scalar1=1.0e6 - 1.0, scalar2=None, op0=ALU.add), reads=[incl], writes=[incl])
            S.op("dve", lambda e: e.tensor_copy(out=posI[:].rearrange("p f e -> p (f e)"), in_=incl[:, :]), reads=[incl], writes=[posI])
            S.end_phase()
        xn_d = self.dint(f"xn_d_{self.cn}", [TOK, D], BF16)
        meta_ds = [self.dint(f"meta{e}_{self.cn}", [CMAX, 2], F32) for e in range(NE)]
        d_xn = S.buf("d_xn"); d_xacc = S.buf("d_xacc"); d_meta = [S.buf(f"d_meta{e}") for e in range(NE)]
        with ExitStack() as ph:
            sb = lambda n, s, d=F32: self.sb(ph, n, s, d)
            P = sb("mpar", [128, D])
            S.dma("sp", P[:], par.ap(), writes=[P])
            x_ts = [sb(f"m_xt{i}", [128, 4, D]) for i in range(2)]
            xn_bs = [sb(f"m_xn{i}", [128, 4, D], BF16) for i in range(2)]
            junk = sb("m_junk", [128, D], BF16); ss = sb("m_ss", [128, 8]); rstd = sb("m_rstd", [128, 8])
            onesm = sb("m_ones", [128, 2 * (CMAX // 128)])
            d_in = S.buf("d_xin")
            S.op("pool", lambda e: e.memset(onesm[:, :], 1.0), writes=[onesm])
            for ex in range(NE):
                S.dma("sp", meta_ds[ex].ap().rearrange("(p r) c -> p (r c)", p=128), onesm[:, :], reads=[onesm], writes=[d_meta[ex]])
            for i in range(NT):
                t0 = i * 512
                x_t, xn_b = x_ts[i % 2], xn_bs[i % 2]
                S.dma("sp", x_t[:], x_in.ap()[t0:t0 + 512, :].rearrange("(b p) d -> p b d", p=128), reads=[d_in], writes=[x_t])
                S.dma("act", x_out.ap()[t0:t0 + 512, :].rearrange("(b p) d -> p b d", p=128), x_t[:], reads=[x_t], writes=[d_xacc])
                self._rms(x_t, 4, P, 0, xn_b, ss, rstd, junk, 128)
                S.dma("sp", xn_d.ap()[t0:t0 + 512, :].rearrange("(b p) d -> p b d", p=128), xn_b[:], reads=[xn_b], writes=[d_xn])
            S.end_phase()
        with ExitStack() as ph:
            sb = lambda n, s, d=F32: self.sb(ph, n, s, d)
            w1 = sb("w1", [128, 8, FF], BF16); w3 = sb("w3", [128, 8, FF], BF16); w2 = sb("w2", [128, NF, D], BF16)
            xe_t = sb("xe_t", [128, 4, D], BF16)
            xeT = sb("xeT", [128, 8, 512], BF16)
            hidT = sb("hidT", [128, NF, 512], BF16)
            s1s = [sb(f"s1_{i}", [128, 512]) for i in range(2)]
            y_ts = [sb(f"y_t{i}", [128, D]) for i in range(2)]
            scs = [sb(f"sc{i}", [128, 2, D], BF16) for i in range(2)]
            metaes = [sb(f"metae{i}", [128, NB, 2]) for i in range(2)]
            metats = [sb(f"metat{i}", [128, 2]) for i in range(4)]
            tokidx = sb("tokidx", [128, NB], I32)
            S.op("pool", lambda e: e.iota(tokidx[:, :], pattern=[[128, NB]], base=0, channel_multiplier=1), writes=[tokidx])
            yn = 0; sn = 0; mn = 0
            tiles_ = [(s0, min(512, CMAX - s0)) for s0 in range(0, CMAX, 512)]
            for ex in range(NE):
                me = metaes[ex % 2]
                me_i = me.t.bitcast(I32)
                S.op("dve", lambda e, me=me, ex=ex: e.tensor_copy(out=me[:, :, 0:1], in_=gate[:, :, ex:ex + 1]), reads=[gate], writes=[me])
                S.op("dve", lambda e, me=me, me_i=me_i: e.tensor_copy(out=me_i[:, :, 1:2], in_=tokidx[:, :].rearrange("p (f o) -> p f o", o=1)), reads=[tokidx], writes=[me])
                for g2 in range(NB // 2):
                    sc = scs[sn % 2]; sn += 1
                    S.dma("sp", sc[:], xn_d.ap()[g2 * 256:(g2 + 1) * 256, :].rearrange("(b p) d -> p b d", p=128), reads=[d_xn], writes=[sc])
                    for b in range(2):
                        f = g2 * 2 + b
                        S.op("pool", lambda e, ex=ex, f=f, b=b, sc=sc: e.indirect_dma_start(
                            out=xes[ex].ap(), out_offset=bass.IndirectOffsetOnAxis(ap=posI[:, f, ex:ex + 1], axis=0),
                            in_=sc[:, b, :], in_offset=None, bounds_check=self.breg(e), oob_is_err=False),
                            reads=[sc, posI], writes=[d_xe[ex]], dma_key=d_xe[ex])
                        S.op("pool", lambda e, ex=ex, f=f, me=me: e.indirect_dma_start(
                            out=meta_ds[ex].ap(), out_offset=bass.IndirectOffsetOnAxis(ap=posI[:, f, ex:ex + 1], axis=0),
                            in_=me[:, f, :], in_offset=None, bounds_check=self.breg(e), oob_is_err=False),
                            reads=[me, posI, d_meta[ex]], writes=[d_meta[ex]], dma_key=d_meta[ex])
                self.load_w(w1, W["w1"].ap()[ex], 8)
                self.load_w(w3, W["w3"].ap()[ex], 8)
                self.load_w(w2, W["w2"].ap()[ex], NF)
                for (s0, tw) in tiles_:
                    nbk = tw // 128
                    S.dma("sp", xe_t[:, 0:nbk, :], xes[ex].ap()[s0:s0 + tw, :].rearrange("(b p) d -> p b d", p=128), reads=[d_xe[ex]], writes=[xe_t])
                    self.transpose_blocks(xe_t, nbk, xeT)
                    for fc in range(NF):
                        p1 = self.nps(); p3 = self.nps()
                        self.proj_fm(p1, w1, fc * 128, xeT, n=tw)
                        self.proj_fm(p3, w3, fc * 128, xeT, n=tw)
                        s1 = s1s[fc % 2]
                        S.op("act", lambda e, p1=p1, s1=s1, tw=tw: e.activation(out=s1[:, 0:tw], in_=p1[:, 0:tw], func=AF.Silu), reads=[p1], writes=[s1])
                        S.op("dve", lambda e, p3=p3, s1=s1, fc=fc, tw=tw: e.tensor_tensor(out=hidT[:, fc, 0:tw], in0=p3[:, 0:tw], in1=s1[:, 0:tw], op=ALU.mult), reads=[p3, s1], writes=[hidT])
                    for b in range(nbk):
                        y_t = y_ts[yn % 2]; yn += 1
                        mt = metats[mn % 4]; mn += 1
                        mt_i = mt.t.bitcast(I32)
                        S.dma("sp", mt[:, :], meta_ds[ex].ap()[s0 + b * 128:s0 + (b + 1) * 128, :], reads=[d_meta[ex]], writes=[mt])
                        for hh in range(2):
                            py = self.nps()
                            for fc in range(NF):
                                S.op("pe", lambda e, py=py, fc=fc, b=b, hh=hh: e.matmul(py[:, :], hidT[:, fc, b * 128:(b + 1) * 128], w2[:, fc, hh * 512:(hh + 1) * 512],
                                                                                        start=(fc == 0), stop=(fc == NF - 1)), reads=[hidT, w2], writes=[py])
                            if hh == 0:
                                S.op("act", lambda e, py=py, y_t=y_t, mt=mt: e.activation(out=y_t[:, 0:512], in_=py[:, :], func=AF.Copy, scale=mt[:, 0:1]), reads=[py, mt], writes=[y_t])
                            else:
                                S.op("dve", lambda e, py=py, y_t=y_t, mt=mt: e.tensor_scalar(out=y_t[:, 512:1024], in0=py[:, :], scalar1=mt[:, 0:1], scalar2=None, op0=ALU.mult), reads=[py, mt], writes=[y_t])
                        S.op("pool", lambda e, y_t=y_t, mt_i=mt_i: e.indirect_dma_start(
                            out=x_out.ap(), out_offset=bass.IndirectOffsetOnAxis(ap=mt_i[:, 1:2], axis=0),
                            in_=y_t[:, :], in_offset=None, bounds_check=self.breg2(e), oob_is_err=False, compute_op=ALU.add),
                            reads=[y_t, mt, d_xacc], writes=[d_xacc], dma_key=d_xacc)
            S.end_phase()


Prog.moe = _moe


def group_mask(cfg):
    res = []
    for segs in core_segments():
        m = np.zeros((cfg.NB,), np.float32)
        bps = cfg.SEG // 128
        for si, (g, q, j) in enumerate(segs):
            m[si * bps:(si + 1) * bps] = 1.0 if g == "p" else 0.0
        res.append(np.ascontiguousarray(np.broadcast_to(m[None, :], (128, cfg.NB))))
    return res


OP, OP_N = _mk_layout([("gamma", D), ("hg", D), ("bg", 16)])


def _odd_mixer(self, x_in, x_out, W, par, flags):
    cfg, S, nc = self.cfg, self.S, self.nc
    TOK, NT = cfg.TOK, cfg.NT
    HD = 256
    hf_d = self.dint(f"hf_{self.cn}", [TOK, HD], F32)
    d_hf = S.buf("d_hf")
    d_xo = S.buf("d_xo_odd")
    for h in range(4):
        for dirn in (0, 1):
            with ExitStack() as ph:
                sb = lambda n, s_, d=F32: self.sb(ph, n, s_, d)
                bwd = dirn == 1
                wq = sb("o_wq", [128, 8, HD], BF16); wk = sb("o_wk", [128, 8, HD], BF16); wv = sb("o_wv", [128, 8, HD], BF16)
                wgt = sb("o_wg", [128, 8, 256], BF16)
                P = sb("o_par", [128, OP_N]); fl = sb("o_fl", [128, 2 * NT])
                win = W["w_in"].ap()
                self.load_w(wq, win[:, h * HD:(h + 1) * HD], 8, ndma=2)
                self.load_w(wk, win[:, D + h * HD:D + (h + 1) * HD], 8, ndma=2)
                self.load_w(wv, win[:, 2 * D + h * HD:2 * D + (h + 1) * HD], 8, ndma=2)
                ci = (8 if bwd else 0) + h
                cf = (12 if bwd else 4) + h
                wgr = W["wg_rep"].ap().rearrange("(k p) n -> p k n", p=128)
                S.dma("pool", wgt[:, :, 0:128], wgr[:, :, ci * 128:(ci + 1) * 128], writes=[wgt])
                S.dma("pool", wgt[:, :, 128:256], wgr[:, :, cf * 128:(cf + 1) * 128], writes=[wgt])
                S.dma("sp", P[:], par.ap(), writes=[P])
                S.dma("sp", fl[:], flags.ap(), writes=[fl])
                if bwd:
                    wo = sb("o_wo", [128, 8, HD], BF16); wout = sb("o_wout", [128, 2, D], BF16)
                    self.load_w(wo, win[:, 3 * D + h * HD:3 * D + (h + 1) * HD], 8, ndma=2)
                    self.load_w(wout, W["w_out"].ap()[h * HD:(h + 1) * HD, :], 2, ndma=1)
                    o_sig = sb("o_sig", [128, 4, HD], BF16)
                    hf_t = sb("hf_t", [128, 4, HD]); hsq = sb("hsq", [128, HD], BF16)
                    hss = sb("hss", [128, 4]); hrs = sb("hrs", [128, 4])
                    hs_b = sb("hs_b", [128, 4, HD], BF16); outT = sb("outT", [128, 2, 512], BF16)
                    xacc = sb("xacc", [128, 4, D])
                x_t = sb("o_xt", [128, 4, D]); xn_b = sb("o_xn", [128, 4, D], BF16); xnT = sb("o_xnT", [128, 8, 512], BF16)
                junk = sb("o_junk", [128, D], BF16); ss = sb("o_ss", [128, 8]); rstd = sb("o_rstd", [128, 8])
                qT = sb("o_qT", [128, 2, 512], BF16); kT = sb("o_kT", [128, 2, 512], BF16)
                ktok = sb("o_ktok", [128, 4, HD], BF16); v_aug = sb("o_vaug", [128, 4, HD + 1], BF16)
                ib = sb("o_ib", [128, 512]); Lb = sb("o_Lb", [128, 512]); Bnb = sb("o_Bnb", [128, 512]); betab = sb("o_beta", [128, 512])
                Rb = sb("o_Rb", [128, 512]); nRb = sb("o_nRb", [128, 512]); BmRb = sb("o_BmR", [128, 512])
                cB = sb("o_cB", [128, 1]); cR = sb("o_cR", [128, 1]); nbf = sb("o_nbf", [128, 1])
                cols = [sb(f"o_cols{i}", [128, 8]) for i in range(2)]
                dj = sb("o_dj", [128, 128])
                Es = [sb(f"o_E{i}", [128, 128]) for i in range(2)]
                smTs = [sb(f"o_smT{i}", [128, 128], BF16) for i in range(2)]
                Bss = [sb(f"o_Bs{i}", [128, HD + 1]) for i in range(2)]
                numas = [sb(f"o_num{i}", [128, HD + 1]) for i in range(2)]
                wgvs = [sb(f"o_wgv{i}", [128, HD + 1], BF16) for i in range(2)]
                C = sb("o_C", [128, 2, HD + 1]); C_bf = sb("o_Cbf", [128, 2, HD + 1], BF16)
                h_t = sb("o_ht", [128, 4, HD])
                d_in = S.buf("d_xin_o")
                S.op("pool", lambda e: e.memset(C[:], 0.0), writes=[C])
                S.op("pool", lambda e: e.memset(C_bf[:], 0.0), writes=[C_bf])
                S.op("pool", lambda e: e.memset(cB[:], 0.0), writes=[cB])
                S.op("pool", lambda e: e.memset(cR[:], 0.0), writes=[cR])
                S.op("pool", lambda e: e.memset(v_aug[:, :, HD:HD + 1], 1.0), writes=[v_aug])
                bfc = OP["bg"] + cf; bic = OP["bg"] + ci
                S.op("dve", lambda e: e.tensor_scalar(out=nbf[:, :], in0=P[:, bfc:bfc + 1], scalar1=-1.0, scalar2=None, op0=ALU.mult), reads=[P], writes=[nbf])
                mask = self.tril_f if bwd else self.tri_f
                tiles = range(NT - 1, -1, -1) if bwd else range(NT)
                cn = 0
                for i in tiles:
                    t0 = i * 512
                    S.dma("sp", x_t[:], x_in.ap()[t0:t0 + 512, :].rearrange("(b p) d -> p b d", p=128), reads=[d_in], writes=[x_t])
                    if bwd:
                        S.dma("sp", hf_t[:], hf_d.ap()[t0:t0 + 512, :].rearrange("(b p) d -> p b d", p=128), reads=[d_hf], writes=[hf_t])
                        src = x_in if h == 0 else x_out
                        S.dma("sp", xacc[:], src.ap()[t0:t0 + 512, :].rearrange("(b p) d -> p b d", p=128), reads=[d_in, d_xo], writes=[xacc])
                    self._rms(x_t, 4, P, OP["gamma"], xn_b, ss, rstd, junk, 128)
                    self.transpose_blocks(xn_b, 4, xnT)
                    for ec in range(2):
                        pq = self.nps(); self.proj_fm(pq, wq, ec * 128, xnT)
                        S.op("act", lambda e, pq=pq, ec=ec: e.copy(out=qT[:, ec, :], in_=pq[:, :]), reads=[pq], writes=[qT])
                        pk = self.nps(); self.proj_fm(pk, wk, ec * 128, xnT)
                        S.op("dve", lambda e, pk=pk, ec=ec: e.tensor_scalar(out=kT[:, ec, :], in0=pk[:, :], scalar1=1.0 / 16.0, scalar2=None, op0=ALU.mult), reads=[pk], writes=[kT])
                    for b in range(4):
                        pk2 = self.nps(); self.proj_tm(pk2, xnT, b * 128, wk, 0, n=HD)
                        S.op("act", lambda e, pk2=pk2, b=b: e.activation(out=ktok[:, b, :], in_=pk2[:, 0:HD], func=AF.Copy, scale=1.0 / 16.0), reads=[pk2], writes=[ktok])
                        pv = self.nps(); self.proj_tm(pv, xnT, b * 128, wv, 0, n=HD)
                        S.op("dve", lambda e, pv=pv, b=b: e.tensor_copy(out=v_aug[:, b, 0:HD], in_=pv[:, 0:HD]), reads=[pv], writes=[v_aug])
                        if bwd:
                            po_ = self.nps(); self.proj_tm(po_, xnT, b * 128, wo, 0, n=HD)
                            S.op("act", lambda e, po_=po_, b=b: e.activation(out=o_sig[:, b, :], in_=po_[:, 0:HD], func=AF.Sigmoid), reads=[po_], writes=[o_sig])
                    pgi = self.nps(); self.proj_fm(pgi, wgt, 0, xnT)
                    pgf = self.nps(); self.proj_fm(pgf, wgt, 128, xnT)
                    S.op("act", lambda e, pgi=pgi: e.activation(out=ib[:, :], in_=pgi[:, :], func=AF.Identity, bias=P[:, bic:bic + 1], scale=1.0), reads=[pgi, P], writes=[ib])
                    S.op("act", lambda e, pgf=pgf: e.activation(out=Lb[:, :], in_=pgf[:, :], func=AF.Exp, bias=nbf[:, 0:1], scale=-1.0), reads=[pgf, nbf], writes=[Lb])
                    S.op("act", lambda e: e.activation(out=Lb[:, :], in_=Lb[:, :], func=AF.Ln, bias=1.0, scale=1.0), reads=[Lb], writes=[Lb])
                    fcol = 2 * i + (1 if bwd else 0)
                    S.op("dve", lambda e, fcol=fcol: e.tensor_scalar(out=cB[:, :], in0=cB[:, :], scalar1=fl[:, fcol:fcol + 1], scalar2=None, op0=ALU.mult), reads=[cB, fl], writes=[cB])
                    S.op("dve", lambda e, fcol=fcol: e.tensor_scalar(out=cR[:, :], in0=cR[:, :], scalar1=fl[:, fcol:fcol + 1], scalar2=None, op0=ALU.mult), reads=[cR, fl], writes=[cR])
                    S.op("dve", lambda e, fcol=fcol: e.tensor_scalar(out=C[:].rearrange("p a b -> p (a b)"), in0=C[:].rearrange("p a b -> p (a b)"),
                                                                     scalar1=fl[:, fcol:fcol + 1], scalar2=None, op0=ALU.mult), reads=[C, fl], writes=[C])
                    S.op("act", lambda e: e.copy(out=C_bf[:].rearrange("p a b -> p (a b)"), in_=C[:].rearrange("p a b -> p (a b)")), reads=[C], writes=[C_bf])
                    rv = (lambda t: t.t[:, ::-1]) if bwd else (lambda t: t.t[:, :])
                    S.op("dve", lambda e: e.tensor_tensor_scan(out=rv(Bnb), data0=self.ones512[:, :], data1=rv(Lb), initial=cB[:, 0:1], op0=ALU.mult, op1=ALU.add),
                         reads=[Lb, cB, self.ones512], writes=[Bnb])
                    S.op("dve", lambda e: e.tensor_tensor(out=betab[:, :], in0=ib[:, :], in1=Bnb[:, :], op=ALU.add), reads=[ib, Bnb], writes=[betab])
                    S.op("dve", lambda e: e.tensor_tensor_scan(out=rv(Rb), data0=self.ones512[:, :], data1=rv(betab), initial=cR[:, 0:1], op0=ALU.mult, op1=ALU.max),
                         reads=[betab, cR, self.ones512], writes=[Rb])
                    S.op("dve", lambda e: e.tensor_scalar(out=nRb[:, :], in0=Rb[:, :], scalar1=-1.0, scalar2=None, op0=ALU.mult), reads=[Rb], writes=[nRb])
                    S.op("dve", lambda e: e.tensor_tensor(out=BmRb[:, :], in0=Bnb[:, :], in1=Rb[:, :], op=ALU.subtract), reads=[Bnb, Rb], writes=[BmRb])
                    chunks = range(3, -1, -1) if bwd else range(4)
                    first = True
                    for c in chunks:
                        c0 = c * 128
                        endc = c0 if bwd else c0 + 127
                        if first:
                            mu_buf, mu_ap = cR, cR.t[:, 0:1]
                        else:
                            pc_ = (c + 1) * 128 if bwd else c0 - 1
                            mu_buf, mu_ap = Rb, Rb.t[:, pc_:pc_ + 1]
                        first = False
                        col = cols[cn % 2]; E = Es[cn % 2]; smT = smTs[cn % 2]; Bs = Bss[cn % 2]; numa = numas[cn % 2]; wgv = wgvs[cn % 2]
                        cn += 1
                        for (src_b, ci_) in ((betab, 0), (Rb, 1), (BmRb, 2)):
                            S.op("dve", lambda e, src_b=src_b, ci_=ci_, col=col, c0=c0: e.scalar_tensor_tensor(out=dj[:, :], in0=src_b[:, c0:c0 + 128], scalar=1.0, in1=self.id_f[:, :],
                                                                                                       op0=ALU.mult, op1=ALU.mult, accum_out=col[:, ci_:ci_ + 1]),
                                 reads=[src_b, self.id_f], writes=[dj, col])
                        S.op("act", lambda e, col=col, mu_ap=mu_ap: e.activation(out=col[:, 3:4], in_=col[:, 1:2], func=AF.Exp, bias=mu_ap, scale=-1.0), reads=[col, mu_buf], writes=[col])
                        S.op("act", lambda e, col=col: e.activation(out=col[:, 4:5], in_=col[:, 2:3], func=AF.Exp), reads=[col], writes=[col])
                        S.op("act", lambda e, col=col, endc=endc: e.activation(out=col[:, 5:6], in_=col[:, 0:1], func=AF.Exp, bias=nRb[:, endc:endc + 1], scale=1.0), reads=[col, nRb], writes=[col])
                        S.op("act", lambda e, col=col, endc=endc, mu_ap=mu_ap: e.activation(out=col[:, 6:7], in_=mu_ap, func=AF.Exp, bias=nRb[:, endc:endc + 1], scale=1.0),
                             reads=[col, nRb, mu_buf], writes=[col])
                        psS = self.nps()
                        for ec in range(2):
                            S.op("pe", lambda e, psS=psS, ec=ec, c0=c0: e.matmul(psS[:, 0:128], kT[:, ec, c0:c0 + 128], qT[:, ec, c0:c0 + 128], start=(ec == 0), stop=(ec == 1)),
                                 reads=[kT, qT], writes=[psS])
                        S.op("act", lambda e, E=E, col=col, c0=c0: e.activation(out=E[:, :], in_=Rb[:, c0:c0 + 128], func=AF.Exp, bias=col[:, 0:1], scale=-1.0), reads=[Rb, col], writes=[E])
                        S.op("dve", lambda e, E=E: e.tensor_tensor(out=E[:, :], in0=E[:, :], in1=mask[:, :], op=ALU.mult), reads=[E, mask], writes=[E])
                        S.op("dve", lambda e, E=E, smT=smT, psS=psS: e.tensor_tensor(out=smT[:, :], in0=psS[:, 0:128], in1=E[:, :], op=ALU.mult), reads=[psS, E], writes=[smT])
                        psA = self.nps(); psB = self.nps()
                        S.op("pe", lambda e, psA=psA, smT=smT, c=c: e.matmul(psA[:, 0:HD + 1], smT[:, :], v_aug[:, c, :], start=True, stop=True), reads=[smT, v_aug], writes=[psA])
                        for ec in range(2):
                            S.op("pe", lambda e, psB=psB, ec=ec, c0=c0: e.matmul(psB[:, 0:HD + 1], qT[:, ec, c0:c0 + 128], C_bf[:, ec, :], start=(ec == 0), stop=(ec == 1)),
                                 reads=[qT, C_bf], writes=[psB])
                        S.op("act", lambda e, psB=psB, Bs=Bs, col=col: e.activation(out=Bs[:, :], in_=psB[:, 0:HD + 1], func=AF.Copy, scale=col[:, 3:4]), reads=[psB, col], writes=[Bs])
                        S.op("dve", lambda e, psA=psA, Bs=Bs, numa=numa: e.tensor_tensor(out=numa[:, :], in0=psA[:, 0:HD + 1], in1=Bs[:, :], op=ALU.add), reads=[psA, Bs], writes=[numa])
                        S.op("act", lambda e, numa=numa, col=col: e.activation(out=col[:, 7:8], in_=numa[:, HD:HD + 1], func=AF.Abs), reads=[numa], writes=[col])
                        S.op("dve", lambda e, col=col: e.tensor_tensor(out=col[:, 7:8], in0=col[:, 7:8], in1=col[:, 4:5], op=ALU.max), reads=[col], writes=[col])
                        S.op("dve", lambda e, col=col: e.reciprocal(out=col[:, 7:8], in_=col[:, 7:8]), reads=[col], writes=[col])
                        S.op("act", lambda e, numa=numa, col=col, c=c: e.activation(out=h_t[:, c, :], in_=numa[:, 0:HD], func=AF.Copy, scale=col[:, 7:8]), reads=[numa, col], writes=[h_t])
                        S.op("dve", lambda e, wgv=wgv, col=col, c=c: e.tensor_scalar(out=wgv[:, :], in0=v_aug[:, c, :], scalar1=col[:, 5:6], scalar2=None, op0=ALU.mult), reads=[v_aug, col], writes=[wgv])
                        for ec in range(2):
                            psU = self.nps()
                            S.op("pe", lambda e, psU=psU, ec=ec, wgv=wgv, c=c: e.matmul(psU[:, 0:HD + 1], ktok[:, c, ec * 128:(ec + 1) * 128], wgv[:, :], start=True, stop=True),
                                 reads=[ktok, wgv], writes=[psU])
                            S.op("dve", lambda e, psU=psU, ec=ec, col=col: e.scalar_tensor_tensor(out=C[:, ec, :], in0=C[:, ec, :], scalar=col[:, 6:7], in1=psU[:, 0:HD + 1],
                                                                                               op0=ALU.mult, op1=ALU.add), reads=[C, col, psU], writes=[C])
                        S.op("act", lambda e: e.copy(out=C_bf[:].rearrange("p a b -> p (a b)"), in_=C[:].rearrange("p a b -> p (a b)")), reads=[C], writes=[C_bf])
                    endt = 0 if bwd else 511
                    S.op("dve", lambda e, endt=endt: e.tensor_copy(out=cB[:, :], in_=Bnb[:, endt:endt + 1]), reads=[Bnb], writes=[cB])
                    S.op("dve", lambda e, endt=endt: e.tensor_copy(out=cR[:, :], in_=Rb[:, endt:endt + 1]), reads=[Rb], writes=[cR])
                    if not bwd:
                        S.dma("sp", hf_d.ap()[t0:t0 + 512, :].rearrange("(b p) d -> p b d", p=128), h_t[:], reads=[h_t], writes=[d_hf])
                    else:
                        S.op("dve", lambda e: e.tensor_tensor(out=h_t[:], in0=h_t[:], in1=hf_t[:], op=ALU.add), reads=[h_t, hf_t], writes=[h_t])
                        for b in range(4):
                            S.op("act", lambda e, b=b: e.activation(out=hsq[:, :], in_=h_t[:, b, :], func=AF.Square, accum_out=hss[:, b:b + 1]), reads=[h_t], writes=[hsq, hss])
                        S.op("act", lambda e: e.activation(out=hrs[:, :], in_=hss[:, :], func=AF.Ln, bias=EPS, scale=1.0 / HD), reads=[hss], writes=[hrs])
                        S.op("act", lambda e: e.activation(out=hrs[:, :], in_=hrs[:, :], func=AF.Exp, scale=-0.5), reads=[hrs], writes=[hrs])
                        hg0 = OP["hg"] + h * HD
                        for b in range(4):
                            S.op("dve", lambda e, b=b: e.scalar_tensor_tensor(out=h_t[:, b, :], in0=h_t[:, b, :], scalar=hrs[:, b:b + 1], in1=P[:, hg0:hg0 + HD], op0=ALU.mult, op1=ALU.mult),
                                 reads=[h_t, hrs, P], writes=[h_t])
                        S.op("dve", lambda e: e.tensor_tensor(out=hs_b[:], in0=h_t[:], in1=o_sig[:], op=ALU.mult), reads=[h_t, o_sig], writes=[hs_b])
                        for b in range(4):
                            pt = self.nps(); ptb = pt.t.bitcast(BF16)
                            for dc in range(2):
                                S.op("pe", lambda e, ptb=ptb, pt=pt, b=b, dc=dc: e.transpose(ptb[:, dc * 128:(dc + 1) * 128], hs_b[:, b, dc * 128:(dc + 1) * 128], self.id_b[:, :]),
                                     reads=[hs_b, self.id_b], writes=[pt])
                            S.op("act", lambda e, ptb=ptb, pt=pt, b=b: e.copy(out=outT[:, :, b * 128:(b + 1) * 128], in_=ptb[:, 0:256].rearrange("p (k t) -> p k t", k=2)), reads=[pt], writes=[outT])
                        for b in range(4):
                            for hh in range(2):
                                pp = self.nps()
                                for dc in range(2):
                                    S.op("pe", lambda e, pp=pp, b=b, hh=hh, dc=dc: e.matmul(pp[:, :], outT[:, dc, b * 128:(b + 1) * 128], wout[:, dc, hh * 512:(hh + 1) * 512],
                                                                                           start=(dc == 0), stop=(dc == 1)), reads=[outT, wout], writes=[pp])
                                S.op("dve", lambda e, pp=pp, b=b, hh=hh: e.tensor_tensor(out=xacc[:, b, hh * 512:(hh + 1) * 512], in0=xacc[:, b, hh * 512:(hh + 1) * 512], in1=pp[:, :], op=ALU.add),
                                     reads=[xacc, pp], writes=[xacc])
                        S.dma("sp", x_out.ap()[t0:t0 + 512, :].rearrange("(b p) d -> p b d", p=128), xacc[:], reads=[xacc], writes=[d_xo])
                S.end_phase()


Prog.odd_mixer = _odd_mixer


def pack_odd_params(norm_mix_l, hnorm_g, b_gate):
    P = np.zeros((128, OP_N), np.float32)
    P[:, OP["gamma"]:OP["gamma"] + D] = bc(norm_mix_l)
    P[:, OP["hg"]:OP["hg"] + D] = bc(hnorm_g.reshape(-1))
    P[:, OP["bg"]:OP["bg"] + 16] = bc(b_gate.reshape(-1))
    return P


def gate_rep(w_in_odd):
    g = w_in_odd[:, 4 * D:4 * D + 16]
    return np.ascontiguousarray(np.repeat(g, 128, axis=1))


def _xattn_inputs(p):
    return ({"wq": p.din("wq", [D, D]), "wkv": p.din("wkv", [D, 2 * D]), "wo": p.din("wo", [D, D]), "wr": p.din("wr", [D, NE])},
            p.din("x_par", [128, XP_N]), p.din("mem", [p.cfg.NSEG, MEM, D]))


def _moe_inputs(p):
    cfg = p.cfg
    return (p.din("aff_all", [128, NCORES * cfg.NB * NE]), p.din("aff_loc", [128, cfg.NB * NE]), p.din("gm", [128, cfg.NB]),
            {"w1": p.din("w1", [NE, D, cfg.FF]), "w3": p.din("w3", [NE, D, cfg.FF]), "w2": p.din("w2", [NE, cfg.FF, D])},
            p.din("m_par", [128, D]), p.dout("cnt", [128, NE]))


def build_L1(cfg):
    p = Prog(cfg)
    TOK, NT = cfg.TOK, cfg.NT
    x = p.din("x", [TOK, D]); flags = p.din("flags", [128, 2 * NT])
    ew = {"w_in": p.din("e_w_in", [D, 2048]), "w_out": p.din("e_w_out", [D, D]), "wsT": p.din("e_wsT", [128, 4, 128])}
    epar = p.din("e_par", [128, EP_N])
    xw, xpar, mem = _xattn_inputs(p)
    x1 = p.dint("x1", [TOK, D]); xo = p.dout("xo", [TOK, D]); aff = p.dout("aff", [128, cfg.NB * NE])
    p.consts()
    p.even_mixer(x, x1, ew, epar, flags)
    p.xattn_router(x1, xo, aff, xw, xpar, mem)
    p.st.close()
    return p


def build_L2(cfg):
    p = Prog(cfg)
    TOK, NT = cfg.TOK, cfg.NT
    x = p.din("x", [TOK, D]); flags = p.din("flags", [128, 2 * NT])
    aff_all, aff_loc, gm, mw, mpar, cnt = _moe_inputs(p)
    ow = {"w_in": p.din("o_w_in", [D, 4 * D + 16]), "wg_rep": p.din("o_wg_rep", [D, 16 * 128]), "w_out": p.din("o_w_out", [D, D])}
    opar = p.din("o_par", [128, OP_N])
    xw, xpar, mem = _xattn_inputs(p)
    x3 = p.dint("x3", [TOK, D]); x4 = p.dint("x4", [TOK, D]); xo = p.dout("xo", [TOK, D]); aff = p.dout("aff", [128, cfg.NB * NE])
    p.consts()
    p.moe(x, x3, aff_all, aff_loc, gm, mw, mpar, cnt)
    p.odd_mixer(x3, x4, ow, opar, flags)
    p.xattn_router(x4, xo, aff, xw, xpar, mem)
    p.st.close()
    return p


def build_L3(cfg):
    p = Prog(cfg)
    TOK = cfg.TOK
    x = p.din("x", [TOK, D])
    aff_all, aff_loc, gm, mw, mpar, cnt = _moe_inputs(p)
    fpar = p.din("f_par", [128, D])
    x6 = p.dint("x6", [TOK, D]); y = p.dout("y", [TOK, D])
    p.consts()
    p.moe(x, x6, aff_all, aff_loc, gm, mw, mpar, cnt)
    p.final_norm(x6, y, fpar)
    p.st.close()
    return p


def _launch(p, in_maps):
    res = run_bass_kernel_spmd(p.nc, in_maps, core_ids=list(range(NCORES)))
    return res.results


def _check_capacity(cfg, results):
    import sys
    allc = np.stack([r["cnt"][0] for r in results], 0)
    print(f"[moe] per-(core,expert) counts: mean={allc.mean():.1f} min={allc.min():.0f} max={allc.max():.0f} CMAX={cfg.CMAX}", file=sys.stderr)
    for c, r in enumerate(results):
        mx = float(np.max(r["cnt"][0]))
        if mx > cfg.CMAX:
            raise RuntimeError(f"MoE per-core expert capacity exceeded on core {c}: {mx} > {cfg.CMAX} "
                               "(static capacity assumption violated; refusing to return a silently wrong result)")


def run_model(cfg, I):
    f32 = lambda a: np.ascontiguousarray(np.asarray(a, dtype=np.float32))
    I = {k: f32(v) for k, v in I.items()}
    xsh = shard_tokens(cfg, I["x_prompt"], I["x_sample"])
    msh = shard_mem(cfg, I["mem_prompt"], I["mem_sample"])
    fl = tile_flags(cfg)
    gms = group_mask(cfg)

    def xattn_common(l):
        return {"wq": I["xa_wq"][l], "wkv": I["xa_wkv"][l], "wo": I["xa_wo"][l], "wr": I["moe_router"][l],
                "x_par": pack_xattn_params(I["norm_xattn"][l], I["norm_ffn"][l])}

    def moe_common(l, aff_locs):
        aff_all = np.ascontiguousarray(np.concatenate(aff_locs, axis=1))
        return {"aff_all": aff_all, "w1": I["moe_w1"][l], "w3": I["moe_w3"][l], "w2": I["moe_w2"][l],
                "m_par": np.ascontiguousarray(bc(I["norm_ffn"][l]))}

    p1 = build_L1(cfg)
    c1 = dict(xattn_common(0))
    c1.update({"e_w_in": I["even_w_in"][0], "e_w_out": I["even_w_out"][0],
               "e_wsT": np.ascontiguousarray(I["a_ws"][0].transpose(2, 0, 1)),
               "e_par": pack_even_params(I["norm_mix"][0], I["a_ln_g"][0], I["a_ln_b"][0], I["a_bs"][0], I["b_conv_w"][0],
                                         I["b_conv_b"][0], I["b_ln_g"][0], I["b_ln_b"][0])})
    r1 = _launch(p1, [dict(c1, x=xsh[c], mem=msh[c], flags=fl[c]) for c in range(NCORES)])
    del p1
    p2 = build_L2(cfg)
    c2 = dict(xattn_common(1))
    c2.update(moe_common(0, [r["aff"] for r in r1]))
    c2.update({"o_w_in": I["odd_w_in"][0], "o_wg_rep": gate_rep(I["odd_w_in"][0]), "o_w_out": I["odd_w_out"][0],
               "o_par": pack_odd_params(I["norm_mix"][1], I["odd_hnorm_g"][0], I["odd_b_gate"][0])})
    r2 = _launch(p2, [dict(c2, x=r1[c]["xo"], aff_loc=r1[c]["aff"], gm=gms[c], mem=msh[c], flags=fl[c]) for c in range(NCORES)])
    _check_capacity(cfg, r2)
    del p2, r1
    p3 = build_L3(cfg)
    c3 = moe_common(1, [r["aff"] for r in r2])
    c3["f_par"] = np.ascontiguousarray(bc(I["norm_final"]))
    r3 = _launch(p3, [dict(c3, x=r2[c]["xo"], aff_loc=r2[c]["aff"], gm=gms[c]) for c in range(NCORES)])
    _check_capacity(cfg, r3)
    yp, ys = unshard_tokens(cfg, [r["y"] for r in r3])
    return yp, ys


def kernel(**inputs):
    cfg = Cfg(seg=2048, ff=2048)
    yp, ys = run_model(cfg, inputs)
    return (yp.astype(np.float32), ys.astype(np.float32))
```

```python
import numpy as np
import concourse.bass as bass
import concourse.mybir as mybir
from concourse.bass_utils import run_bass_kernel_spmd
from contextlib import ExitStack

ENGS = ("pe", "act", "dve", "pool", "sp")
SEM_WINDOW = 20000
DMA_SEM_MAX = 3900


class Buf:
    __slots__ = ("name", "t", "last_w", "reads", "dsem", "dcnt")

    def __init__(self, name, t=None):
        self.name = name
        self.t = t
        self.last_w = None
        self.reads = []
        self.dsem = None
        self.dcnt = 0

    def __getitem__(self, k):
        return self.t[k]


class Sched:
    def __init__(self, nc, stack, same_engine_sync=True):
        self.nc = nc
        self.stack = stack
        self.ops = {e: [] for e in ENGS}
        self.signal = {e: set() for e in ENGS}
        self.known = {e: {} for e in ENGS}
        self.same_engine_sync = same_engine_sync
        self.free_dma_sems = []
        self.all_dma = []
        self.nsem = 0
        self.dma_bufs = []
        self.bufs = []
        self.emitted = {e: 0 for e in ENGS}
        self.semval = {}
        self.semstate = {e: [None, 0] for e in ENGS}

    def buf(self, name, t=None):
        b = Buf(name, t)
        self.bufs.append(b)
        return b

    def end_phase(self):
        self.barrier()
        self.emit()
        for b in self.bufs:
            b.last_w = None
            b.reads = []
        for b in self.dma_bufs:
            if b.dsem is not None:
                self.free_dma_sems.append((b.dsem, b.dcnt))
                b.dsem = None
        self.dma_bufs = []
        self.bufs = [b for b in self.bufs if b.t is None or getattr(b, "keep", False)] if False else self.bufs

    def new_sem(self, name):
        self.nsem += 1
        return self.stack.enter_context(self.nc.semaphore(f"{name}_{self.nsem}"))

    def _dma_sem(self, buf):
        if buf.dsem is None or buf.dcnt >= DMA_SEM_MAX:
            got = False
            if buf.dsem is None:
                while self.free_dma_sems:
                    sem, cnt = self.free_dma_sems.pop()
                    if cnt < DMA_SEM_MAX - 200:
                        buf.dsem, buf.dcnt = sem, cnt
                        got = True
                        break
            if not got:
                buf.dsem, buf.dcnt = self.new_sem("d"), 0
            if buf not in self.dma_bufs:
                self.dma_bufs.append(buf)
        return buf.dsem

    def release(self, *bufs):
        for b in bufs:
            if b.dsem is not None:
                self.free_dma_sems.append((b.dsem, b.dcnt))
                b.dsem = None
            if b in self.dma_bufs:
                self.dma_bufs.remove(b)

    def _need(self, eng, ev, waits, raw=False):
        if ev is None:
            return
        if ev[0] == "c":
            _, e2, idx = ev
            if e2 == eng and not (raw and eng != "pe" and self.same_engine_sync):
                return
            k = ("c", e2)
            if self.known[eng].get(k, -1) >= idx:
                return
            self.known[eng][k] = idx
            self.signal[e2].add(idx)
            waits.append(ev)
        else:
            _, sem, val = ev
            k = ("d", id(sem))
            if self.known[eng].get(k, -1) >= val:
                return
            self.known[eng][k] = val
            waits.append(ev)

    def op(self, eng, fn, reads=(), writes=(), dma_key=None):
        waits = []
        for r in reads:
            self._need(eng, r.last_w, waits, raw=True)
        for w in writes:
            self._need(eng, w.last_w, waits)
            for ev in w.reads:
                self._need(eng, ev, waits)
        idx = len(self.ops[eng])
        if dma_key is not None:
            sem = self._dma_sem(dma_key)
            dma_key.dcnt += 1
            ev = ("d", sem, 16 * dma_key.dcnt)
        else:
            ev = ("c", eng, idx)
        self.ops[eng].append([waits, fn, ev])
        for r in reads:
            r.reads.append(ev)
        for w in writes:
            w.last_w = ev
            w.reads = []
        return ev

    def dma(self, eng, out_ap, in_ap, reads=(), writes=(), key=None, **kw):
        key = key if key is not None else (writes[0] if writes else reads[0])
        return self.op(eng, lambda e: e.dma_start(out=out_ap, in_=in_ap, **kw),
                       reads=reads, writes=writes, dma_key=key)

    def barrier(self):
        last = {e: len(self.ops[e]) - 1 for e in ENGS}
        for e in ENGS:
            waits = []
            for e2 in ENGS:
                if e2 != e and last[e2] >= self.emitted[e2] and self.ops[e2][last[e2]][2][0] == "c":
                    self._need(e, ("c", e2, last[e2]), waits)
                elif e2 != e and last[e2] >= self.emitted[e2]:
                    for j in range(last[e2], self.emitted[e2] - 1, -1):
                        if self.ops[e2][j][2][0] == "c":
                            self._need(e, ("c", e2, j), waits)
                            break
            for b in list(self.dma_bufs):
                if b.dsem is not None and b.dcnt > 0:
                    self._need(e, ("d", b.dsem, 16 * b.dcnt), waits)
            if waits:
                self.ops[e].append([waits, None, ("c", e, len(self.ops[e]))])

    def emit(self):
        nc = self.nc
        semval = self.semval
        for e in ENGS:
            sem, n = self.semstate[e]
            for idx in sorted(i for i in self.signal[e] if i >= self.emitted[e]):
                if sem is None or n >= SEM_WINDOW:
                    sem = self.new_sem("p" + e)
                    n = 0
                n += 1
                semval[(e, idx)] = (sem, n)
            self.semstate[e] = [sem, n]
        engobj = {"pe": "tensor", "act": "scalar", "dve": "vector", "pool": "gpsimd", "sp": "sync"}

        def run(ename, eng):
            start = self.emitted[ename]
            for idx in range(start, len(self.ops[ename])):
                waits, fn, ev = self.ops[ename][idx]
                for w in waits:
                    if w[0] == "c":
                        s, v = semval[(w[1], w[2])]
                        eng.wait_ge(s, v)
                    else:
                        eng.wait_ge(w[1], w[2])
                if fn is None:
                    if (ename, idx) in semval:
                        s, v = semval[(ename, idx)]
                        eng.sem_inc(s, 1)
                    continue
                ins = fn(eng)
                if ev[0] == "d":
                    ins.then_inc(ev[1], 16)
                elif (ename, idx) in semval:
                    ins.then_inc(semval[(ename, idx)][0], 1)

        with nc.Block() as block:
            @block.tensor
            def _(e):
                run("pe", e)

            @block.scalar
            def _(e):
                run("act", e)

            @block.vector
            def _(e):
                run("dve", e)

            @block.gpsimd
            def _(e):
                run("pool", e)

            @block.sync
            def _(e):
                run("sp", e)
        for e in ENGS:
            self.emitted[e] = len(self.ops[e])

F32 = mybir.dt.float32
BF16 = mybir.dt.bfloat16
I32 = mybir.dt.int32
AF = mybir.ActivationFunctionType
ALU = mybir.AluOpType
AX = mybir.AxisListType

D = 1024
NE = 16
MEM = 256
EPS = 1e-6
NCORES = 8


class Cfg:
    def __init__(self, seg=2048, ff=2048, cmax=None):
        self.SEG = seg
        self.NSEG = 6
        self.TOK = self.NSEG * seg
        self.NT = self.TOK // 512
        self.NB = self.TOK // 128
        self.FF = ff
        self.NF = ff // 128
        self.NBALL = NCORES * self.NB
        self.CMAX = cmax if cmax is not None else ((self.TOK // 8) * 3 // 2 + 255) // 256 * 256
        self.CAP_P = 2 * (32 * seg) // NE
        self.CAP_S = 2 * (4 * 4 * seg) // NE


class Prog:
    def __init__(self, cfg):
        self.cfg = cfg
        self.nc = bass.Bass("TRN2", target_bir_lowering=False)
        self.st = ExitStack()
        self.S = Sched(self.nc, self.st)
        self.psn = 0
        self.ps = [self.S.buf(f"ps{i}", self.st.enter_context(self.nc.psum_tensor(f"ps{i}", [128, 512], F32)))
                   for i in range(8)]
        self.ins = {}
        self.outs = {}
        self.cn = 0
        self.consts_done = False

    def din(self, name, shape, dt=F32):
        h = self.nc.dram_tensor(name, list(shape), dt, kind="ExternalInput")
        self.ins[name] = h
        return h

    def dout(self, name, shape, dt=F32):
        h = self.nc.dram_tensor(name, list(shape), dt, kind="ExternalOutput")
        self.outs[name] = h
        return h

    def dint(self, name, shape, dt=F32):
        return self.nc.dram_tensor(name, list(shape), dt, kind="Internal")

    def sb(self, stack, name, shape, dt=F32):
        self.cn += 1
        return self.S.buf(name, stack.enter_context(self.nc.sbuf_tensor(f"{name}_{self.cn}", list(shape), dt)))

    def dbg(self, name, buf, ap, shape, dt=F32):
        if not getattr(self, "debug", False) or name in self.outs:
            return
        h = self.dout("dbg_" + name, shape, dt)
        self.S.dma("sp", h.ap(), ap, reads=[buf], writes=[self.S.buf("dbg_" + name)])

    def nps(self):
        b = self.ps[self.psn % 7]
        self.psn += 1
        return b

    @property
    def psx(self):
        return self.ps[7]

    def breg2(self, e):
        if getattr(self, "_bc2", None) is None:
            self._bc2 = e.to_reg(self.cfg.TOK - 1)
        return self._bc2

    def breg(self, e):
        if getattr(self, "_bc", None) is None:
            self._bc = e.to_reg(self.cfg.CMAX - 1)
        return self._bc

    def consts(self):
        S, nc, st = self.S, self.nc, self.st
        self.ones_f = self.sb(st, "ones_f", [128, 128], F32)
        self.ones_b = self.sb(st, "ones_b", [128, 128], BF16)
        self.id_f = self.sb(st, "id_f", [128, 128], F32)
        self.id_b = self.sb(st, "id_b", [128, 128], BF16)
        self.tri_b = self.sb(st, "tri_b", [128, 128], BF16)
        self.tri_f = self.sb(st, "tri_f", [128, 128], F32)
        of, ob, idf, idb, trb, trf = self.ones_f, self.ones_b, self.id_f, self.id_b, self.tri_b, self.tri_f
        S.op("pool", lambda e: e.memset(of[:], 1.0), writes=[of])
        S.op("pool", lambda e: e.memset(ob[:], 1.0), writes=[ob])
        S.op("pool", lambda e: e.memset(idf[:], 0.0), writes=[idf])
        S.op("pool", lambda e: e.memset(trf[:], 0.0), writes=[trf])
        S.op("pool", lambda e: e.affine_select(out=idf[:], in_=of[:], pattern=[[-1, 128]], compare_op=ALU.is_equal,
                                               fill=0.0, base=0, channel_multiplier=1), reads=[of], writes=[idf])
        S.op("pool", lambda e: e.affine_select(out=trf[:], in_=of[:], pattern=[[1, 128]], compare_op=ALU.is_ge,
                                               fill=0.0, base=0, channel_multiplier=-1), reads=[of], writes=[trf])
        self.tril_f = self.sb(st, "tril_f", [128, 128], F32)
        self.ones512 = self.sb(st, "ones512", [128, 512], F32)
        tl, o5 = self.tril_f, self.ones512
        S.op("pool", lambda e: e.memset(tl[:], 0.0), writes=[tl])
        S.op("pool", lambda e: e.memset(o5[:], 1.0), writes=[o5])
        S.op("pool", lambda e: e.affine_select(out=tl[:], in_=of[:], pattern=[[-1, 128]], compare_op=ALU.is_ge,
                                               fill=0.0, base=0, channel_multiplier=1), reads=[of], writes=[tl])
        S.op("pool", lambda e: e.tensor_copy(out=idb[:], in_=idf[:]), reads=[idf], writes=[idb])
        S.op("pool", lambda e: e.tensor_copy(out=trb[:], in_=trf[:]), reads=[trf], writes=[trb])

    def load_w(self, wsb, wdram_ap, nk, ndma=4, dbuf=None):
        src = wdram_ap.rearrange("(k p) n -> p k n", p=128)
        step = max(1, nk // ndma)
        for k0 in range(0, nk, step):
            k1 = min(nk, k0 + step)
            self.S.dma("pool", wsb[:, k0:k1, :], src[:, k0:k1, :], reads=[dbuf] if dbuf else [], writes=[wsb])

    def transpose_blocks(self, xn_b, nb, xnT, rows=128, evac=("act", "dve")):
        S = self.S
        for b in range(nb):
            ps = self.nps()
            psb = ps.t.bitcast(BF16)
            for k in range(8):
                S.op("pe", lambda e, b=b, k=k, psb=psb: e.transpose(psb[:, k * rows:(k + 1) * rows],
                                                                   xn_b[0:rows, b, k * 128:(k + 1) * 128],
                                                                   self.id_b[0:rows, 0:rows]),
                     reads=[xn_b, self.id_b], writes=[ps])
            eng = evac[b % len(evac)]
            src = psb[:, 0:8 * rows].rearrange("p (k t) -> p k t", k=8)
            if eng == "act":
                S.op("act", lambda e, b=b, src=src: e.copy(out=xnT[:, :, b * rows:(b + 1) * rows], in_=src),
                     reads=[ps], writes=[xnT])
            else:
                S.op("dve", lambda e, b=b, src=src: e.tensor_copy(out=xnT[:, :, b * rows:(b + 1) * rows], in_=src),
                     reads=[ps], writes=[xnT])

    def proj_fm(self, ps, w, c0, xT, n=512, nk=8, cw=128):
        for k in range(nk):
            self.S.op("pe", lambda e, k=k: e.matmul(ps[0:cw, 0:n], w[:, k, c0:c0 + cw], xT[:, k, 0:n],
                                                    start=(k == 0), stop=(k == nk - 1)),
                      reads=[w, xT], writes=[ps])

    def proj_tm(self, ps, xT, t0, w, n0, n=512, nk=8):
        for k in range(nk):
            self.S.op("pe", lambda e, k=k: e.matmul(ps[:, 0:n], xT[:, k, t0:t0 + 128], w[:, k, n0:n0 + n],
                                                    start=(k == 0), stop=(k == nk - 1)),
                      reads=[w, xT], writes=[ps])

    def even_mixer(self, x_in, x_out, W, par, flags):
        cfg, S, nc = self.cfg, self.S, self.nc
        TOK, NT = cfg.TOK, cfg.NT
        with ExitStack() as ph:
            sb = lambda n, s, d=F32: self.sb(ph, n, s, d)
            w_in = sb("w_in", [128, 8, 2048], BF16)
            w_out = sb("w_out", [128, 8, D], BF16)
            wsT = sb("wsT", [128, 4, 128], BF16)
            P = sb("par", [128, EP_N])
            fl = sb("fl", [128, 2 * NT])
            self.load_w(w_in, W["w_in"].ap(), 8)
            self.load_w(w_out, W["w_out"].ap(), 8)
            S.dma("pool", wsT[:], W["wsT"].ap(), writes=[wsT])
            S.dma("sp", P[:], par.ap(), writes=[P])
            S.dma("sp", fl[:], flags.ap(), writes=[fl])
            x_ts = [sb(f"x_t{i}", [128, 4, D]) for i in range(2)]
            xhs = [sb(f"xh{i}", [32, 1, D]) for i in range(2)]
            xn_b = sb("xn_b", [128, 4, D], BF16)
            xnh_b = sb("xnh_b", [32, 1, D], BF16)
            xnTs = [sb(f"xnT{i}", [128, 8, 512], BF16) for i in range(2)]
            xnhT = sb("xnhT", [128, 8, 32], BF16)
            junk = sb("junk", [128, D], BF16)
            ss = sb("ss", [128, 8]); rstd = sb("rstd", [128, 8])
            ssh = sb("ssh", [32, 8]); rstdh = sb("rstdh", [32, 8])
            uT = sb("uT", [128, 4, 512])
            gv = sb("gv", [128, 512])
            bst = sb("bst", [128, 1, 6]); mv = sb("mv", [128, 2]); vr = sb("vr", [128, 1])
            v_b = sb("v_b", [128, 4, 512], BF16)
            sig = sb("sig", [128, 512])
            gh = sb("gh", [128, 4, 32]); sigh = sb("sigh", [128, 4, 32])
            gT = sb("gT", [128, 4, 544])
            cT = sb("cT", [128, 4, 512])
            cT3 = sb("cT3", [128, 512])
            ctmp = [sb(f"ctmp{j}", [128, 512]) for j in range(3)]
            csq = sb("csq", [128, 4, 512])
            mean = sb("mean", [128, 512]); var = sb("var", [128, 512]); msq = sb("msq", [128, 512])
            tmp4 = sb("tmp4", [128, 4, 128])
            actT = sb("actT", [128, 8, 512], BF16)
            d_in = S.buf("d_xin"); d_out = S.buf("d_xout")
            for i in range(NT):
                x_t = x_ts[i % 2]; xh = xhs[i % 2]; xnT = xnTs[i % 2]
                t0 = i * 512
                lo0 = max(t0 - 16, 0); hi0 = min(t0 + 512, TOK - 16)
                S.dma("sp", x_t[:], x_in.ap()[t0:t0 + 512, :].rearrange("(b p) d -> p b d", p=128), reads=[d_in], writes=[x_t])
                S.dma("sp", xh[0:16, 0, :], x_in.ap()[lo0:lo0 + 16, :], reads=[d_in], writes=[xh])
                S.dma("sp", xh[16:32, 0, :], x_in.ap()[hi0:hi0 + 16, :], reads=[d_in], writes=[xh])
                self._rms(x_t, 4, P, EP["gamma"], xn_b, ss, rstd, junk, 128)
                self._rms(xh, 1, P, EP["gamma"], xnh_b, ssh, rstdh, junk, 32)
                self.transpose_blocks(xn_b, 4, xnT)
                self.transpose_blocks(xnh_b, 1, xnhT, rows=32)
                if i == 1:
                    self.dbg("e_xn", xn_b, xn_b[:], [128, 4, D], BF16)
                    self.dbg("e_xnT", xnT, xnT[:], [128, 8, 512], BF16)
                    self.dbg("e_xnhT", xnhT, xnhT[:], [128, 8, 32], BF16)
                psh = self.psx
                for c in range(4):
                    pv = self.nps(); pg = self.nps()
                    self.proj_fm(pv, w_in, 1024 + c * 128, xnT)
                    self.proj_fm(pg, w_in, 1536 + c * 128, xnT)
                    S.op("act", lambda e, pg=pg: e.activation(out=sig[:, :], in_=pg[:, :], func=AF.Sigmoid), reads=[pg], writes=[sig])
                    S.op("dve", lambda e, pv=pv, c=c: e.tensor_tensor(out=gT[:, c, 16:528], in0=pv[:, :], in1=sig[:, :], op=ALU.mult),
                         reads=[pv, sig], writes=[gT])
                    for k in range(8):
                        S.op("pe", lambda e, k=k, c=c: e.matmul(psh[:, c * 64:c * 64 + 32], w_in[:, k, 1024 + c * 128:1024 + (c + 1) * 128],
                                                                xnhT[:, k, :], start=(k == 0), stop=(k == 7)), reads=[w_in, xnhT], writes=[psh])
                    for k in range(8):
                        S.op("pe", lambda e, k=k, c=c: e.matmul(psh[:, c * 64 + 32:c * 64 + 64], w_in[:, k, 1536 + c * 128:1536 + (c + 1) * 128],
                                                                xnhT[:, k, :], start=(k == 0), stop=(k == 7)), reads=[w_in, xnhT], writes=[psh])
                psh3 = psh.t[:, 0:256].rearrange("p (c t) -> p c t", c=4)
                S.op("act", lambda e, psh3=psh3: e.activation(out=sigh[:], in_=psh3[:, :, 32:64], func=AF.Sigmoid), reads=[psh], writes=[sigh])
                S.op("dve", lambda e, psh3=psh3: e.tensor_tensor(out=gh[:], in0=psh3[:, :, 0:32], in1=sigh[:], op=ALU.mult), reads=[psh, sigh], writes=[gh])
                S.op("dve", lambda e, i=i: e.tensor_scalar(out=gT[:, :, 0:16], in0=gh[:, :, 0:16], scalar1=fl[:, 2 * i:2 * i + 1], scalar2=None, op0=ALU.mult),
                     reads=[gh, fl], writes=[gT])
                S.op("dve", lambda e, i=i: e.tensor_scalar(out=gT[:, :, 528:544], in0=gh[:, :, 16:32], scalar1=fl[:, 2 * i + 1:2 * i + 2], scalar2=None, op0=ALU.mult),
                     reads=[gh, fl], writes=[gT])
                if i == 1:
                    self.dbg("e_gT", gT, gT[:], [128, 4, 544])
                cw0 = EP["cw"]; cb0 = EP["cb"]
                for c in range(3):
                    S.op("dve", lambda e, c=c: e.tensor_scalar(out=cT[:, c, :], in0=gT[:, c, 1:513], scalar1=P[:, cw0 + c * 31:cw0 + c * 31 + 1],
                                                               scalar2=P[:, cb0 + c:cb0 + c + 1], op0=ALU.mult, op1=ALU.add),
                         reads=[gT, P], writes=[cT])
                    for k in range(1, 31):
                        S.op("dve", lambda e, c=c, k=k: e.scalar_tensor_tensor(out=cT[:, c, :], in0=gT[:, c, k + 1:k + 513],
                                                                                scalar=P[:, cw0 + c * 31 + k:cw0 + c * 31 + k + 1],
                                                                                in1=cT[:, c, :], op0=ALU.mult, op1=ALU.add),
                             reads=[gT, P, cT], writes=[cT])
                c = 3
                S.op("pool", lambda e: e.tensor_scalar(out=cT3[:, :], in0=gT[:, 3, 1:513], scalar1=P[:, cw0 + 93:cw0 + 94],
                                                       scalar2=P[:, cb0 + 3:cb0 + 4], op0=ALU.mult, op1=ALU.add), reads=[gT, P], writes=[cT3])
                for k in range(1, 31):
                    tk = ctmp[k % 3]
                    S.op("act", lambda e, k=k, tk=tk: e.activation(out=tk[:, :], in_=gT[:, 3, k + 1:k + 513], func=AF.Copy,
                                                                   scale=P[:, cw0 + 93 + k:cw0 + 94 + k]), reads=[gT, P], writes=[tk])
                    S.op("pool", lambda e, tk=tk: e.tensor_tensor(out=cT3[:, :], in0=cT3[:, :], in1=tk[:, :], op=ALU.add), reads=[cT3, tk], writes=[cT3])
                S.op("pool", lambda e: e.tensor_copy(out=cT[:, 3, :], in_=cT3[:, :]), reads=[cT3], writes=[cT])
                if i == 1:
                    self.dbg("e_cT", cT, cT[:], [128, 4, 512])
                S.op("act", lambda e: e.activation(out=csq[:].rearrange("p c t -> p (c t)"), in_=cT[:].rearrange("p c t -> p (c t)"), func=AF.Square),
                     reads=[cT], writes=[csq])
                p1 = self.nps(); p2 = self.nps()
                for c in range(4):
                    S.op("pe", lambda e, c=c, p1=p1: e.matmul(p1[:, :], self.ones_f[:, :], cT[:, c, :], start=(c == 0), stop=(c == 3)),
                         reads=[self.ones_f, cT], writes=[p1])
                for c in range(4):
                    S.op("pe", lambda e, c=c, p2=p2: e.matmul(p2[:, :], self.ones_f[:, :], csq[:, c, :], start=(c == 0), stop=(c == 3)),
                         reads=[self.ones_f, csq], writes=[p2])
                S.op("act", lambda e, p1=p1: e.activation(out=mean[:, :], in_=p1[:, :], func=AF.Copy, scale=1.0 / 512), reads=[p1], writes=[mean])
                S.op("dve", lambda e: e.tensor_tensor(out=msq[:, :], in0=mean[:, :], in1=mean[:, :], op=ALU.mult), reads=[mean], writes=[msq])
                S.op("dve", lambda e, p2=p2: e.scalar_tensor_tensor(out=var[:, :], in0=p2[:, :], scalar=1.0 / 512, in1=msq[:, :], op0=ALU.mult, op1=ALU.subtract),
                     reads=[p2, msq], writes=[var])
                S.op("act", lambda e: e.activation(out=var[:, :], in_=var[:, :], func=AF.Ln, bias=EPS, scale=1.0), reads=[var], writes=[var])
                S.op("act", lambda e: e.activation(out=var[:, :], in_=var[:, :], func=AF.Exp, scale=-0.5), reads=[var], writes=[var])
                mb = mean.t[:, :].rearrange("p (o t) -> p o t", o=1).to_broadcast([128, 4, 512])
                vb = var.t[:, :].rearrange("p (o t) -> p o t", o=1).to_broadcast([128, 4, 512])
                S.op("pool", lambda e, mb=mb: e.tensor_tensor(out=cT[:], in0=cT[:], in1=mb, op=ALU.subtract), reads=[cT, mean], writes=[cT])
                S.op("dve", lambda e, vb=vb: e.tensor_tensor(out=cT[:], in0=cT[:], in1=vb, op=ALU.mult), reads=[cT, var], writes=[cT])
                for c in range(4):
                    S.op("act", lambda e, c=c: e.activation(out=actT[:, 4 + c, :], in_=cT[:, c, :], func=AF.Silu,
                                                            bias=P[:, EP["blb"] + c:EP["blb"] + c + 1], scale=P[:, EP["blg"] + c:EP["blg"] + c + 1]),
                         reads=[cT, P], writes=[actT])
                for c in range(4):
                    pu = self.nps()
                    self.proj_fm(pu, w_in, c * 128, xnT)
                    S.op("act", lambda e, pu=pu, c=c: e.activation(out=uT[:, c, :], in_=pu[:, :], func=AF.Gelu_apprx_tanh), reads=[pu], writes=[uT])
                for b in range(4):
                    pa = self.nps()
                    self.proj_tm(pa, xnT, b * 128, w_in, 512)
                    S.op("act", lambda e, pa=pa: e.activation(out=gv[:, :], in_=pa[:, :], func=AF.Gelu_apprx_tanh), reads=[pa], writes=[gv])
                    S.op("dve", lambda e: e.bn_stats(out=bst[:, 0, :], in_=gv[:, :]), reads=[gv], writes=[bst])
                    S.op("dve", lambda e: e.bn_aggr(out=mv[:, :], in_=bst[:, :, :]), reads=[bst], writes=[mv])
                    S.op("act", lambda e: e.activation(out=vr[:, :], in_=mv[:, 1:2], func=AF.Ln, bias=EPS, scale=1.0), reads=[mv], writes=[vr])
                    S.op("act", lambda e: e.activation(out=vr[:, :], in_=vr[:, :], func=AF.Exp, scale=-0.5), reads=[vr], writes=[vr])
                    S.op("dve", lambda e: e.tensor_scalar(out=gv[:, :], in0=gv[:, :], scalar1=mv[:, 0:1], scalar2=vr[:, 0:1], op0=ALU.subtract, op1=ALU.mult),
                         reads=[gv, mv, vr], writes=[gv])
                    S.op("dve", lambda e: e.tensor_tensor(out=gv[:, :], in0=gv[:, :], in1=P[:, EP["alg"]:EP["alg"] + 512], op=ALU.mult), reads=[gv, P], writes=[gv])
                    S.op("dve", lambda e, b=b: e.tensor_tensor(out=v_b[:, b, :], in0=gv[:, :], in1=P[:, EP["alb"]:EP["alb"] + 512], op=ALU.add), reads=[gv, P], writes=[v_b])
                    pm = self.nps()
                    for g in range(4):
                        S.op("pe", lambda e, g=g, b=b, pm=pm: e.matmul(pm[:, g * 128:(g + 1) * 128], v_b[:, b, g * 128:(g + 1) * 128], wsT[:, g, :], start=True, stop=True),
                             reads=[v_b, wsT], writes=[pm])
                    pm3 = pm.t[:, :].rearrange("p (g t) -> p g t", g=4)
                    bs3 = P.t[:, EP["bs"]:EP["bs"] + 512].rearrange("p (g t) -> p g t", g=4)
                    S.op("dve", lambda e, pm3=pm3, bs3=bs3: e.tensor_tensor(out=tmp4[:], in0=pm3, in1=bs3, op=ALU.add), reads=[pm, P], writes=[tmp4])
                    S.op("dve", lambda e, b=b: e.tensor_tensor(out=actT[:, 0:4, b * 128:(b + 1) * 128], in0=tmp4[:], in1=uT[:, :, b * 128:(b + 1) * 128], op=ALU.mult),
                         reads=[tmp4, uT], writes=[actT])
                if i == 1:
                    self.dbg("e_actT", actT, actT[:], [128, 8, 512], BF16)
                    self.dbg("e_uT", uT, uT[:], [128, 4, 512])
                    self.dbg("e_vb", v_b, v_b[:], [128, 4, 512], BF16)
                for b in range(4):
                    for h in range(2):
                        po = self.nps()
                        self.proj_tm(po, actT, b * 128, w_out, h * 512)
                        S.op("dve", lambda e, po=po, b=b, h=h, x_t=x_t: e.tensor_tensor(out=x_t[:, b, h * 512:(h + 1) * 512], in0=x_t[:, b, h * 512:(h + 1) * 512],
                                                                                        in1=po[:, :], op=ALU.add), reads=[x_t, po], writes=[x_t])
                S.dma("sp", x_out.ap()[t0:t0 + 512, :].rearrange("(b p) d -> p b d", p=128), x_t[:], reads=[x_t], writes=[d_out])
            S.end_phase()

    def _rms(self, x_t, nb, P, goff, out, ss, rstd, junk, rows):
        S = self.S
        S.op("pool", lambda e: e.memset(ss[0:rows, 0:nb], 0.0), writes=[ss])
        for b in range(nb):
            S.op("act", lambda e, b=b: e.activation(out=junk[0:rows, :], in_=x_t[0:rows, b, :], func=AF.Square,
                                                    accum_out=ss[0:rows, b:b + 1]), reads=[x_t, ss], writes=[junk, ss])
        S.op("act", lambda e: e.activation(out=rstd[0:rows, 0:nb], in_=ss[0:rows, 0:nb], func=AF.Ln, bias=EPS, scale=1.0 / D),
             reads=[ss], writes=[rstd])
        S.op("act", lambda e: e.activation(out=rstd[0:rows, 0:nb], in_=rstd[0:rows, 0:nb], func=AF.Exp, scale=-0.5),
             reads=[rstd], writes=[rstd])
        if out is None:
            return
        for b in range(nb):
            S.op("dve", lambda e, b=b: e.scalar_tensor_tensor(out=out[0:rows, b, :], in0=x_t[0:rows, b, :], scalar=rstd[0:rows, b:b + 1],
                                                              in1=P[0:rows, goff:goff + D], op0=ALU.mult, op1=ALU.mult),
                 reads=[x_t, rstd, P], writes=[out])


def _mk_layout(items):
    off, d = 0, {}
    for k, w in items:
        d[k] = off
        off += w
    return d, off


EP, EP_N = _mk_layout([("gamma", D), ("alg", 512), ("alb", 512), ("bs", 512), ("cw", 124), ("cb", 4), ("blg", 4), ("blb", 4)])

XP, XP_N = _mk_layout([("gx", D), ("gf", D)])


def _xattn_router(self, x_in, x_out, aff_out, W, par, mem):
    cfg, S, nc = self.cfg, self.S, self.nc
    TOK, NT, NSEG, SEG = cfg.TOK, cfg.NT, cfg.NSEG, cfg.SEG
    TPS = SEG // 512
    with ExitStack() as ph:
        sb = lambda n, s, d=F32: self.sb(ph, n, s, d)
        wq = sb("wq", [128, 8, D], BF16)
        wkv = sb("wkv", [128, 8, 2 * D], BF16)
        wo = sb("wo", [128, 8, D], BF16)
        wr = sb("wr", [128, 8, NE])
        P = sb("xpar", [128, XP_N])
        self.load_w(wq, W["wq"].ap(), 8)
        self.load_w(wkv, W["wkv"].ap(), 8)
        self.load_w(wo, W["wo"].ap(), 8)
        S.dma("sp", wr[:], W["wr"].ap().rearrange("(k p) n -> p k n", p=128), writes=[wr])
        S.dma("sp", P[:], par.ap(), writes=[P])
        mem_b = sb("mem_b", [128, 2, D], BF16)
        memT = sb("memT", [128, 8, MEM], BF16)
        KT = sb("KT", [128, 8, MEM], BF16)
        V_b = sb("V_b", [128, 2, D], BF16)
        x_ts = [sb(f"xa_t{i}", [128, 4, D]) for i in range(1)]
        xn_b = sb("xa_nb", [128, 4, D], BF16)
        xnT = sb("xa_nT", [128, 8, 512], BF16)
        qT = sb("qT", [128, 8, 512], BF16)
        ET = sb("ET", [128, 2, 512], BF16)
        rec = sb("rec", [128, 512])
        oT = sb("oT", [128, 8, 512], BF16)
        junk = sb("xa_junk", [128, D], BF16)
        ss = sb("xa_ss", [128, 8]); rstd = sb("xa_rstd", [128, 8])
        xn2 = sb("xn2", [128, 1, D])
        xn2T = sb("xn2T", [128, 8, 128])
        mx = sb("mx", [128, 1]); sm = sb("sm", [128, 1]); ex = sb("ex", [128, NE])
        aff = sb("aff", [128, cfg.NB, NE])
        d_in = S.buf("d_xin"); d_out = S.buf("d_xout"); d_mem = S.buf("d_mem")
        for i in range(NT):
            seg = i // TPS
            t0 = i * 512
            x_t = x_ts[0]
            S.dma("sp", x_t[:], x_in.ap()[t0:t0 + 512, :].rearrange("(b p) d -> p b d", p=128), reads=[d_in], writes=[x_t])
            if i % TPS == 0:
                S.dma("pool", mem_b[:], mem.ap()[seg].rearrange("(b p) d -> p b d", p=128), reads=[d_mem], writes=[mem_b])
                self.transpose_blocks(mem_b, 2, memT)
                for n in range(8):
                    pk = self.nps()
                    self.proj_fm(pk, wkv, n * 128, memT, n=MEM)
                    if n % 2 == 0:
                        S.op("act", lambda e, pk=pk, n=n: e.copy(out=KT[:, n, :], in_=pk[:, 0:MEM]), reads=[pk], writes=[KT])
                    else:
                        S.op("dve", lambda e, pk=pk, n=n: e.tensor_copy(out=KT[:, n, :], in_=pk[:, 0:MEM]), reads=[pk], writes=[KT])
                for mb in range(2):
                    for h in range(2):
                        pv = self.nps()
                        self.proj_tm(pv, memT, mb * 128, wkv, D + h * 512)
                        if h == 0:
                            S.op("act", lambda e, pv=pv, mb=mb, h=h: e.copy(out=V_b[:, mb, h * 512:(h + 1) * 512], in_=pv[:, :]), reads=[pv], writes=[V_b])
                        else:
                            S.op("dve", lambda e, pv=pv, mb=mb, h=h: e.tensor_copy(out=V_b[:, mb, h * 512:(h + 1) * 512], in_=pv[:, :]), reads=[pv], writes=[V_b])
            self._rms(x_t, 4, P, XP["gx"], xn_b, ss, rstd, junk, 128)
            self.transpose_blocks(xn_b, 4, xnT)
            for n in range(8):
                pq = self.nps()
                self.proj_fm(pq, wq, n * 128, xnT)
                if n % 2 == 0:
                    S.op("act", lambda e, pq=pq, n=n: e.copy(out=qT[:, n, :], in_=pq[:, :]), reads=[pq], writes=[qT])
                else:
                    S.op("dve", lambda e, pq=pq, n=n: e.tensor_copy(out=qT[:, n, :], in_=pq[:, :]), reads=[pq], writes=[qT])
            for h in range(4):
                for mc in range(2):
                    psc = self.nps()
                    for dc in range(2):
                        S.op("pe", lambda e, psc=psc, h=h, mc=mc, dc=dc: e.matmul(psc[:, :], KT[:, 2 * h + dc, mc * 128:(mc + 1) * 128], qT[:, 2 * h + dc, :],
                                                                                  start=(dc == 0), stop=(dc == 1)), reads=[KT, qT], writes=[psc])
                    S.op("act", lambda e, psc=psc, mc=mc: e.activation(out=ET[:, mc, :], in_=psc[:, :], func=AF.Exp, scale=1.0 / 16.0), reads=[psc], writes=[ET])
                pd = self.nps()
                for mc in range(2):
                    S.op("pe", lambda e, pd=pd, mc=mc: e.matmul(pd[:, :], self.ones_b[:, :], ET[:, mc, :], start=(mc == 0), stop=(mc == 1)),
                         reads=[self.ones_b, ET], writes=[pd])
                S.op("dve", lambda e, pd=pd: e.reciprocal(out=rec[:, :], in_=pd[:, :]), reads=[pd], writes=[rec])
                for dc in range(2):
                    po = self.nps()
                    for mc in range(2):
                        S.op("pe", lambda e, po=po, h=h, mc=mc, dc=dc: e.matmul(po[:, :], V_b[:, mc, h * 256 + dc * 128:h * 256 + (dc + 1) * 128], ET[:, mc, :],
                                                                                start=(mc == 0), stop=(mc == 1)), reads=[V_b, ET], writes=[po])
                    S.op("dve", lambda e, po=po, h=h, dc=dc: e.tensor_tensor(out=oT[:, 2 * h + dc, :], in0=po[:, :], in1=rec[:, :], op=ALU.mult),
                         reads=[po, rec], writes=[oT])
            if i == 0:
                self.dbg("x_qT", qT, qT[:], [128, 8, 512], BF16)
                self.dbg("x_KT", KT, KT[:], [128, 8, MEM], BF16)
                self.dbg("x_V", V_b, V_b[:], [128, 2, D], BF16)
                self.dbg("x_oT", oT, oT[:], [128, 8, 512], BF16)
            for b in range(4):
                for hh in range(2):
                    pp = self.nps()
                    self.proj_tm(pp, oT, b * 128, wo, hh * 512)
                    S.op("dve", lambda e, pp=pp, b=b, hh=hh, x_t=x_t: e.tensor_tensor(out=x_t[:, b, hh * 512:(hh + 1) * 512], in0=x_t[:, b, hh * 512:(hh + 1) * 512],
                                                                                      in1=pp[:, :], op=ALU.add), reads=[x_t, pp], writes=[x_t])
            S.dma("sp", x_out.ap()[t0:t0 + 512, :].rearrange("(b p) d -> p b d", p=128), x_t[:], reads=[x_t], writes=[d_out])
            self._rms(x_t, 4, P, XP["gf"], None, ss, rstd, junk, 128)
            for b in range(4):
                S.op("dve", lambda e, b=b, x_t=x_t: e.scalar_tensor_tensor(out=xn2[:, 0, :], in0=x_t[:, b, :], scalar=rstd[:, b:b + 1],
                                                                          in1=P[:, XP["gf"]:XP["gf"] + D], op0=ALU.mult, op1=ALU.mult),
                     reads=[x_t, rstd, P], writes=[xn2])
                pa, pb = self.nps(), self.nps()
                for k in range(8):
                    pt = pa if k < 4 else pb
                    S.op("pe", lambda e, pt=pt, k=k, b=b: e.transpose(pt[:, (k % 4) * 128:(k % 4 + 1) * 128], xn2[:, 0, k * 128:(k + 1) * 128], self.id_f[:, :]),
                         reads=[xn2, self.id_f], writes=[pt])
                S.op("act", lambda e, pa=pa: e.copy(out=xn2T[:, 0:4, :], in_=pa[:, :].rearrange("p (k t) -> p k t", k=4)), reads=[pa], writes=[xn2T])
                S.op("dve", lambda e, pb=pb: e.tensor_copy(out=xn2T[:, 4:8, :], in_=pb[:, :].rearrange("p (k t) -> p k t", k=4)), reads=[pb], writes=[xn2T])
                if i == 0 and b == 0:
                    self.dbg("x_xn2T", xn2T, xn2T[:], [128, 8, 128])
                pr = self.nps()
                for k in range(8):
                    S.op("pe", lambda e, pr=pr, k=k: e.matmul(pr[:, 0:NE], xn2T[:, k, :], wr[:, k, :], start=(k == 0), stop=(k == 7)), reads=[xn2T, wr], writes=[pr])
                S.op("dve", lambda e, pr=pr: e.tensor_reduce(out=mx[:, :], in_=pr[:, 0:NE], axis=AX.X, op=ALU.max), reads=[pr], writes=[mx])
                S.op("dve", lambda e: e.tensor_scalar(out=mx[:, :], in0=mx[:, :], scalar1=-1.0, scalar2=None, op0=ALU.mult), reads=[mx], writes=[mx])
                S.op("pool", lambda e: e.memset(sm[:, :], 0.0), writes=[sm])
                S.op("act", lambda e, pr=pr: e.activation(out=ex[:, :], in_=pr[:, 0:NE], func=AF.Exp, bias=mx[:, 0:1], scale=1.0, accum_out=sm[:, 0:1]),
                     reads=[pr, mx, sm], writes=[ex, sm])
                S.op("dve", lambda e: e.reciprocal(out=sm[:, :], in_=sm[:, :]), reads=[sm], writes=[sm])
                S.op("dve", lambda e, i=i, b=b: e.tensor_scalar(out=aff[:, i * 4 + b, :], in0=ex[:, :], scalar1=sm[:, 0:1], scalar2=None, op0=ALU.mult),
                     reads=[ex, sm], writes=[aff])
        d_aff = S.buf("d_aff")
        S.dma("sp", aff_out.ap(), aff[:].rearrange("p b e -> p (b e)"), reads=[aff], writes=[d_aff])
        S.end_phase()


Prog.xattn_router = _xattn_router


def _final_norm(self, x_in, y_out, par):
    cfg, S = self.cfg, self.S
    with ExitStack() as ph:
        sb = lambda n, s, d=F32: self.sb(ph, n, s, d)
        P = sb("fpar", [128, D])
        S.dma("sp", P[:], par.ap(), writes=[P])
        x_ts = [sb(f"f_t{i}", [128, 4, D]) for i in range(2)]
        y_ts = [sb(f"f_y{i}", [128, 4, D]) for i in range(2)]
        junk = sb("f_junk", [128, D], BF16)
        ss = sb("f_ss", [128, 8]); rstd = sb("f_rstd", [128, 8])
        d_in = S.buf("d_xin"); d_out = S.buf("d_yout")
        for i in range(cfg.NT):
            t0 = i * 512
            x_t, y_t = x_ts[i % 2], y_ts[i % 2]
            S.dma("sp", x_t[:], x_in.ap()[t0:t0 + 512, :].rearrange("(b p) d -> p b d", p=128), reads=[d_in], writes=[x_t])
            self._rms(x_t, 4, P, 0, y_t, ss, rstd, junk, 128)
            S.dma("sp", y_out.ap()[t0:t0 + 512, :].rearrange("(b p) d -> p b d", p=128), y_t[:], reads=[y_t], writes=[d_out])
        S.end_phase()


Prog.final_norm = _final_norm


def core_segments():
    out = []
    for c in range(NCORES):
        if c < 4:
            out.append([("s", c, j) for j in range(4)] + [("p", 2 * c, 0), ("p", 2 * c + 1, 0)])
        else:
            out.append([("p", 8 + 6 * (c - 4) + j, 0) for j in range(6)])
    return out


def shard_tokens(cfg, xp, xs):
    SEG = cfg.SEG
    res = []
    for segs in core_segments():
        parts = []
        for g, q, j in segs:
            parts.append(xp[q] if g == "p" else xs[q, j * SEG:(j + 1) * SEG])
        res.append(np.ascontiguousarray(np.concatenate(parts, 0)))
    return res


def unshard_tokens(cfg, per_core, n_p=32, n_s=4):
    SEG = cfg.SEG
    yp = np.zeros((n_p, SEG, D), np.float32)
    ys = np.zeros((n_s, 4 * SEG, D), np.float32)
    for c, segs in enumerate(core_segments()):
        for si, (g, q, j) in enumerate(segs):
            blk = per_core[c][si * SEG:(si + 1) * SEG]
            if g == "p":
                yp[q] = blk
            else:
                ys[q, j * SEG:(j + 1) * SEG] = blk
    return yp, ys


def shard_mem(cfg, mp, ms):
    res = []
    for segs in core_segments():
        res.append(np.ascontiguousarray(np.stack([mp[q] if g == "p" else ms[q] for g, q, j in segs], 0)))
    return res


def tile_flags(cfg):
    SEG, NT, NSEG = cfg.SEG, cfg.NT, cfg.NSEG
    res = []
    for segs in core_segments():
        cont = [False] + [segs[s][0] == segs[s - 1][0] and segs[s][1] == segs[s - 1][1] and segs[s][2] == segs[s - 1][2] + 1
                          for s in range(1, NSEG)] + [False]
        f = np.zeros((2 * NT,), np.float32)
        for i in range(NT):
            t0 = i * 512
            s = t0 // SEG
            f[2 * i] = 1.0 if (t0 % SEG != 0 or cont[s]) else 0.0
            e = t0 + 512
            f[2 * i + 1] = 1.0 if (e % SEG != 0 or cont[s + 1]) else 0.0
        res.append(np.ascontiguousarray(np.broadcast_to(f[None, :], (128, 2 * NT))))
    return res


def bc(v, n=128):
    v = np.asarray(v, np.float32).reshape(1, -1)
    return np.broadcast_to(v, (n, v.shape[1]))


def pack_even_params(norm_mix_l, a_ln_g, a_ln_b, a_bs, conv_w, conv_b, b_ln_g, b_ln_b):
    P = np.zeros((128, EP_N), np.float32)
    P[:, EP["gamma"]:EP["gamma"] + D] = bc(norm_mix_l)
    P[:, EP["alg"]:EP["alg"] + 512] = bc(a_ln_g)
    P[:, EP["alb"]:EP["alb"] + 512] = bc(a_ln_b)
    P[:, EP["bs"]:EP["bs"] + 512] = bc(a_bs.reshape(-1))
    P[:, EP["cw"]:EP["cw"] + 124] = conv_w.T.reshape(4, 128, 31).transpose(1, 0, 2).reshape(128, 124)
    P[:, EP["cb"]:EP["cb"] + 4] = conv_b.reshape(4, 128).T
    P[:, EP["blg"]:EP["blg"] + 4] = b_ln_g.reshape(4, 128).T
    P[:, EP["blb"]:EP["blb"] + 4] = b_ln_b.reshape(4, 128).T
    return P


def pack_xattn_params(norm_x_l, norm_f_l):
    P = np.zeros((128, XP_N), np.float32)
    P[:, XP["gx"]:XP["gx"] + D] = bc(norm_x_l)
    P[:, XP["gf"]:XP["gf"] + D] = bc(norm_f_l)
    return P


def _moe(self, x_in, x_out, aff_all, aff_loc, gm_loc, W, par, cnt_out):
    cfg, S, nc = self.cfg, self.S, self.nc
    TOK, NT, NB, SEG, FF, NF, CMAX = cfg.TOK, cfg.NT, cfg.NB, cfg.SEG, cfg.FF, cfg.NF, cfg.CMAX
    NBS_C = 4 * SEG // 128
    NFS = 4 * NBS_C
    NFP = NCORES * NB - NFS
    xes = [self.dint(f"xe{e}_{self.cn}", [CMAX, D], BF16) for e in range(NE)]
    d_xe = [S.buf(f"d_xe{e}") for e in range(NE)]
    with ExitStack() as mo:
        posI = self.sb(mo, "posI", [128, NB, NE], I32)
        gate = self.sb(mo, "gate", [128, NB, NE])
        thr = self.sb(mo, "thr", [128, 2 * NE])
        with ExitStack() as ph:
            sb = lambda n, s, d=F32: self.sb(ph, n, s, d)
            A_P = sb("A_P", [128, NFP, NE]); A_S = sb("A_S", [128, NFS, NE])
            cmp_ = sb("cmp", [128, NFP, NE], BF16)
            lo = sb("lo", [128, 2 * NE]); hi = sb("hi", [128, 2 * NE]); mid = sb("mid", [128, 2 * NE])
            cnt = sb("cnt", [128, 2 * NE]); cond = sb("cond", [128, 2 * NE]); t1 = sb("t1", [128, 2 * NE])
            capm = sb("capm", [128, 2 * NE])
            src = aff_all.ap().rearrange("p (f e) -> p f e", e=NE)
            d_aff = S.buf("d_affall")
            po, so = 0, 0
            for c in range(NCORES):
                f0 = c * NB
                if c < 4:
                    S.dma("sp", A_S[:, so:so + NBS_C, :], src[:, f0:f0 + NBS_C, :], reads=[d_aff], writes=[A_S]); so += NBS_C
                    n = NB - NBS_C
                    S.dma("sp", A_P[:, po:po + n, :], src[:, f0 + NBS_C:f0 + NB, :], reads=[d_aff], writes=[A_P]); po += n
                else:
                    S.dma("sp", A_P[:, po:po + NB, :], src[:, f0:f0 + NB, :], reads=[d_aff], writes=[A_P]); po += NB
            S.op("pool", lambda e: e.memset(lo[:, :], 0.0), writes=[lo])
            S.op("pool", lambda e: e.memset(hi[:, :], 1.0), writes=[hi])
            S.op("pool", lambda e: e.memset(mid[:, :], 0.5), writes=[mid])
            S.op("pool", lambda e: e.memset(capm[:, 0:NE], cfg.CAP_P + 0.5), writes=[capm])
            S.op("pool", lambda e: e.memset(capm[:, NE:2 * NE], cfg.CAP_S + 0.5), writes=[capm])
            for it in range(32):
                for (A, nf, c0) in ((A_P, NFP, 0), (A_S, NFS, NE)):
                    mb = mid.t[:, c0:c0 + NE].rearrange("p (o e) -> p o e", o=1).to_broadcast([128, nf, NE])
                    S.op("dve", lambda e, A=A, nf=nf, mb=mb: e.tensor_tensor(out=cmp_[:, 0:nf, :], in0=A[:, :, :], in1=mb, op=ALU.is_gt),
                         reads=[A, mid], writes=[cmp_])
                    S.op("dve", lambda e, nf=nf, c0=c0: e.tensor_reduce(out=cnt[:, c0:c0 + NE], in_=cmp_[:, 0:nf, :].rearrange("p f e -> p e f"), axis=AX.X, op=ALU.add),
                         reads=[cmp_], writes=[cnt])
                pt = self.nps()
                S.op("pe", lambda e, pt=pt: e.matmul(pt[:, 0:2 * NE], self.ones_f[:, :], cnt[:, :], start=True, stop=True), reads=[self.ones_f, cnt], writes=[pt])
                S.op("dve", lambda e, pt=pt: e.tensor_tensor(out=cond[:, :], in0=pt[:, 0:2 * NE], in1=capm[:, :], op=ALU.is_gt), reads=[pt, capm], writes=[cond])
                S.op("dve", lambda e: e.tensor_tensor(out=t1[:, :], in0=cond[:, :], in1=mid[:, :], op=ALU.mult), reads=[cond, mid], writes=[t1])
                S.op("dve", lambda e: e.tensor_tensor(out=lo[:, :], in0=lo[:, :], in1=t1[:, :], op=ALU.max), reads=[lo, t1], writes=[lo])
                S.op("dve", lambda e: e.scalar_tensor_tensor(out=t1[:, :], in0=cond[:, :], scalar=4.0, in1=mid[:, :], op0=ALU.mult, op1=ALU.add), reads=[cond, mid], writes=[t1])
                S.op("dve", lambda e: e.tensor_tensor(out=hi[:, :], in0=hi[:, :], in1=t1[:, :], op=ALU.min), reads=[hi, t1], writes=[hi])
                S.op("dve", lambda e: e.tensor_tensor(out=t1[:, :], in0=lo[:, :], in1=hi[:, :], op=ALU.add), reads=[lo, hi], writes=[t1])
                S.op("dve", lambda e: e.tensor_scalar(out=mid[:, :], in0=t1[:, :], scalar1=0.5, scalar2=None, op0=ALU.mult), reads=[t1], writes=[mid])
            S.op("dve", lambda e: e.tensor_copy(out=thr[:, :], in_=hi[:, :]), reads=[hi], writes=[thr])
            S.end_phase()
        with ExitStack() as ph:
            sb = lambda n, s, d=F32: self.sb(ph, n, s, d)
            NC_ = NB * NE
            AL = sb("AL", [128, NB, NE]); gm = sb("gm", [128, NB])
            tt = sb("tt", [128, NB, NE]); sel = sb("sel", [128, NB, NE]); sel_b = sb("sel_b", [128, NB, NE], BF16)
            incl = sb("incl", [128, NC_]); ca = sb("ca", [128, NC_]); cb = sb("cb", [128, NC_]); cn0 = sb("cn0", [128, NC_])
            dthr = sb("dthr", [128, NE])
            d_al = S.buf("d_al")
            S.dma("sp", AL[:], aff_loc.ap().rearrange("p (f e) -> p f e", e=NE), reads=[d_al], writes=[AL])
            S.dma("sp", gm[:], gm_loc.ap(), reads=[d_al], writes=[gm])
            S.op("dve", lambda e: e.tensor_tensor(out=dthr[:, :], in0=thr[:, 0:NE], in1=thr[:, NE:2 * NE], op=ALU.subtract), reads=[thr], writes=[dthr])
            gmb = gm.t[:, :].rearrange("p (f o) -> p f o", o=1).to_broadcast([128, NB, NE])
            dtb = dthr.t[:, :].rearrange("p (o e) -> p o e", o=1).to_broadcast([128, NB, NE])
            tsb = thr.t[:, NE:2 * NE].rearrange("p (o e) -> p o e", o=1).to_broadcast([128, NB, NE])
            S.op("dve", lambda e: e.tensor_tensor(out=tt[:], in0=gmb, in1=dtb, op=ALU.mult), reads=[gm, dthr], writes=[tt])
            S.op("dve", lambda e: e.tensor_tensor(out=tt[:], in0=tt[:], in1=tsb, op=ALU.add), reads=[tt, thr], writes=[tt])
            S.op("dve", lambda e: e.tensor_tensor(out=sel[:], in0=AL[:], in1=tt[:], op=ALU.is_gt), reads=[AL, tt], writes=[sel])
            S.op("dve", lambda e: e.tensor_copy(out=sel_b[:], in_=sel[:]), reads=[sel], writes=[sel_b])
            S.op("dve", lambda e: e.tensor_tensor(out=gate[:], in0=AL[:], in1=sel[:], op=ALU.mult), reads=[AL, sel], writes=[gate])
            selb2 = sel_b.t[:].rearrange("p f e -> p (f e)")
            for c0 in range(0, NC_, 512):
                n = min(512, NC_ - c0)
                pi = self.nps(); pc = self.nps()
                S.op("pe", lambda e, pi=pi, c0=c0, n=n: e.matmul(pi[:, 0:n], self.tri_b[:, :], selb2[:, c0:c0 + n], start=True, stop=True), reads=[self.tri_b, sel_b], writes=[pi])
                S.op("pe", lambda e, pc=pc, c0=c0, n=n: e.matmul(pc[:, 0:n], self.ones_b[:, :], selb2[:, c0:c0 + n], start=True, stop=True), reads=[self.ones_b, sel_b], writes=[pc])
                S.op("act", lambda e, pi=pi, c0=c0, n=n: e.copy(out=incl[:, c0:c0 + n], in_=pi[:, 0:n]), reads=[pi], writes=[incl])
                S.op("dve", lambda e, pc=pc, c0=c0, n=n: e.tensor_copy(out=cn0[:, c0:c0 + n], in_=pc[:, 0:n]), reads=[pc], writes=[cn0])
            cur = cn0
            sft = 1
            k = 0
            while sft < NB:
                nxt = ca if k % 2 == 0 else cb
                w = sft * NE
                S.op("dve", lambda e, cur=cur, nxt=nxt, w=w: e.tensor_copy(out=nxt[:, 0:w], in_=cur[:, 0:w]), reads=[cur], writes=[nxt])
                S.op("dve", lambda e, cur=cur, nxt=nxt, w=w: e.tensor_tensor(out=nxt[:, w:NC_], in0=cur[:, w:NC_], in1=cur[:, 0:NC_ - w], op=ALU.add), reads=[cur], writes=[nxt])
                cur = nxt
                sft *= 2
                k += 1
            d_cnt = S.buf("d_cnt")
            S.dma("sp", cnt_out.ap(), cur[:, NC_ - NE:NC_], reads=[cur], writes=[d_cnt])
            S.op("dve", lambda e, cur=cur: e.tensor_tensor(out=incl[:, :], in0=incl[:, :], in1=cur[:, :], op=ALU.add), reads=[incl, cur], writes=[incl])
            S.op("dve", lambda e: e.tensor_tensor(out=incl[:, :], in0=incl[:, :], in1=cn0[:, :], op=ALU.subtract), reads=[incl, cn0], writes=[incl])
            sel2 = sel.t[:].rearrange("p f e -> p (f e)")
            S.op("dve", lambda e: e.scalar_tensor_tensor(out=incl[:, :], in0=sel2, scalar=-1.0e6, in1=incl[:, :], op0=ALU.mult, op1=ALU.add), reads=[sel, incl], writes=[incl])
            S.op("dve", lambda e: e.tensor_scalar(out=incl[:, :], in0=incl[:, :], scalar1=1.0e6 - 1.0, scalar2=None, op0=ALU.add), reads=[incl], writes=[incl])
            S.op("dve", lambda e: e.tensor_copy(out=posI[:].rearrange("p f e -> p (f e)"), in_=incl[:, :]), reads=[incl], writes=[posI])
            S.end_phase()
        xn_d = self.dint(f"xn_d_{self.cn}", [TOK, D], BF16)
        meta_ds = [self.dint(f"meta{e}_{self.cn}", [CMAX, 2], F32) for e in range(NE)]
        d_xn = S.buf("d_xn"); d_xacc = S.buf("d_xacc"); d_meta = [S.buf(f"d_meta{e}") for e in range(NE)]
        with ExitStack() as ph:
            sb = lambda n, s, d=F32: self.sb(ph, n, s, d)
            P = sb("mpar", [128, D])
            S.dma("sp", P[:], par.ap(), writes=[P])
            x_ts = [sb(f"m_xt{i}", [128, 4, D]) for i in range(2)]
            xn_bs = [sb(f"m_xn{i}", [128, 4, D], BF16) for i in range(2)]
            junk = sb("m_junk", [128, D], BF16); ss = sb("m_ss", [128, 8]); rstd = sb("m_rstd", [128, 8])
            onesm = sb("m_ones", [128, 2 * (CMAX // 128)])
            d_in = S.buf("d_xin")
            S.op("pool", lambda e: e.memset(onesm[:, :], 1.0), writes=[onesm])
            for ex in range(NE):
                S.dma("sp", meta_ds[ex].ap().rearrange("(p r) c -> p (r c)", p=128), onesm[:, :], reads=[onesm], writes=[d_meta[ex]])
            for i in range(NT):
                t0 = i * 512
                x_t, xn_b = x_ts[i % 2], xn_bs[i % 2]
                S.dma("sp", x_t[:], x_in.ap()[t0:t0 + 512, :].rearrange("(b p) d -> p b d", p=128), reads=[d_in], writes=[x_t])
                S.dma("act", x_out.ap()[t0:t0 + 512, :].rearrange("(b p) d -> p b d", p=128), x_t[:], reads=[x_t], writes=[d_xacc])
                self._rms(x_t, 4, P, 0, xn_b, ss, rstd, junk, 128)
                S.dma("sp", xn_d.ap()[t0:t0 + 512, :].rearrange("(b p) d -> p b d", p=128), xn_b[:], reads=[xn_b], writes=[d_xn])
            S.end_phase()
        with ExitStack() as ph:
            sb = lambda n, s, d=F32: self.sb(ph, n, s, d)
            w1 = sb("w1", [128, 8, FF], BF16); w3 = sb("w3", [128, 8, FF], BF16); w2 = sb("w2", [128, NF, D], BF16)
            xe_t = sb("xe_t", [128, 4, D], BF16)
            xeT = sb("xeT", [128, 8, 512], BF16)
            hidT = sb("hidT", [128, NF, 512], BF16)
            s1s = [sb(f"s1_{i}", [128, 512]) for i in range(2)]
            y_ts = [sb(f"y_t{i}", [128, D]) for i in range(2)]
            scs = [sb(f"sc{i}", [128, 2, D], BF16) for i in range(2)]
            metaes = [sb(f"metae{i}", [128, NB, 2]) for i in range(2)]
            metats = [sb(f"metat{i}", [128, 2]) for i in range(4)]
            tokidx = sb("tokidx", [128, NB], I32)
            S.op("pool", lambda e: e.iota(tokidx[:, :], pattern=[[128, NB]], base=0, channel_multiplier=1), writes=[tokidx])
            yn = 0; sn = 0; mn = 0
            tiles_ = [(s0, min(512, CMAX - s0)) for s0 in range(0, CMAX, 512)]
            def scatter_expert(ex):
                nonlocal sn
                me = metaes[ex % 2]
                me_i = me.t.bitcast(I32)
                S.op("dve", lambda e, me=me, ex=ex: e.tensor_copy(out=me[:, :, 0:1], in_=gate[:, :, ex:ex + 1]), reads=[gate], writes=[me])
                S.op("dve", lambda e, me=me, me_i=me_i: e.tensor_copy(out=me_i[:, :, 1:2], in_=tokidx[:, :].rearrange("p (f o) -> p f o", o=1)), reads=[tokidx], writes=[me])
                for g2 in range(NB // 2):
                    sc = scs[sn % 2]; sn += 1
                    S.dma("sp", sc[:], xn_d.ap()[g2 * 256:(g2 + 1) * 256, :].rearrange("(b p) d -> p b d", p=128), reads=[d_xn], writes=[sc])
                    for b in range(2):
                        f = g2 * 2 + b
                        S.op("pool", lambda e, ex=ex, f=f, b=b, sc=sc: e.indirect_dma_start(
                            out=xes[ex].ap(), out_offset=bass.IndirectOffsetOnAxis(ap=posI[:, f, ex:ex + 1], axis=0),
                            in_=sc[:, b, :], in_offset=None, bounds_check=self.breg(e), oob_is_err=False),
                            reads=[sc, posI], writes=[d_xe[ex]], dma_key=d_xe[ex])
                        S.op("pool", lambda e, ex=ex, f=f, me=me: e.indirect_dma_start(
                            out=meta_ds[ex].ap(), out_offset=bass.IndirectOffsetOnAxis(ap=posI[:, f, ex:ex + 1], axis=0),
                            in_=me[:, f, :], in_offset=None, bounds_check=self.breg(e), oob_is_err=False),
                            reads=[me, posI, d_meta[ex]], writes=[d_meta[ex]], dma_key=d_meta[ex])

            scatter_expert(0)
            for ex in range(NE):
                self.load_w(w1, W["w1"].ap()[ex], 8)
                self.load_w(w3, W["w3"].ap()[ex], 8)
                self.load_w(w2, W["w2"].ap()[ex], NF)
                if ex + 1 < NE:
                    scatter_expert(ex + 1)
                for (s0, tw) in tiles_:
                    nbk = tw // 128
                    S.dma("sp", xe_t[:, 0:nbk, :], xes[ex].ap()[s0:s0 + tw, :].rearrange("(b p) d -> p b d", p=128), reads=[d_xe[ex]], writes=[xe_t])
                    self.transpose_blocks(xe_t, nbk, xeT)
                    for fc in range(NF):
                        p1 = self.nps(); p3 = self.nps()
                        self.proj_fm(p1, w1, fc * 128, xeT, n=tw)
                        self.proj_fm(p3, w3, fc * 128, xeT, n=tw)
                        s1 = s1s[fc % 2]
                        S.op("act", lambda e, p1=p1, s1=s1, tw=tw: e.activation(out=s1[:, 0:tw], in_=p1[:, 0:tw], func=AF.Silu), reads=[p1], writes=[s1])
                        S.op("dve", lambda e, p3=p3, s1=s1, fc=fc, tw=tw: e.tensor_tensor(out=hidT[:, fc, 0:tw], in0=p3[:, 0:tw], in1=s1[:, 0:tw], op=ALU.mult), reads=[p3, s1], writes=[hidT])
                    for b in range(nbk):
                        y_t = y_ts[yn % 2]; yn += 1
                        mt = metats[mn % 4]; mn += 1
                        mt_i = mt.t.bitcast(I32)
                        S.dma("sp", mt[:, :], meta_ds[ex].ap()[s0 + b * 128:s0 + (b + 1) * 128, :], reads=[d_meta[ex]], writes=[mt])
                        for hh in range(2):
                            py = self.nps()
                            for fc in range(NF):
                                S.op("pe", lambda e, py=py, fc=fc, b=b, hh=hh: e.matmul(py[:, :], hidT[:, fc, b * 128:(b + 1) * 128], w2[:, fc, hh * 512:(hh + 1) * 512],
                                                                                        start=(fc == 0), stop=(fc == NF - 1)), reads=[hidT, w2], writes=[py])
                            if hh == 0:
                                S.op("act", lambda e, py=py, y_t=y_t, mt=mt: e.activation(out=y_t[:, 0:512], in_=py[:, :], func=AF.Copy, scale=mt[:, 0:1]), reads=[py, mt], writes=[y_t])
                            else:
                                S.op("dve", lambda e, py=py, y_t=y_t, mt=mt: e.tensor_scalar(out=y_t[:, 512:1024], in0=py[:, :], scalar1=mt[:, 0:1], scalar2=None, op0=ALU.mult), reads=[py, mt], writes=[y_t])
                        S.op("pool", lambda e, y_t=y_t, mt_i=mt_i: e.indirect_dma_start(
                            out=x_out.ap(), out_offset=bass.IndirectOffsetOnAxis(ap=mt_i[:, 1:2], axis=0),
                            in_=y_t[:, :], in_offset=None, bounds_check=self.breg2(e), oob_is_err=False, compute_op=ALU.add),
                            reads=[y_t, mt, d_xacc], writes=[d_xacc], dma_key=d_xacc)
            S.end_phase()


Prog.moe = _moe


def group_mask(cfg):
    res = []
    for segs in core_segments():
        m = np.zeros((cfg.NB,), np.float32)
        bps = cfg.SEG // 128
        for si, (g, q, j) in enumerate(segs):
            m[si * bps:(si + 1) * bps] = 1.0 if g == "p" else 0.0
        res.append(np.ascontiguousarray(np.broadcast_to(m[None, :], (128, cfg.NB))))
    return res


OP, OP_N = _mk_layout([("gamma", D), ("hg", D), ("bg", 16)])


def _odd_mixer(self, x_in, x_out, W, par, flags):
    cfg, S, nc = self.cfg, self.S, self.nc
    TOK, NT = cfg.TOK, cfg.NT
    HD = 256
    hf_d = self.dint(f"hf_{self.cn}", [TOK, HD], F32)
    d_hf = S.buf("d_hf")
    d_xo = S.buf("d_xo_odd")
    for h in range(4):
        for dirn in (0, 1):
            with ExitStack() as ph:
                sb = lambda n, s_, d=F32: self.sb(ph, n, s_, d)
                bwd = dirn == 1
                wq = sb("o_wq", [128, 8, HD], BF16); wk = sb("o_wk", [128, 8, HD], BF16); wv = sb("o_wv", [128, 8, HD], BF16)
                wgt = sb("o_wg", [128, 8, 256], BF16)
                P = sb("o_par", [128, OP_N]); fl = sb("o_fl", [128, 2 * NT])
                win = W["w_in"].ap()
                self.load_w(wq, win[:, h * HD:(h + 1) * HD], 8, ndma=2)
                self.load_w(wk, win[:, D + h * HD:D + (h + 1) * HD], 8, ndma=2)
                self.load_w(wv, win[:, 2 * D + h * HD:2 * D + (h + 1) * HD], 8, ndma=2)
                ci = (8 if bwd else 0) + h
                cf = (12 if bwd else 4) + h
                wgr = W["wg_rep"].ap().rearrange("(k p) n -> p k n", p=128)
                S.dma("pool", wgt[:, :, 0:128], wgr[:, :, ci * 128:(ci + 1) * 128], writes=[wgt])
                S.dma("pool", wgt[:, :, 128:256], wgr[:, :, cf * 128:(cf + 1) * 128], writes=[wgt])
                S.dma("sp", P[:], par.ap(), writes=[P])
                S.dma("sp", fl[:], flags.ap(), writes=[fl])
                if bwd:
                    wo = sb("o_wo", [128, 8, HD], BF16); wout = sb("o_wout", [128, 2, D], BF16)
                    self.load_w(wo, win[:, 3 * D + h * HD:3 * D + (h + 1) * HD], 8, ndma=2)
                    self.load_w(wout, W["w_out"].ap()[h * HD:(h + 1) * HD, :], 2, ndma=1)
                    o_sig = sb("o_sig", [128, 4, HD], BF16)
                    hf_t = sb("hf_t", [128, 4, HD]); hsq = sb("hsq", [128, HD], BF16)
                    hss = sb("hss", [128, 4]); hrs = sb("hrs", [128, 4])
                    hs_b = sb("hs_b", [128, 4, HD], BF16); outT = sb("outT", [128, 2, 512], BF16)
                    xacc = sb("xacc", [128, 4, D])
                x_t = sb("o_xt", [128, 4, D]); xn_b = sb("o_xn", [128, 4, D], BF16); xnT = sb("o_xnT", [128, 8, 512], BF16)
                junk = sb("o_junk", [128, D], BF16); ss = sb("o_ss", [128, 8]); rstd = sb("o_rstd", [128, 8])
                qT = sb("o_qT", [128, 2, 512], BF16); kT = sb("o_kT", [128, 2, 512], BF16)
                ktok = sb("o_ktok", [128, 4, HD], BF16); v_aug = sb("o_vaug", [128, 4, HD + 1], BF16)
                ib = sb("o_ib", [128, 512]); Lb = sb("o_Lb", [128, 512]); Bnb = sb("o_Bnb", [128, 512]); betab = sb("o_beta", [128, 512])
                Rb = sb("o_Rb", [128, 512]); nRb = sb("o_nRb", [128, 512]); BmRb = sb("o_BmR", [128, 512])
                cB = sb("o_cB", [128, 1]); cR = sb("o_cR", [128, 1]); nbf = sb("o_nbf", [128, 1])
                cols = [sb(f"o_cols{i}", [128, 8]) for i in range(2)]
                dj = sb("o_dj", [128, 128])
                Es = [sb(f"o_E{i}", [128, 128]) for i in range(2)]
                smTs = [sb(f"o_smT{i}", [128, 128], BF16) for i in range(2)]
                Bss = [sb(f"o_Bs{i}", [128, HD + 1]) for i in range(2)]
                numas = [sb(f"o_num{i}", [128, HD + 1]) for i in range(2)]
                wgvs = [sb(f"o_wgv{i}", [128, HD + 1], BF16) for i in range(2)]
                C = sb("o_C", [128, 2, HD + 1]); C_bf = sb("o_Cbf", [128, 2, HD + 1], BF16)
                h_t = sb("o_ht", [128, 4, HD])
                d_in = S.buf("d_xin_o")
                S.op("pool", lambda e: e.memset(C[:], 0.0), writes=[C])
                S.op("pool", lambda e: e.memset(C_bf[:], 0.0), writes=[C_bf])
                S.op("pool", lambda e: e.memset(cB[:], 0.0), writes=[cB])
                S.op("pool", lambda e: e.memset(cR[:], 0.0), writes=[cR])
                S.op("pool", lambda e: e.memset(v_aug[:, :, HD:HD + 1], 1.0), writes=[v_aug])
                bfc = OP["bg"] + cf; bic = OP["bg"] + ci
                S.op("dve", lambda e: e.tensor_scalar(out=nbf[:, :], in0=P[:, bfc:bfc + 1], scalar1=-1.0, scalar2=None, op0=ALU.mult), reads=[P], writes=[nbf])
                mask = self.tril_f if bwd else self.tri_f
                tiles = range(NT - 1, -1, -1) if bwd else range(NT)
                cn = 0
                for i in tiles:
                    t0 = i * 512
                    S.dma("sp", x_t[:], x_in.ap()[t0:t0 + 512, :].rearrange("(b p) d -> p b d", p=128), reads=[d_in], writes=[x_t])
                    if bwd:
                        S.dma("sp", hf_t[:], hf_d.ap()[t0:t0 + 512, :].rearrange("(b p) d -> p b d", p=128), reads=[d_hf], writes=[hf_t])
                        src = x_in if h == 0 else x_out
                        S.dma("sp", xacc[:], src.ap()[t0:t0 + 512, :].rearrange("(b p) d -> p b d", p=128), reads=[d_in, d_xo], writes=[xacc])
                    self._rms(x_t, 4, P, OP["gamma"], xn_b, ss, rstd, junk, 128)
                    self.transpose_blocks(xn_b, 4, xnT)
                    for ec in range(2):
                        pq = self.nps(); self.proj_fm(pq, wq, ec * 128, xnT)
                        S.op("act", lambda e, pq=pq, ec=ec: e.copy(out=qT[:, ec, :], in_=pq[:, :]), reads=[pq], writes=[qT])
                        pk = self.nps(); self.proj_fm(pk, wk, ec * 128, xnT)
                        S.op("dve", lambda e, pk=pk, ec=ec: e.tensor_scalar(out=kT[:, ec, :], in0=pk[:, :], scalar1=1.0 / 16.0, scalar2=None, op0=ALU.mult), reads=[pk], writes=[kT])
                    for b in range(4):
                        pk2 = self.nps(); self.proj_tm(pk2, xnT, b * 128, wk, 0, n=HD)
                        S.op("act", lambda e, pk2=pk2, b=b: e.activation(out=ktok[:, b, :], in_=pk2[:, 0:HD], func=AF.Copy, scale=1.0 / 16.0), reads=[pk2], writes=[ktok])
                        pv = self.nps(); self.proj_tm(pv, xnT, b * 128, wv, 0, n=HD)
                        S.op("dve", lambda e, pv=pv, b=b: e.tensor_copy(out=v_aug[:, b, 0:HD], in_=pv[:, 0:HD]), reads=[pv], writes=[v_aug])
                        if bwd:
                            po_ = self.nps(); self.proj_tm(po_, xnT, b * 128, wo, 0, n=HD)
                            S.op("act", lambda e, po_=po_, b=b: e.activation(out=o_sig[:, b, :], in_=po_[:, 0:HD], func=AF.Sigmoid), reads=[po_], writes=[o_sig])
                    pgi = self.nps(); self.proj_fm(pgi, wgt, 0, xnT)
                    pgf = self.nps(); self.proj_fm(pgf, wgt, 128, xnT)
                    S.op("act", lambda e, pgi=pgi: e.activation(out=ib[:, :], in_=pgi[:, :], func=AF.Identity, bias=P[:, bic:bic + 1], scale=1.0), reads=[pgi, P], writes=[ib])
                    S.op("act", lambda e, pgf=pgf: e.activation(out=Lb[:, :], in_=pgf[:, :], func=AF.Exp, bias=nbf[:, 0:1], scale=-1.0), reads=[pgf, nbf], writes=[Lb])
                    S.op("act", lambda e: e.activation(out=Lb[:, :], in_=Lb[:, :], func=AF.Ln, bias=1.0, scale=1.0), reads=[Lb], writes=[Lb])
                    fcol = 2 * i + (1 if bwd else 0)
                    S.op("dve", lambda e, fcol=fcol: e.tensor_scalar(out=cB[:, :], in0=cB[:, :], scalar1=fl[:, fcol:fcol + 1], scalar2=None, op0=ALU.mult), reads=[cB, fl], writes=[cB])
                    S.op("dve", lambda e, fcol=fcol: e.tensor_scalar(out=cR[:, :], in0=cR[:, :], scalar1=fl[:, fcol:fcol + 1], scalar2=None, op0=ALU.mult), reads=[cR, fl], writes=[cR])
                    S.op("dve", lambda e, fcol=fcol: e.tensor_scalar(out=C[:].rearrange("p a b -> p (a b)"), in0=C[:].rearrange("p a b -> p (a b)"),
                                                                     scalar1=fl[:, fcol:fcol + 1], scalar2=None, op0=ALU.mult), reads=[C, fl], writes=[C])
                    S.op("act", lambda e: e.copy(out=C_bf[:].rearrange("p a b -> p (a b)"), in_=C[:].rearrange("p a b -> p (a b)")), reads=[C], writes=[C_bf])
                    rv = (lambda t: t.t[:, ::-1]) if bwd else (lambda t: t.t[:, :])
                    S.op("dve", lambda e: e.tensor_tensor_scan(out=rv(Bnb), data0=self.ones512[:, :], data1=rv(Lb), initial=cB[:, 0:1], op0=ALU.mult, op1=ALU.add),
                         reads=[Lb, cB, self.ones512], writes=[Bnb])
                    S.op("dve", lambda e: e.tensor_tensor(out=betab[:, :], in0=ib[:, :], in1=Bnb[:, :], op=ALU.add), reads=[ib, Bnb], writes=[betab])
                    S.op("dve", lambda e: e.tensor_tensor_scan(out=rv(Rb), data0=self.ones512[:, :], data1=rv(betab), initial=cR[:, 0:1], op0=ALU.mult, op1=ALU.max),
                         reads=[betab, cR, self.ones512], writes=[Rb])
                    S.op("dve", lambda e: e.tensor_scalar(out=nRb[:, :], in0=Rb[:, :], scalar1=-1.0, scalar2=None, op0=ALU.mult), reads=[Rb], writes=[nRb])
                    S.op("dve", lambda e: e.tensor_tensor(out=BmRb[:, :], in0=Bnb[:, :], in1=Rb[:, :], op=ALU.subtract), reads=[Bnb, Rb], writes=[BmRb])
                    chunks = range(3, -1, -1) if bwd else range(4)
                    first = True
                    for c in chunks:
                        c0 = c * 128
                        endc = c0 if bwd else c0 + 127
                        if first:
                            mu_buf, mu_ap = cR, cR.t[:, 0:1]
                        else:
                            pc_ = (c + 1) * 128 if bwd else c0 - 1
                            mu_buf, mu_ap = Rb, Rb.t[:, pc_:pc_ + 1]
                        first = False
                        col = cols[cn % 2]; E = Es[cn % 2]; smT = smTs[cn % 2]; Bs = Bss[cn % 2]; numa = numas[cn % 2]; wgv = wgvs[cn % 2]
                        cn += 1
                        for (src_b, ci_) in ((betab, 0), (Rb, 1), (BmRb, 2)):
                            S.op("dve", lambda e, src_b=src_b, ci_=ci_, col=col, c0=c0: e.scalar_tensor_tensor(out=dj[:, :], in0=src_b[:, c0:c0 + 128], scalar=1.0, in1=self.id_f[:, :],
                                                                                                       op0=ALU.mult, op1=ALU.mult, accum_out=col[:, ci_:ci_ + 1]),
                                 reads=[src_b, self.id_f], writes=[dj, col])
                        S.op("act", lambda e, col=col, mu_ap=mu_ap: e.activation(out=col[:, 3:4], in_=col[:, 1:2], func=AF.Exp, bias=mu_ap, scale=-1.0), reads=[col, mu_buf], writes=[col])
                        S.op("act", lambda e, col=col: e.activation(out=col[:, 4:5], in_=col[:, 2:3], func=AF.Exp), reads=[col], writes=[col])
                        S.op("act", lambda e, col=col, endc=endc: e.activation(out=col[:, 5:6], in_=col[:, 0:1], func=AF.Exp, bias=nRb[:, endc:endc + 1], scale=1.0), reads=[col, nRb], writes=[col])
                        S.op("act", lambda e, col=col, endc=endc, mu_ap=mu_ap: e.activation(out=col[:, 6:7], in_=mu_ap, func=AF.Exp, bias=nRb[:, endc:endc + 1], scale=1.0),
                             reads=[col, nRb, mu_buf], writes=[col])
                        psS = self.nps()
                        for ec in range(2):
                            S.op("pe", lambda e, psS=psS, ec=ec, c0=c0: e.matmul(psS[:, 0:128], kT[:, ec, c0:c0 + 128], qT[:, ec, c0:c0 + 128], start=(ec == 0), stop=(ec == 1)),
                                 reads=[kT, qT], writes=[psS])
                        S.op("act", lambda e, E=E, col=col, c0=c0: e.activation(out=E[:, :], in_=Rb[:, c0:c0 + 128], func=AF.Exp, bias=col[:, 0:1], scale=-1.0), reads=[Rb, col], writes=[E])
                        S.op("dve", lambda e, E=E: e.tensor_tensor(out=E[:, :], in0=E[:, :], in1=mask[:, :], op=ALU.mult), reads=[E, mask], writes=[E])
                        S.op("dve", lambda e, E=E, smT=smT, psS=psS: e.tensor_tensor(out=smT[:, :], in0=psS[:, 0:128], in1=E[:, :], op=ALU.mult), reads=[psS, E], writes=[smT])
                        psA = self.nps(); psB = self.nps()
                        S.op("pe", lambda e, psA=psA, smT=smT, c=c: e.matmul(psA[:, 0:HD + 1], smT[:, :], v_aug[:, c, :], start=True, stop=True), reads=[smT, v_aug], writes=[psA])
                        for ec in range(2):
                            S.op("pe", lambda e, psB=psB, ec=ec, c0=c0: e.matmul(psB[:, 0:HD + 1], qT[:, ec, c0:c0 + 128], C_bf[:, ec, :], start=(ec == 0), stop=(ec == 1)),
                                 reads=[qT, C_bf], writes=[psB])
                        S.op("act", lambda e, psB=psB, Bs=Bs, col=col: e.activation(out=Bs[:, :], in_=psB[:, 0:HD + 1], func=AF.Copy, scale=col[:, 3:4]), reads=[psB, col], writes=[Bs])
                        S.op("dve", lambda e, psA=psA, Bs=Bs, numa=numa: e.tensor_tensor(out=numa[:, :], in0=psA[:, 0:HD + 1], in1=Bs[:, :], op=ALU.add), reads=[psA, Bs], writes=[numa])
                        S.op("act", lambda e, numa=numa, col=col: e.activation(out=col[:, 7:8], in_=numa[:, HD:HD + 1], func=AF.Abs), reads=[numa], writes=[col])
                        S.op("dve", lambda e, col=col: e.tensor_tensor(out=col[:, 7:8], in0=col[:, 7:8], in1=col[:, 4:5], op=ALU.max), reads=[col], writes=[col])
                        S.op("dve", lambda e, col=col: e.reciprocal(out=col[:, 7:8], in_=col[:, 7:8]), reads=[col], writes=[col])
                        S.op("act", lambda e, numa=numa, col=col, c=c: e.activation(out=h_t[:, c, :], in_=numa[:, 0:HD], func=AF.Copy, scale=col[:, 7:8]), reads=[numa, col], writes=[h_t])
                        S.op("dve", lambda e, wgv=wgv, col=col, c=c: e.tensor_scalar(out=wgv[:, :], in0=v_aug[:, c, :], scalar1=col[:, 5:6], scalar2=None, op0=ALU.mult), reads=[v_aug, col], writes=[wgv])
                        for ec in range(2):
                            psU = self.nps()
                            S.op("pe", lambda e, psU=psU, ec=ec, wgv=wgv, c=c: e.matmul(psU[:, 0:HD + 1], ktok[:, c, ec * 128:(ec + 1) * 128], wgv[:, :], start=True, stop=True),
                                 reads=[ktok, wgv], writes=[psU])
                            S.op("dve", lambda e, psU=psU, ec=ec, col=col: e.scalar_tensor_tensor(out=C[:, ec, :], in0=C[:, ec, :], scalar=col[:, 6:7], in1=psU[:, 0:HD + 1],
                                                                                               op0=ALU.mult, op1=ALU.add), reads=[C, col, psU], writes=[C])
                        S.op("act", lambda e: e.copy(out=C_bf[:].rearrange("p a b -> p (a b)"), in_=C[:].rearrange("p a b -> p (a b)")), reads=[C], writes=[C_bf])
                    endt = 0 if bwd else 511
                    S.op("dve", lambda e, endt=endt: e.tensor_copy(out=cB[:, :], in_=Bnb[:, endt:endt + 1]), reads=[Bnb], writes=[cB])
                    S.op("dve", lambda e, endt=endt: e.tensor_copy(out=cR[:, :], in_=Rb[:, endt:endt + 1]), reads=[Rb], writes=[cR])
                    if not bwd:
                        S.dma("sp", hf_d.ap()[t0:t0 + 512, :].rearrange("(b p) d -> p b d", p=128), h_t[:], reads=[h_t], writes=[d_hf])
                    else:
                        S.op("dve", lambda e: e.tensor_tensor(out=h_t[:], in0=h_t[:], in1=hf_t[:], op=ALU.add), reads=[h_t, hf_t], writes=[h_t])
                        for b in range(4):
                            S.op("act", lambda e, b=b: e.activation(out=hsq[:, :], in_=h_t[:, b, :], func=AF.Square, accum_out=hss[:, b:b + 1]), reads=[h_t], writes=[hsq, hss])
                        S.op("act", lambda e: e.activation(out=hrs[:, :], in_=hss[:, :], func=AF.Ln, bias=EPS, scale=1.0 / HD), reads=[hss], writes=[hrs])
                        S.op("act", lambda e: e.activation(out=hrs[:, :], in_=hrs[:, :], func=AF.Exp, scale=-0.5), reads=[hrs], writes=[hrs])
                        hg0 = OP["hg"] + h * HD
                        for b in range(4):
                            S.op("dve", lambda e, b=b: e.scalar_tensor_tensor(out=h_t[:, b, :], in0=h_t[:, b, :], scalar=hrs[:, b:b + 1], in1=P[:, hg0:hg0 + HD], op0=ALU.mult, op1=ALU.mult),
                                 reads=[h_t, hrs, P], writes=[h_t])
                        S.op("dve", lambda e: e.tensor_tensor(out=hs_b[:], in0=h_t[:], in1=o_sig[:], op=ALU.mult), reads=[h_t, o_sig], writes=[hs_b])
                        for b in range(4):
                            pt = self.nps(); ptb = pt.t.bitcast(BF16)
                            for dc in range(2):
                                S.op("pe", lambda e, ptb=ptb, pt=pt, b=b, dc=dc: e.transpose(ptb[:, dc * 128:(dc + 1) * 128], hs_b[:, b, dc * 128:(dc + 1) * 128], self.id_b[:, :]),
                                     reads=[hs_b, self.id_b], writes=[pt])
                            S.op("act", lambda e, ptb=ptb, pt=pt, b=b: e.copy(out=outT[:, :, b * 128:(b + 1) * 128], in_=ptb[:, 0:256].rearrange("p (k t) -> p k t", k=2)), reads=[pt], writes=[outT])
                        for b in range(4):
                            for hh in range(2):
                                pp = self.nps()
                                for dc in range(2):
                                    S.op("pe", lambda e, pp=pp, b=b, hh=hh, dc=dc: e.matmul(pp[:, :], outT[:, dc, b * 128:(b + 1) * 128], wout[:, dc, hh * 512:(hh + 1) * 512],
                                                                                           start=(dc == 0), stop=(dc == 1)), reads=[outT, wout], writes=[pp])
                                S.op("dve", lambda e, pp=pp, b=b, hh=hh: e.tensor_tensor(out=xacc[:, b, hh * 512:(hh + 1) * 512], in0=xacc[:, b, hh * 512:(hh + 1) * 512], in1=pp[:, :], op=ALU.add),
                                     reads=[xacc, pp], writes=[xacc])
                        S.dma("sp", x_out.ap()[t0:t0 + 512, :].rearrange("(b p) d -> p b d", p=128), xacc[:], reads=[xacc], writes=[d_xo])
                S.end_phase()


Prog.odd_mixer = _odd_mixer


def pack_odd_params(norm_mix_l, hnorm_g, b_gate):
    P = np.zeros((128, OP_N), np.float32)
    P[:, OP["gamma"]:OP["gamma"] + D] = bc(norm_mix_l)
    P[:, OP["hg"]:OP["hg"] + D] = bc(hnorm_g.reshape(-1))
    P[:, OP["bg"]:OP["bg"] + 16] = bc(b_gate.reshape(-1))
    return P


def gate_rep(w_in_odd):
    g = w_in_odd[:, 4 * D:4 * D + 16]
    return np.ascontiguousarray(np.repeat(g, 128, axis=1))


def _xattn_inputs(p):
    return ({"wq": p.din("wq", [D, D]), "wkv": p.din("wkv", [D, 2 * D]), "wo": p.din("wo", [D, D]), "wr": p.din("wr", [D, NE])},
            p.din("x_par", [128, XP_N]), p.din("mem", [p.cfg.NSEG, MEM, D]))


def _moe_inputs(p):
    cfg = p.cfg
    return (p.din("aff_all", [128, NCORES * cfg.NB * NE]), p.din("aff_loc", [128, cfg.NB * NE]), p.din("gm", [128, cfg.NB]),
            {"w1": p.din("w1", [NE, D, cfg.FF]), "w3": p.din("w3", [NE, D, cfg.FF]), "w2": p.din("w2", [NE, cfg.FF, D])},
            p.din("m_par", [128, D]), p.dout("cnt", [128, NE]))


def build_L1(cfg):
    p = Prog(cfg)
    TOK, NT = cfg.TOK, cfg.NT
    x = p.din("x", [TOK, D]); flags = p.din("flags", [128, 2 * NT])
    ew = {"w_in": p.din("e_w_in", [D, 2048]), "w_out": p.din("e_w_out", [D, D]), "wsT": p.din("e_wsT", [128, 4, 128])}
    epar = p.din("e_par", [128, EP_N])
    xw, xpar, mem = _xattn_inputs(p)
    x1 = p.dint("x1", [TOK, D]); xo = p.dout("xo", [TOK, D]); aff = p.dout("aff", [128, cfg.NB * NE])
    p.consts()
    p.even_mixer(x, x1, ew, epar, flags)
    p.xattn_router(x1, xo, aff, xw, xpar, mem)
    p.st.close()
    return p


def build_L2(cfg):
    p = Prog(cfg)
    TOK, NT = cfg.TOK, cfg.NT
    x = p.din("x", [TOK, D]); flags = p.din("flags", [128, 2 * NT])
    aff_all, aff_loc, gm, mw, mpar, cnt = _moe_inputs(p)
    ow = {"w_in": p.din("o_w_in", [D, 4 * D + 16]), "wg_rep": p.din("o_wg_rep", [D, 16 * 128]), "w_out": p.din("o_w_out", [D, D])}
    opar = p.din("o_par", [128, OP_N])
    xw, xpar, mem = _xattn_inputs(p)
    x3 = p.dint("x3", [TOK, D]); x4 = p.dint("x4", [TOK, D]); xo = p.dout("xo", [TOK, D]); aff = p.dout("aff", [128, cfg.NB * NE])
    p.consts()
    p.moe(x, x3, aff_all, aff_loc, gm, mw, mpar, cnt)
    p.odd_mixer(x3, x4, ow, opar, flags)
    p.xattn_router(x4, xo, aff, xw, xpar, mem)
    p.st.close()
    return p


def build_L3(cfg):
    p = Prog(cfg)
    TOK = cfg.TOK
    x = p.din("x", [TOK, D])
    aff_all, aff_loc, gm, mw, mpar, cnt = _moe_inputs(p)
    fpar = p.din("f_par", [128, D])
    x6 = p.dint("x6", [TOK, D]); y = p.dout("y", [TOK, D])
    p.consts()
    p.moe(x, x6, aff_all, aff_loc, gm, mw, mpar, cnt)
    p.final_norm(x6, y, fpar)
    p.st.close()
    return p


def _launch(p, in_maps):
    res = run_bass_kernel_spmd(p.nc, in_maps, core_ids=list(range(NCORES)))
    return res.results


def _check_capacity(cfg, results):
    import sys
    allc = np.stack([r["cnt"][0] for r in results], 0)
    print(f"[moe] per-(core,expert) counts: mean={allc.mean():.1f} min={allc.min():.0f} max={allc.max():.0f} CMAX={cfg.CMAX}", file=sys.stderr)
    for c, r in enumerate(results):
        mx = float(np.max(r["cnt"][0]))
        if mx > cfg.CMAX:
            raise RuntimeError(f"MoE per-core expert capacity exceeded on core {c}: {mx} > {cfg.CMAX} "
                               "(static capacity assumption violated; refusing to return a silently wrong result)")


def run_model(cfg, I):
    f32 = lambda a: np.ascontiguousarray(np.asarray(a, dtype=np.float32))
    I = {k: f32(v) for k, v in I.items()}
    xsh = shard_tokens(cfg, I["x_prompt"], I["x_sample"])
    msh = shard_mem(cfg, I["mem_prompt"], I["mem_sample"])
    fl = tile_flags(cfg)
    gms = group_mask(cfg)

    def xattn_common(l):
        return {"wq": I["xa_wq"][l], "wkv": I["xa_wkv"][l], "wo": I["xa_wo"][l], "wr": I["moe_router"][l],
                "x_par": pack_xattn_params(I["norm_xattn"][l], I["norm_ffn"][l])}

    def moe_common(l, aff_locs):
        aff_all = np.ascontiguousarray(np.concatenate(aff_locs, axis=1))
        return {"aff_all": aff_all, "w1": I["moe_w1"][l], "w3": I["moe_w3"][l], "w2": I["moe_w2"][l],
                "m_par": np.ascontiguousarray(bc(I["norm_ffn"][l]))}

    p1 = build_L1(cfg)
    c1 = dict(xattn_common(0))
    c1.update({"e_w_in": I["even_w_in"][0], "e_w_out": I["even_w_out"][0],
               "e_wsT": np.ascontiguousarray(I["a_ws"][0].transpose(2, 0, 1)),
               "e_par": pack_even_params(I["norm_mix"][0], I["a_ln_g"][0], I["a_ln_b"][0], I["a_bs"][0], I["b_conv_w"][0],
                                         I["b_conv_b"][0], I["b_ln_g"][0], I["b_ln_b"][0])})
    r1 = _launch(p1, [dict(c1, x=xsh[c], mem=msh[c], flags=fl[c]) for c in range(NCORES)])
    del p1
    p2 = build_L2(cfg)
    c2 = dict(xattn_common(1))
    c2.update(moe_common(0, [r["aff"] for r in r1]))
    c2.update({"o_w_in": I["odd_w_in"][0], "o_wg_rep": gate_rep(I["odd_w_in"][0]), "o_w_out": I["odd_w_out"][0],
               "o_par": pack_odd_params(I["norm_mix"][1], I["odd_hnorm_g"][0], I["odd_b_gate"][0])})
    r2 = _launch(p2, [dict(c2, x=r1[c]["xo"], aff_loc=r1[c]["aff"], gm=gms[c], mem=msh[c], flags=fl[c]) for c in range(NCORES)])
    _check_capacity(cfg, r2)
    del p2, r1
    p3 = build_L3(cfg)
    c3 = moe_common(1, [r["aff"] for r in r2])
    c3["f_par"] = np.ascontiguousarray(bc(I["norm_final"]))
    r3 = _launch(p3, [dict(c3, x=r2[c]["xo"], aff_loc=r2[c]["aff"], gm=gms[c]) for c in range(NCORES)])
    _check_capacity(cfg, r3)
    yp, ys = unshard_tokens(cfg, [r["y"] for r in r3])
    return yp, ys


def kernel(**inputs):
    cfg = Cfg(seg=2048, ff=2048)
    yp, ys = run_model(cfg, inputs)
    return (yp.astype(np.float32), ys.astype(np.float32))


def _odd_mixer2(self, x_in, x_out, W, par, flags):
    cfg, S, nc = self.cfg, self.S, self.nc
    TOK, NT = cfg.TOK, cfg.NT
    HD = 256
    hf_ds = [self.dint(f"hf{h}_{self.cn}", [TOK, HD], F32) for h in range(4)]
    d_hf = [S.buf(f"d_hf{h}") for h in range(4)]
    d_xo = S.buf("d_xo_odd")
    win = W["w_in"].ap()
    wgr = W["wg_rep"].ap().rearrange("(k p) n -> p k n", p=128)
    for pair in ((0, 1), (2, 3)):
        for dirn in (0, 1):
            with ExitStack() as ph:
                sb = lambda n, s_, d=F32: self.sb(ph, n, s_, d)
                bwd = dirn == 1
                P = sb("o_par", [128, OP_N]); fl = sb("o_fl", [128, 2 * NT])
                S.dma("sp", P[:], par.ap(), writes=[P])
                S.dma("sp", fl[:], flags.ap(), writes=[fl])
                x_t = sb("o_xt", [128, 4, D]); xn_b = sb("o_xn", [128, 4, D], BF16); xnT = sb("o_xnT", [128, 8, 512], BF16)
                junk = sb("o_junk", [128, D], BF16); ss = sb("o_ss", [128, 8]); rstd = sb("o_rstd", [128, 8])
                d_in = S.buf("d_xin_o")
                mask = self.tril_f if bwd else self.tri_f
                HB = {}
                for h in pair:
                    B = {}
                    B["wq"] = sb(f"wq{h}", [128, 8, HD], BF16); B["wk"] = sb(f"wk{h}", [128, 8, HD], BF16); B["wv"] = sb(f"wv{h}", [128, 8, HD], BF16)
                    B["wgt"] = sb(f"wg{h}", [128, 8, 256], BF16)
                    self.load_w(B["wq"], win[:, h * HD:(h + 1) * HD], 8, ndma=2)
                    self.load_w(B["wk"], win[:, D + h * HD:D + (h + 1) * HD], 8, ndma=2)
                    self.load_w(B["wv"], win[:, 2 * D + h * HD:2 * D + (h + 1) * HD], 8, ndma=2)
                    ci = (8 if bwd else 0) + h
                    cf = (12 if bwd else 4) + h
                    B["ci"], B["cf"] = ci, cf
                    S.dma("pool", B["wgt"][:, :, 0:128], wgr[:, :, ci * 128:(ci + 1) * 128], writes=[B["wgt"]])
                    S.dma("pool", B["wgt"][:, :, 128:256], wgr[:, :, cf * 128:(cf + 1) * 128], writes=[B["wgt"]])
                    if bwd:
                        B["wo"] = sb(f"wo{h}", [128, 8, HD], BF16); B["wout"] = sb(f"wout{h}", [128, 2, D], BF16)
                        self.load_w(B["wo"], win[:, 3 * D + h * HD:3 * D + (h + 1) * HD], 8, ndma=2)
                        self.load_w(B["wout"], W["w_out"].ap()[h * HD:(h + 1) * HD, :], 2, ndma=1)
                        B["o_sig"] = sb(f"osig{h}", [128, 4, HD], BF16)
                        B["hf_t"] = sb(f"hft{h}", [128, 4, HD])
                        B["hss"] = sb(f"hss{h}", [128, 4]); B["hrs"] = sb(f"hrs{h}", [128, 4])
                        B["hs_b"] = sb(f"hsb{h}", [128, 4, HD], BF16); B["outT"] = sb(f"outT{h}", [128, 2, 512], BF16)
                    B["qT"] = sb(f"qT{h}", [128, 2, 512], BF16); B["kT"] = sb(f"kT{h}", [128, 2, 512], BF16)
                    B["ktok"] = sb(f"ktok{h}", [128, 4, HD], BF16); B["v_aug"] = sb(f"vaug{h}", [128, 4, HD + 1], BF16)
                    B["ib"] = sb(f"ib{h}", [128, 512]); B["Lb"] = sb(f"Lb{h}", [128, 512]); B["Bnb"] = sb(f"Bnb{h}", [128, 512])
                    B["betab"] = sb(f"beta{h}", [128, 512]); B["Rb"] = sb(f"Rb{h}", [128, 512])
                    B["cB"] = sb(f"cB{h}", [128, 1]); B["cR"] = sb(f"cR{h}", [128, 1]); B["nbf"] = sb(f"nbf{h}", [128, 1])
                    B["cols"] = [sb(f"cols{h}_{i}", [128, 12]) for i in range(2)]
                    B["dj"] = sb(f"dj{h}", [128, 128])
                    B["E"] = sb(f"E{h}", [128, 128]); B["smT"] = sb(f"smT{h}", [128, 128], BF16)
                    B["Bs"] = sb(f"Bs{h}", [128, HD + 1]); B["numa"] = sb(f"num{h}", [128, HD + 1]); B["wgv"] = sb(f"wgv{h}", [128, HD + 1], BF16)
                    B["C"] = sb(f"C{h}", [128, 2, HD + 1]); B["C_bf"] = sb(f"Cbf{h}", [128, 2, HD + 1], BF16)
                    B["h_t"] = sb(f"ht{h}", [128, 4, HD])
                    B["cn"] = 0
                    C, C_bf, cB, cR, v_aug, nbf = B["C"], B["C_bf"], B["cB"], B["cR"], B["v_aug"], B["nbf"]
                    S.op("pool", lambda e, C=C: e.memset(C[:], 0.0), writes=[C])
                    S.op("pool", lambda e, C_bf=C_bf: e.memset(C_bf[:], 0.0), writes=[C_bf])
                    S.op("pool", lambda e, cB=cB: e.memset(cB[:], 0.0), writes=[cB])
                    S.op("pool", lambda e, cR=cR: e.memset(cR[:], 0.0), writes=[cR])
                    S.op("pool", lambda e, v_aug=v_aug: e.memset(v_aug[:, :, HD:HD + 1], 1.0), writes=[v_aug])
                    bfc = OP["bg"] + cf
                    S.op("dve", lambda e, nbf=nbf, bfc=bfc: e.tensor_scalar(out=nbf[:, :], in0=P[:, bfc:bfc + 1], scalar1=-1.0, scalar2=None, op0=ALU.mult), reads=[P], writes=[nbf])
                    HB[h] = B

                def gen_head(h, i):
                    B = HB[h]
                    wq, wk, wv, wgt = B["wq"], B["wk"], B["wv"], B["wgt"]
                    qT, kT, ktok, v_aug = B["qT"], B["kT"], B["ktok"], B["v_aug"]
                    ib, Lb, Bnb, betab, Rb = B["ib"], B["Lb"], B["Bnb"], B["betab"], B["Rb"]
                    cB, cR, nbf, dj = B["cB"], B["cR"], B["nbf"], B["dj"]
                    E, smT, Bs, numa, wgv, C, C_bf, h_t = B["E"], B["smT"], B["Bs"], B["numa"], B["wgv"], B["C"], B["C_bf"], B["h_t"]
                    bic = OP["bg"] + B["ci"]
                    t0 = i * 512
                    for ec in range(2):
                        pq = self.nps(); self.proj_fm(pq, wq, ec * 128, xnT)
                        S.op("act", lambda e, pq=pq, ec=ec: e.copy(out=qT[:, ec, :], in_=pq[:, :]), reads=[pq], writes=[qT])
                        pk = self.nps(); self.proj_fm(pk, wk, ec * 128, xnT)
                        S.op("dve", lambda e, pk=pk, ec=ec: e.tensor_scalar(out=kT[:, ec, :], in0=pk[:, :], scalar1=1.0 / 16.0, scalar2=None, op0=ALU.mult), reads=[pk], writes=[kT])
                        yield
                    for b in range(4):
                        pk2 = self.nps(); self.proj_tm(pk2, xnT, b * 128, wk, 0, n=HD)
                        S.op("act", lambda e, pk2=pk2, b=b: e.activation(out=ktok[:, b, :], in_=pk2[:, 0:HD], func=AF.Copy, scale=1.0 / 16.0), reads=[pk2], writes=[ktok])
                        pv = self.nps(); self.proj_tm(pv, xnT, b * 128, wv, 0, n=HD)
                        S.op("dve", lambda e, pv=pv, b=b: e.tensor_copy(out=v_aug[:, b, 0:HD], in_=pv[:, 0:HD]), reads=[pv], writes=[v_aug])
                        if bwd:
                            po_ = self.nps(); self.proj_tm(po_, xnT, b * 128, B["wo"], 0, n=HD)
                            S.op("act", lambda e, po_=po_, b=b: e.activation(out=B["o_sig"][:, b, :], in_=po_[:, 0:HD], func=AF.Sigmoid), reads=[po_], writes=[B["o_sig"]])
                        yield
                    pgi = self.nps(); self.proj_fm(pgi, wgt, 0, xnT)
                    pgf = self.nps(); self.proj_fm(pgf, wgt, 128, xnT)
                    S.op("act", lambda e, pgi=pgi: e.activation(out=ib[:, :], in_=pgi[:, :], func=AF.Identity, bias=P[:, bic:bic + 1], scale=1.0), reads=[pgi, P], writes=[ib])
                    S.op("act", lambda e, pgf=pgf: e.activation(out=Lb[:, :], in_=pgf[:, :], func=AF.Exp, bias=nbf[:, 0:1], scale=-1.0), reads=[pgf, nbf], writes=[Lb])
                    S.op("act", lambda e: e.activation(out=Lb[:, :], in_=Lb[:, :], func=AF.Ln, bias=1.0, scale=1.0), reads=[Lb], writes=[Lb])
                    fcol = 2 * i + (1 if bwd else 0)
                    S.op("dve", lambda e: e.tensor_scalar(out=cB[:, :], in0=cB[:, :], scalar1=fl[:, fcol:fcol + 1], scalar2=None, op0=ALU.mult), reads=[cB, fl], writes=[cB])
                    S.op("dve", lambda e: e.tensor_scalar(out=cR[:, :], in0=cR[:, :], scalar1=fl[:, fcol:fcol + 1], scalar2=None, op0=ALU.mult), reads=[cR, fl], writes=[cR])
                    S.op("dve", lambda e: e.tensor_scalar(out=C[:].rearrange("p a b -> p (a b)"), in0=C[:].rearrange("p a b -> p (a b)"),
                                                          scalar1=fl[:, fcol:fcol + 1], scalar2=None, op0=ALU.mult), reads=[C, fl], writes=[C])
                    S.op("act", lambda e: e.copy(out=C_bf[:].rearrange("p a b -> p (a b)"), in_=C[:].rearrange("p a b -> p (a b)")), reads=[C], writes=[C_bf])
                    yield
                    rv = (lambda t: t.t[:, ::-1]) if bwd else (lambda t: t.t[:, :])
                    S.op("dve", lambda e: e.tensor_tensor_scan(out=rv(Bnb), data0=self.ones512[:, :], data1=rv(Lb), initial=cB[:, 0:1], op0=ALU.mult, op1=ALU.add),
                         reads=[Lb, cB, self.ones512], writes=[Bnb])
                    S.op("dve", lambda e: e.tensor_tensor(out=betab[:, :], in0=ib[:, :], in1=Bnb[:, :], op=ALU.add), reads=[ib, Bnb], writes=[betab])
                    S.op("dve", lambda e: e.tensor_tensor_scan(out=rv(Rb), data0=self.ones512[:, :], data1=rv(betab), initial=cR[:, 0:1], op0=ALU.mult, op1=ALU.max),
                         reads=[betab, cR, self.ones512], writes=[Rb])
                    yield
                    chunks = range(3, -1, -1) if bwd else range(4)
                    first = True
                    for c in chunks:
                        c0 = c * 128
                        endc = c0 if bwd else c0 + 127
                        if first:
                            mu_buf, mu_ap = cR, cR.t[:, 0:1]
                        else:
                            pc_ = (c + 1) * 128 if bwd else c0 - 1
                            mu_buf, mu_ap = Rb, Rb.t[:, pc_:pc_ + 1]
                        first = False
                        col = B["cols"][B["cn"] % 2]; B["cn"] += 1
                        for (src_b, ci_) in ((betab, 0), (Rb, 1), (Bnb, 2)):
                            S.op("dve", lambda e, src_b=src_b, ci_=ci_, col=col, c0=c0: e.scalar_tensor_tensor(out=dj[:, :], in0=src_b[:, c0:c0 + 128], scalar=1.0, in1=self.id_f[:, :],
                                                                                                              op0=ALU.mult, op1=ALU.mult, accum_out=col[:, ci_:ci_ + 1]),
                                 reads=[src_b, self.id_f], writes=[dj, col])
                        S.op("dve", lambda e, col=col, endc=endc: e.tensor_scalar(out=col[:, 8:9], in0=Rb[:, endc:endc + 1], scalar1=-1.0, scalar2=None, op0=ALU.mult), reads=[Rb], writes=[col])
                        S.op("dve", lambda e, col=col: e.tensor_tensor(out=col[:, 9:10], in0=col[:, 2:3], in1=col[:, 1:2], op=ALU.subtract), reads=[col], writes=[col])
                        yield
                        S.op("act", lambda e, col=col, mu_ap=mu_ap, mu_buf=mu_buf: e.activation(out=col[:, 3:4], in_=col[:, 1:2], func=AF.Exp, bias=mu_ap, scale=-1.0), reads=[col, mu_buf], writes=[col])
                        S.op("act", lambda e, col=col: e.activation(out=col[:, 4:5], in_=col[:, 9:10], func=AF.Exp), reads=[col], writes=[col])
                        S.op("act", lambda e, col=col: e.activation(out=col[:, 5:6], in_=col[:, 0:1], func=AF.Exp, bias=col[:, 8:9], scale=1.0), reads=[col], writes=[col])
                        S.op("act", lambda e, col=col, mu_ap=mu_ap, mu_buf=mu_buf: e.activation(out=col[:, 6:7], in_=mu_ap, func=AF.Exp, bias=col[:, 8:9], scale=1.0),
                             reads=[col, mu_buf], writes=[col])
                        psS = self.nps()
                        for ec in range(2):
                            S.op("pe", lambda e, psS=psS, ec=ec, c0=c0: e.matmul(psS[:, 0:128], kT[:, ec, c0:c0 + 128], qT[:, ec, c0:c0 + 128], start=(ec == 0), stop=(ec == 1)),
                                 reads=[kT, qT], writes=[psS])
                        S.op("act", lambda e, col=col, c0=c0: e.activation(out=E[:, :], in_=Rb[:, c0:c0 + 128], func=AF.Exp, bias=col[:, 0:1], scale=-1.0), reads=[Rb, col], writes=[E])
                        yield
                        S.op("dve", lambda e: e.tensor_tensor(out=E[:, :], in0=E[:, :], in1=mask[:, :], op=ALU.mult), reads=[E, mask], writes=[E])
                        S.op("dve", lambda e, psS=psS: e.tensor_tensor(out=smT[:, :], in0=psS[:, 0:128], in1=E[:, :], op=ALU.mult), reads=[psS, E], writes=[smT])
                        psA = self.nps(); psB = self.nps()
                        S.op("pe", lambda e, psA=psA, c=c: e.matmul(psA[:, 0:HD + 1], smT[:, :], v_aug[:, c, :], start=True, stop=True), reads=[smT, v_aug], writes=[psA])
                        for ec in range(2):
                            S.op("pe", lambda e, psB=psB, ec=ec, c0=c0: e.matmul(psB[:, 0:HD + 1], qT[:, ec, c0:c0 + 128], C_bf[:, ec, :], start=(ec == 0), stop=(ec == 1)),
                                 reads=[qT, C_bf], writes=[psB])
                        S.op("act", lambda e, psB=psB, col=col: e.activation(out=Bs[:, :], in_=psB[:, 0:HD + 1], func=AF.Copy, scale=col[:, 3:4]), reads=[psB, col], writes=[Bs])
                        yield
                        S.op("dve", lambda e, psA=psA: e.tensor_tensor(out=numa[:, :], in0=psA[:, 0:HD + 1], in1=Bs[:, :], op=ALU.add), reads=[psA, Bs], writes=[numa])
                        S.op("act", lambda e, col=col: e.activation(out=col[:, 7:8], in_=numa[:, HD:HD + 1], func=AF.Abs), reads=[numa], writes=[col])
                        S.op("dve", lambda e, col=col: e.tensor_tensor(out=col[:, 7:8], in0=col[:, 7:8], in1=col[:, 4:5], op=ALU.max), reads=[col], writes=[col])
                        S.op("dve", lambda e, col=col: e.reciprocal(out=col[:, 7:8], in_=col[:, 7:8]), reads=[col], writes=[col])
                        S.op("act", lambda e, col=col, c=c: e.activation(out=h_t[:, c, :], in_=numa[:, 0:HD], func=AF.Copy, scale=col[:, 7:8]), reads=[numa, col], writes=[h_t])
                        yield
                        S.op("dve", lambda e, col=col, c=c: e.tensor_scalar(out=wgv[:, :], in0=v_aug[:, c, :], scalar1=col[:, 5:6], scalar2=None, op0=ALU.mult), reads=[v_aug, col], writes=[wgv])
                        for ec in range(2):
                            psU = self.nps()
                            S.op("pe", lambda e, psU=psU, ec=ec, c=c: e.matmul(psU[:, 0:HD + 1], ktok[:, c, ec * 128:(ec + 1) * 128], wgv[:, :], start=True, stop=True),
                                 reads=[ktok, wgv], writes=[psU])
                            S.op("dve", lambda e, psU=psU, ec=ec, col=col: e.scalar_tensor_tensor(out=C[:, ec, :], in0=C[:, ec, :], scalar=col[:, 6:7], in1=psU[:, 0:HD + 1],
                                                                                               op0=ALU.mult, op1=ALU.add), reads=[C, col, psU], writes=[C])
                        S.op("act", lambda e: e.copy(out=C_bf[:].rearrange("p a b -> p (a b)"), in_=C[:].rearrange("p a b -> p (a b)")), reads=[C], writes=[C_bf])
                        yield
                    endt = 0 if bwd else 511
                    S.op("dve", lambda e: e.tensor_copy(out=cB[:, :], in_=Bnb[:, endt:endt + 1]), reads=[Bnb], writes=[cB])
                    S.op("dve", lambda e: e.tensor_copy(out=cR[:, :], in_=Rb[:, endt:endt + 1]), reads=[Rb], writes=[cR])
                    if not bwd:
                        S.dma("sp", hf_ds[h].ap()[t0:t0 + 512, :].rearrange("(b p) d -> p b d", p=128), h_t[:], reads=[h_t], writes=[d_hf[h]])
                    else:
                        hf_t, hss, hrs, hs_b, outT, o_sig, wout = B["hf_t"], B["hss"], B["hrs"], B["hs_b"], B["outT"], B["o_sig"], B["wout"]
                        S.op("dve", lambda e: e.tensor_tensor(out=h_t[:], in0=h_t[:], in1=hf_t[:], op=ALU.add), reads=[h_t, hf_t], writes=[h_t])
                        for b in range(4):
                            S.op("act", lambda e, b=b: e.activation(out=dj[:, :].bitcast(BF16), in_=h_t[:, b, :], func=AF.Square, accum_out=hss[:, b:b + 1]), reads=[h_t], writes=[dj, hss])
                        S.op("act", lambda e: e.activation(out=hrs[:, :], in_=hss[:, :], func=AF.Ln, bias=EPS, scale=1.0 / HD), reads=[hss], writes=[hrs])
                        S.op("act", lambda e: e.activation(out=hrs[:, :], in_=hrs[:, :], func=AF.Exp, scale=-0.5), reads=[hrs], writes=[hrs])
                        yield
                        hg0 = OP["hg"] + h * HD
                        for b in range(4):
                            S.op("dve", lambda e, b=b: e.scalar_tensor_tensor(out=h_t[:, b, :], in0=h_t[:, b, :], scalar=hrs[:, b:b + 1], in1=P[:, hg0:hg0 + HD], op0=ALU.mult, op1=ALU.mult),
                                 reads=[h_t, hrs, P], writes=[h_t])
                        S.op("dve", lambda e: e.tensor_tensor(out=hs_b[:], in0=h_t[:], in1=o_sig[:], op=ALU.mult), reads=[h_t, o_sig], writes=[hs_b])
                        for b in range(4):
                            pt = self.nps(); ptb = pt.t.bitcast(BF16)
                            for dc in range(2):
                                S.op("pe", lambda e, ptb=ptb, b=b, dc=dc: e.transpose(ptb[:, dc * 128:(dc + 1) * 128], hs_b[:, b, dc * 128:(dc + 1) * 128], self.id_b[:, :]),
                                     reads=[hs_b, self.id_b], writes=[pt])
                            S.op("act", lambda e, ptb=ptb, b=b: e.copy(out=outT[:, :, b * 128:(b + 1) * 128], in_=ptb[:, 0:256].rearrange("p (k t) -> p k t", k=2)), reads=[pt], writes=[outT])
                        yield
                        for b in range(4):
                            for hh in range(2):
                                pp = self.nps()
                                for dc in range(2):
                                    S.op("pe", lambda e, pp=pp, b=b, hh=hh, dc=dc: e.matmul(pp[:, :], outT[:, dc, b * 128:(b + 1) * 128], wout[:, dc, hh * 512:(hh + 1) * 512],
                                                                                           start=(dc == 0), stop=(dc == 1)), reads=[outT, wout], writes=[pp])
                                S.op("dve", lambda e, pp=pp, b=b, hh=hh: e.tensor_tensor(out=x_t[:, b, hh * 512:(hh + 1) * 512], in0=x_t[:, b, hh * 512:(hh + 1) * 512], in1=pp[:, :], op=ALU.add),
                                     reads=[x_t, pp], writes=[x_t])
                            yield

                tiles = range(NT - 1, -1, -1) if bwd else range(NT)
                for i in tiles:
                    t0 = i * 512
                    S.dma("sp", x_t[:], x_in.ap()[t0:t0 + 512, :].rearrange("(b p) d -> p b d", p=128), reads=[d_in, d_xo], writes=[x_t])
                    if bwd:
                        for h in pair:
                            S.dma("sp", HB[h]["hf_t"][:], hf_ds[h].ap()[t0:t0 + 512, :].rearrange("(b p) d -> p b d", p=128), reads=[d_hf[h]], writes=[HB[h]["hf_t"]])
                    self._rms(x_t, 4, P, OP["gamma"], xn_b, ss, rstd, junk, 128)
                    self.transpose_blocks(xn_b, 4, xnT)
                    if bwd and pair[0] != 0:
                        S.dma("sp", x_t[:], x_out.ap()[t0:t0 + 512, :].rearrange("(b p) d -> p b d", p=128), reads=[d_xo, xn_b], writes=[x_t])
                    gens = [gen_head(h, i) for h in pair]
                    live = list(gens)
                    while live:
                        for g in list(live):
                            try:
                                next(g)
                            except StopIteration:
                                live.remove(g)
                    if bwd:
                        S.dma("sp", x_out.ap()[t0:t0 + 512, :].rearrange("(b p) d -> p b d", p=128), x_t[:], reads=[x_t], writes=[d_xo])
                S.end_phase()


Prog.odd_mixer = _odd_mixer2
```

```python
import numpy as np
import concourse.bass as bass
import concourse.mybir as mybir
from concourse.bass_utils import run_bass_kernel_spmd
from contextlib import ExitStack

ENGS = ("pe", "act", "dve", "pool", "sp")
SEM_WINDOW = 20000
DMA_SEM_MAX = 3900


class Buf:
    __slots__ = ("name", "t", "last_w", "reads", "dsem", "dcnt")

    def __init__(self, name, t=None):
        self.name = name
        self.t = t
        self.last_w = None
        self.reads = []
        self.dsem = None
        self.dcnt = 0

    def __getitem__(self, k):
        return self.t[k]


class Sched:
    def __init__(self, nc, stack, same_engine_sync=True):
        self.nc = nc
        self.stack = stack
        self.ops = {e: [] for e in ENGS}
        self.signal = {e: set() for e in ENGS}
        self.known = {e: {} for e in ENGS}
        self.same_engine_sync = same_engine_sync
        self.free_dma_sems = []
        self.all_dma = []
        self.nsem = 0
        self.dma_bufs = []
        self.bufs = []
        self.emitted = {e: 0 for e in ENGS}
        self.semval = {}
        self.semstate = {e: [None, 0] for e in ENGS}

    def buf(self, name, t=None):
        b = Buf(name, t)
        self.bufs.append(b)
        return b

    def end_phase(self):
        self.barrier()
        self.emit()
        for b in self.bufs:
            b.last_w = None
            b.reads = []
        for b in self.dma_bufs:
            if b.dsem is not None:
                self.free_dma_sems.append((b.dsem, b.dcnt))
                b.dsem = None
        self.dma_bufs = []
        self.bufs = [b for b in self.bufs if b.t is None or getattr(b, "keep", False)] if False else self.bufs

    def new_sem(self, name):
        self.nsem += 1
        return self.stack.enter_context(self.nc.semaphore(f"{name}_{self.nsem}"))

    def _dma_sem(self, buf):
        if buf.dsem is None or buf.dcnt >= DMA_SEM_MAX:
            got = False
            if buf.dsem is None:
                while self.free_dma_sems:
                    sem, cnt = self.free_dma_sems.pop()
                    if cnt < DMA_SEM_MAX - 200:
                        buf.dsem, buf.dcnt = sem, cnt
                        got = True
                        break
            if not got:
                buf.dsem, buf.dcnt = self.new_sem("d"), 0
            if buf not in self.dma_bufs:
                self.dma_bufs.append(buf)
        return buf.dsem

    def release(self, *bufs):
        for b in bufs:
            if b.dsem is not None:
                self.free_dma_sems.append((b.dsem, b.dcnt))
                b.dsem = None
            if b in self.dma_bufs:
                self.dma_bufs.remove(b)

    def _need(self, eng, ev, waits, raw=False):
        if ev is None:
            return
        if ev[0] == "c":
            _, e2, idx = ev
            if e2 == eng and not (raw and eng != "pe" and self.same_engine_sync):
                return
            k = ("c", e2)
            if self.known[eng].get(k, -1) >= idx:
                return
            self.known[eng][k] = idx
            self.signal[e2].add(idx)
            waits.append(ev)
        else:
            _, sem, val = ev
            k = ("d", id(sem))
            if self.known[eng].get(k, -1) >= val:
                return
            self.known[eng][k] = val
            waits.append(ev)

    def op(self, eng, fn, reads=(), writes=(), dma_key=None):
        waits = []
        for r in reads:
            self._need(eng, r.last_w, waits, raw=True)
        for w in writes:
            self._need(eng, w.last_w, waits)
            for ev in w.reads:
                self._need(eng, ev, waits)
        idx = len(self.ops[eng])
        if dma_key is not None:
            sem = self._dma_sem(dma_key)
            dma_key.dcnt += 1
            ev = ("d", sem, 16 * dma_key.dcnt)
        else:
            ev = ("c", eng, idx)
        self.ops[eng].append([waits, fn, ev])
        for r in reads:
            r.reads.append(ev)
        for w in writes:
            w.last_w = ev
            w.reads = []
        return ev

    def dma(self, eng, out_ap, in_ap, reads=(), writes=(), key=None, **kw):
        key = key if key is not None else (writes[0] if writes else reads[0])
        return self.op(eng, lambda e: e.dma_start(out=out_ap, in_=in_ap, **kw),
                       reads=reads, writes=writes, dma_key=key)

    def barrier(self):
        last = {e: len(self.ops[e]) - 1 for e in ENGS}
        for e in ENGS:
            waits = []
            for e2 in ENGS:
                if e2 != e and last[e2] >= self.emitted[e2] and self.ops[e2][last[e2]][2][0] == "c":
                    self._need(e, ("c", e2, last[e2]), waits)
                elif e2 != e and last[e2] >= self.emitted[e2]:
                    for j in range(last[e2], self.emitted[e2] - 1, -1):
                        if self.ops[e2][j][2][0] == "c":
                            self._need(e, ("c", e2, j), waits)
                            break
            for b in list(self.dma_bufs):
                if b.dsem is not None and b.dcnt > 0:
                    self._need(e, ("d", b.dsem, 16 * b.dcnt), waits)
            if waits:
                self.ops[e].append([waits, None, ("c", e, len(self.ops[e]))])

    def emit(self):
        nc = self.nc
        semval = self.semval
        for e in ENGS:
            sem, n = self.semstate[e]
            for idx in sorted(i for i in self.signal[e] if i >= self.emitted[e]):
                if sem is None or n >= SEM_WINDOW:
                    sem = self.new_sem("p" + e)
                    n = 0
                n += 1
                semval[(e, idx)] = (sem, n)
            self.semstate[e] = [sem, n]
        engobj = {"pe": "tensor", "act": "scalar", "dve": "vector", "pool": "gpsimd", "sp": "sync"}

        def run(ename, eng):
            start = self.emitted[ename]
            for idx in range(start, len(self.ops[ename])):
                waits, fn, ev = self.ops[ename][idx]
                for w in waits:
                    if w[0] == "c":
                        s, v = semval[(w[1], w[2])]
                        eng.wait_ge(s, v)
                    else:
                        eng.wait_ge(w[1], w[2])
                if fn is None:
                    if (ename, idx) in semval:
                        s, v = semval[(ename, idx)]
                        eng.sem_inc(s, 1)
                    continue
                ins = fn(eng)
                if ev[0] == "d":
                    ins.then_inc(ev[1], 16)
                elif (ename, idx) in semval:
                    ins.then_inc(semval[(ename, idx)][0], 1)

        with nc.Block() as block:
            @block.tensor
            def _(e):
                run("pe", e)

            @block.scalar
            def _(e):
                run("act", e)

            @block.vector
            def _(e):
                run("dve", e)

            @block.gpsimd
            def _(e):
                run("pool", e)

            @block.sync
            def _(e):
                run("sp", e)
        for e in ENGS:
            self.emitted[e] = len(self.ops[e])

F32 = mybir.dt.float32
BF16 = mybir.dt.bfloat16
I32 = mybir.dt.int32
AF = mybir.ActivationFunctionType
ALU = mybir.AluOpType
AX = mybir.AxisListType

D = 1024
NE = 16
MEM = 256
EPS = 1e-6
NCORES = 8


class Cfg:
    def __init__(self, seg=2048, ff=2048, cmax=None):
        self.SEG = seg
        self.NSEG = 6
        self.TOK = self.NSEG * seg
        self.NT = self.TOK // 512
        self.NB = self.TOK // 128
        self.FF = ff
        self.NF = ff // 128
        self.NBALL = NCORES * self.NB
        self.CMAX = cmax if cmax is not None else ((self.TOK // 8) * 3 // 2 + 255) // 256 * 256
        self.CAP_P = 2 * (32 * seg) // NE
        self.CAP_S = 2 * (4 * 4 * seg) // NE


class Prog:
    def __init__(self, cfg):
        self.cfg = cfg
        self.nc = bass.Bass("TRN2", target_bir_lowering=False)
        self.st = ExitStack()
        self.S = Sched(self.nc, self.st)
        self.psn = 0
        self.ps = [self.S.buf(f"ps{i}", self.st.enter_context(self.nc.psum_tensor(f"ps{i}", [128, 512], F32)))
                   for i in range(8)]
        self.ins = {}
        self.outs = {}
        self.cn = 0
        self.consts_done = False

    def din(self, name, shape, dt=F32):
        h = self.nc.dram_tensor(name, list(shape), dt, kind="ExternalInput")
        self.ins[name] = h
        return h

    def dout(self, name, shape, dt=F32):
        h = self.nc.dram_tensor(name, list(shape), dt, kind="ExternalOutput")
        self.outs[name] = h
        return h

    def dint(self, name, shape, dt=F32):
        return self.nc.dram_tensor(name, list(shape), dt, kind="Internal")

    def sb(self, stack, name, shape, dt=F32):
        self.cn += 1
        return self.S.buf(name, stack.enter_context(self.nc.sbuf_tensor(f"{name}_{self.cn}", list(shape), dt)))

    def dbg(self, name, buf, ap, shape, dt=F32):
        if not getattr(self, "debug", False) or name in self.outs:
            return
        h = self.dout("dbg_" + name, shape, dt)
        self.S.dma("sp", h.ap(), ap, reads=[buf], writes=[self.S.buf("dbg_" + name)])

    def nps(self):
        b = self.ps[self.psn % 7]
        self.psn += 1
        return b

    @property
    def psx(self):
        return self.ps[7]

    def breg2(self, e):
        if getattr(self, "_bc2", None) is None:
            self._bc2 = e.to_reg(self.cfg.TOK - 1)
        return self._bc2

    def breg(self, e):
        if getattr(self, "_bc", None) is None:
            self._bc = e.to_reg(self.cfg.CMAX - 1)
        return self._bc

    def consts(self):
        S, nc, st = self.S, self.nc, self.st
        self.ones_f = self.sb(st, "ones_f", [128, 128], F32)
        self.ones_b = self.sb(st, "ones_b", [128, 128], BF16)
        self.id_f = self.sb(st, "id_f", [128, 128], F32)
        self.id_b = self.sb(st, "id_b", [128, 128], BF16)
        self.tri_b = self.sb(st, "tri_b", [128, 128], BF16)
        self.tri_f = self.sb(st, "tri_f", [128, 128], F32)
        of, ob, idf, idb, trb, trf = self.ones_f, self.ones_b, self.id_f, self.id_b, self.tri_b, self.tri_f
        S.op("pool", lambda e: e.memset(of[:], 1.0), writes=[of])
        S.op("pool", lambda e: e.memset(ob[:], 1.0), writes=[ob])
        S.op("pool", lambda e: e.memset(idf[:], 0.0), writes=[idf])
        S.op("pool", lambda e: e.memset(trf[:], 0.0), writes=[trf])
        S.op("pool", lambda e: e.affine_select(out=idf[:], in_=of[:], pattern=[[-1, 128]], compare_op=ALU.is_equal,
                                               fill=0.0, base=0, channel_multiplier=1), reads=[of], writes=[idf])
        S.op("pool", lambda e: e.affine_select(out=trf[:], in_=of[:], pattern=[[1, 128]], compare_op=ALU.is_ge,
                                               fill=0.0, base=0, channel_multiplier=-1), reads=[of], writes=[trf])
        self.tril_f = self.sb(st, "tril_f", [128, 128], F32)
        self.ones512 = self.sb(st, "ones512", [128, 512], F32)
        tl, o5 = self.tril_f, self.ones512
        S.op("pool", lambda e: e.memset(tl[:], 0.0), writes=[tl])
        S.op("pool", lambda e: e.memset(o5[:], 1.0), writes=[o5])
        S.op("pool", lambda e: e.affine_select(out=tl[:], in_=of[:], pattern=[[-1, 128]], compare_op=ALU.is_ge,
                                               fill=0.0, base=0, channel_multiplier=1), reads=[of], writes=[tl])
        S.op("pool", lambda e: e.tensor_copy(out=idb[:], in_=idf[:]), reads=[idf], writes=[idb])
        S.op("pool", lambda e: e.tensor_copy(out=trb[:], in_=trf[:]), reads=[trf], writes=[trb])

    def load_w(self, wsb, wdram_ap, nk, ndma=4, dbuf=None):
        src = wdram_ap.rearrange("(k p) n -> p k n", p=128)
        step = max(1, nk // ndma)
        for k0 in range(0, nk, step):
            k1 = min(nk, k0 + step)
            self.S.dma("pool", wsb[:, k0:k1, :], src[:, k0:k1, :], reads=[dbuf] if dbuf else [], writes=[wsb])

    def transpose_blocks(self, xn_b, nb, xnT, rows=128, evac=("act", "dve")):
        S = self.S
        for b in range(nb):
            ps = self.nps()
            psb = ps.t.bitcast(BF16)
            for k in range(8):
                S.op("pe", lambda e, b=b, k=k, psb=psb: e.transpose(psb[:, k * rows:(k + 1) * rows],
                                                                   xn_b[0:rows, b, k * 128:(k + 1) * 128],
                                                                   self.id_b[0:rows, 0:rows]),
                     reads=[xn_b, self.id_b], writes=[ps])
            eng = evac[b % len(evac)]
            src = psb[:, 0:8 * rows].rearrange("p (k t) -> p k t", k=8)
            if eng == "act":
                S.op("act", lambda e, b=b, src=src: e.copy(out=xnT[:, :, b * rows:(b + 1) * rows], in_=src),
                     reads=[ps], writes=[xnT])
            else:
                S.op("dve", lambda e, b=b, src=src: e.tensor_copy(out=xnT[:, :, b * rows:(b + 1) * rows], in_=src),
                     reads=[ps], writes=[xnT])

    def proj_fm(self, ps, w, c0, xT, n=512, nk=8, cw=128):
        for k in range(nk):
            self.S.op("pe", lambda e, k=k: e.matmul(ps[0:cw, 0:n], w[:, k, c0:c0 + cw], xT[:, k, 0:n],
                                                    start=(k == 0), stop=(k == nk - 1)),
                      reads=[w, xT], writes=[ps])

    def proj_tm(self, ps, xT, t0, w, n0, n=512, nk=8):
        for k in range(nk):
            self.S.op("pe", lambda e, k=k: e.matmul(ps[:, 0:n], xT[:, k, t0:t0 + 128], w[:, k, n0:n0 + n],
                                                    start=(k == 0), stop=(k == nk - 1)),
                      reads=[w, xT], writes=[ps])

    def even_mixer(self, x_in, x_out, W, par, flags):
        cfg, S, nc = self.cfg, self.S, self.nc
        TOK, NT = cfg.TOK, cfg.NT
        with ExitStack() as ph:
            sb = lambda n, s, d=F32: self.sb(ph, n, s, d)
            w_in = sb("w_in", [128, 8, 2048], BF16)
            w_out = sb("w_out", [128, 8, D], BF16)
            wsT = sb("wsT", [128, 4, 128], BF16)
            P = sb("par", [128, EP_N])
            fl = sb("fl", [128, 2 * NT])
            self.load_w(w_in, W["w_in"].ap(), 8)
            self.load_w(w_out, W["w_out"].ap(), 8)
            S.dma("pool", wsT[:], W["wsT"].ap(), writes=[wsT])
            S.dma("sp", P[:], par.ap(), writes=[P])
            S.dma("sp", fl[:], flags.ap(), writes=[fl])
            x_ts = [sb(f"x_t{i}", [128, 4, D]) for i in range(2)]
            xhs = [sb(f"xh{i}", [32, 1, D]) for i in range(2)]
            xn_b = sb("xn_b", [128, 4, D], BF16)
            xnh_b = sb("xnh_b", [32, 1, D], BF16)
            xnTs = [sb(f"xnT{i}", [128, 8, 512], BF16) for i in range(2)]
            xnhT = sb("xnhT", [128, 8, 32], BF16)
            junk = sb("junk", [128, D], BF16)
            ss = sb("ss", [128, 8]); rstd = sb("rstd", [128, 8])
            ssh = sb("ssh", [32, 8]); rstdh = sb("rstdh", [32, 8])
            uT = sb("uT", [128, 4, 512])
            gv = sb("gv", [128, 512])
            bst = sb("bst", [128, 1, 6]); mv = sb("mv", [128, 2]); vr = sb("vr", [128, 1])
            v_b = sb("v_b", [128, 4, 512], BF16)
            sig = sb("sig", [128, 512])
            gh = sb("gh", [128, 4, 32]); sigh = sb("sigh", [128, 4, 32])
            gT = sb("gT", [128, 4, 544])
            cT = sb("cT", [128, 4, 512])
            cT3 = sb("cT3", [128, 512])
            ctmp = [sb(f"ctmp{j}", [128, 512]) for j in range(3)]
            csq = sb("csq", [128, 4, 512])
            mean = sb("mean", [128, 512]); var = sb("var", [128, 512]); msq = sb("msq", [128, 512])
            tmp4 = sb("tmp4", [128, 4, 128])
            actT = sb("actT", [128, 8, 512], BF16)
            d_in = S.buf("d_xin"); d_out = S.buf("d_xout")
            for i in range(NT):
                x_t = x_ts[i % 2]; xh = xhs[i % 2]; xnT = xnTs[i % 2]
                t0 = i * 512
                lo0 = max(t0 - 16, 0); hi0 = min(t0 + 512, TOK - 16)
                S.dma("sp", x_t[:], x_in.ap()[t0:t0 + 512, :].rearrange("(b p) d -> p b d", p=128), reads=[d_in], writes=[x_t])
                S.dma("sp", xh[0:16, 0, :], x_in.ap()[lo0:lo0 + 16, :], reads=[d_in], writes=[xh])
                S.dma("sp", xh[16:32, 0, :], x_in.ap()[hi0:hi0 + 16, :], reads=[d_in], writes=[xh])
                self._rms(x_t, 4, P, EP["gamma"], xn_b, ss, rstd, junk, 128)
                self._rms(xh, 1, P, EP["gamma"], xnh_b, ssh, rstdh, junk, 32)
                self.transpose_blocks(xn_b, 4, xnT)
                self.transpose_blocks(xnh_b, 1, xnhT, rows=32)
                if i == 1:
                    self.dbg("e_xn", xn_b, xn_b[:], [128, 4, D], BF16)
                    self.dbg("e_xnT", xnT, xnT[:], [128, 8, 512], BF16)
                    self.dbg("e_xnhT", xnhT, xnhT[:], [128, 8, 32], BF16)
                psh = self.psx
                for c in range(4):
                    pv = self.nps(); pg = self.nps()
                    self.proj_fm(pv, w_in, 1024 + c * 128, xnT)
                    self.proj_fm(pg, w_in, 1536 + c * 128, xnT)
                    S.op("act", lambda e, pg=pg: e.activation(out=sig[:, :], in_=pg[:, :], func=AF.Sigmoid), reads=[pg], writes=[sig])
                    S.op("dve", lambda e, pv=pv, c=c: e.tensor_tensor(out=gT[:, c, 16:528], in0=pv[:, :], in1=sig[:, :], op=ALU.mult),
                         reads=[pv, sig], writes=[gT])
                    for k in range(8):
                        S.op("pe", lambda e, k=k, c=c: e.matmul(psh[:, c * 64:c * 64 + 32], w_in[:, k, 1024 + c * 128:1024 + (c + 1) * 128],
                                                                xnhT[:, k, :], start=(k == 0), stop=(k == 7)), reads=[w_in, xnhT], writes=[psh])
                    for k in range(8):
                        S.op("pe", lambda e, k=k, c=c: e.matmul(psh[:, c * 64 + 32:c * 64 + 64], w_in[:, k, 1536 + c * 128:1536 + (c + 1) * 128],
                                                                xnhT[:, k, :], start=(k == 0), stop=(k == 7)), reads=[w_in, xnhT], writes=[psh])
                psh3 = psh.t[:, 0:256].rearrange("p (c t) -> p c t", c=4)
                S.op("act", lambda e, psh3=psh3: e.activation(out=sigh[:], in_=psh3[:, :, 32:64], func=AF.Sigmoid), reads=[psh], writes=[sigh])
                S.op("dve", lambda e, psh3=psh3: e.tensor_tensor(out=gh[:], in0=psh3[:, :, 0:32], in1=sigh[:], op=ALU.mult), reads=[psh, sigh], writes=[gh])
                S.op("dve", lambda e, i=i: e.tensor_scalar(out=gT[:, :, 0:16], in0=gh[:, :, 0:16], scalar1=fl[:, 2 * i:2 * i + 1], scalar2=None, op0=ALU.mult),
                     reads=[gh, fl], writes=[gT])
                S.op("dve", lambda e, i=i: e.tensor_scalar(out=gT[:, :, 528:544], in0=gh[:, :, 16:32], scalar1=fl[:, 2 * i + 1:2 * i + 2], scalar2=None, op0=ALU.mult),
                     reads=[gh, fl], writes=[gT])
                if i == 1:
                    self.dbg("e_gT", gT, gT[:], [128, 4, 544])
                cw0 = EP["cw"]; cb0 = EP["cb"]
                for c in range(3):
                    S.op("dve", lambda e, c=c: e.tensor_scalar(out=cT[:, c, :], in0=gT[:, c, 1:513], scalar1=P[:, cw0 + c * 31:cw0 + c * 31 + 1],
                                                               scalar2=P[:, cb0 + c:cb0 + c + 1], op0=ALU.mult, op1=ALU.add),
                         reads=[gT, P], writes=[cT])
                    for k in range(1, 31):
                        S.op("dve", lambda e, c=c, k=k: e.scalar_tensor_tensor(out=cT[:, c, :], in0=gT[:, c, k + 1:k + 513],
                                                                                scalar=P[:, cw0 + c * 31 + k:cw0 + c * 31 + k + 1],
                                                                                in1=cT[:, c, :], op0=ALU.mult, op1=ALU.add),
                             reads=[gT, P, cT], writes=[cT])
                c = 3
                S.op("pool", lambda e: e.tensor_scalar(out=cT3[:, :], in0=gT[:, 3, 1:513], scalar1=P[:, cw0 + 93:cw0 + 94],
                                                       scalar2=P[:, cb0 + 3:cb0 + 4], op0=ALU.mult, op1=ALU.add), reads=[gT, P], writes=[cT3])
                for k in range(1, 31):
                    tk = ctmp[k % 3]
                    S.op("act", lambda e, k=k, tk=tk: e.activation(out=tk[:, :], in_=gT[:, 3, k + 1:k + 513], func=AF.Copy,
                                                                   scale=P[:, cw0 + 93 + k:cw0 + 94 + k]), reads=[gT, P], writes=[tk])
                    S.op("pool", lambda e, tk=tk: e.tensor_tensor(out=cT3[:, :], in0=cT3[:, :], in1=tk[:, :], op=ALU.add), reads=[cT3, tk], writes=[cT3])
                S.op("pool", lambda e: e.tensor_copy(out=cT[:, 3, :], in_=cT3[:, :]), reads=[cT3], writes=[cT])
                if i == 1:
                    self.dbg("e_cT", cT, cT[:], [128, 4, 512])
                S.op("act", lambda e: e.activation(out=csq[:].rearrange("p c t -> p (c t)"), in_=cT[:].rearrange("p c t -> p (c t)"), func=AF.Square),
                     reads=[cT], writes=[csq])
                p1 = self.nps(); p2 = self.nps()
                for c in range(4):
                    S.op("pe", lambda e, c=c, p1=p1: e.matmul(p1[:, :], self.ones_f[:, :], cT[:, c, :], start=(c == 0), stop=(c == 3)),
                         reads=[self.ones_f, cT], writes=[p1])
                for c in range(4):
                    S.op("pe", lambda e, c=c, p2=p2: e.matmul(p2[:, :], self.ones_f[:, :], csq[:, c, :], start=(c == 0), stop=(c == 3)),
                         reads=[self.ones_f, csq], writes=[p2])
                S.op("act", lambda e, p1=p1: e.activation(out=mean[:, :], in_=p1[:, :], func=AF.Copy, scale=1.0 / 512), reads=[p1], writes=[mean])
                S.op("dve", lambda e: e.tensor_tensor(out=msq[:, :], in0=mean[:, :], in1=mean[:, :], op=ALU.mult), reads=[mean], writes=[msq])
                S.op("dve", lambda e, p2=p2: e.scalar_tensor_tensor(out=var[:, :], in0=p2[:, :], scalar=1.0 / 512, in1=msq[:, :], op0=ALU.mult, op1=ALU.subtract),
                     reads=[p2, msq], writes=[var])
                S.op("act", lambda e: e.activation(out=var[:, :], in_=var[:, :], func=AF.Ln, bias=EPS, scale=1.0), reads=[var], writes=[var])
                S.op("act", lambda e: e.activation(out=var[:, :], in_=var[:, :], func=AF.Exp, scale=-0.5), reads=[var], writes=[var])
                mb = mean.t[:, :].rearrange("p (o t) -> p o t", o=1).to_broadcast([128, 4, 512])
                vb = var.t[:, :].rearrange("p (o t) -> p o t", o=1).to_broadcast([128, 4, 512])
                S.op("pool", lambda e, mb=mb: e.tensor_tensor(out=cT[:], in0=cT[:], in1=mb, op=ALU.subtract), reads=[cT, mean], writes=[cT])
                S.op("dve", lambda e, vb=vb: e.tensor_tensor(out=cT[:], in0=cT[:], in1=vb, op=ALU.mult), reads=[cT, var], writes=[cT])
                for c in range(4):
                    S.op("act", lambda e, c=c: e.activation(out=actT[:, 4 + c, :], in_=cT[:, c, :], func=AF.Silu,
                                                            bias=P[:, EP["blb"] + c:EP["blb"] + c + 1], scale=P[:, EP["blg"] + c:EP["blg"] + c + 1]),
                         reads=[cT, P], writes=[actT])
                for c in range(4):
                    pu = self.nps()
                    self.proj_fm(pu, w_in, c * 128, xnT)
                    S.op("act", lambda e, pu=pu, c=c: e.activation(out=uT[:, c, :], in_=pu[:, :], func=AF.Gelu_apprx_tanh), reads=[pu], writes=[uT])
                for b in range(4):
                    pa = self.nps()
                    self.proj_tm(pa, xnT, b * 128, w_in, 512)
                    S.op("act", lambda e, pa=pa: e.activation(out=gv[:, :], in_=pa[:, :], func=AF.Gelu_apprx_tanh), reads=[pa], writes=[gv])
                    S.op("dve", lambda e: e.bn_stats(out=bst[:, 0, :], in_=gv[:, :]), reads=[gv], writes=[bst])
                    S.op("dve", lambda e: e.bn_aggr(out=mv[:, :], in_=bst[:, :, :]), reads=[bst], writes=[mv])
                    S.op("act", lambda e: e.activation(out=vr[:, :], in_=mv[:, 1:2], func=AF.Ln, bias=EPS, scale=1.0), reads=[mv], writes=[vr])
                    S.op("act", lambda e: e.activation(out=vr[:, :], in_=vr[:, :], func=AF.Exp, scale=-0.5), reads=[vr], writes=[vr])
                    S.op("dve", lambda e: e.tensor_scalar(out=gv[:, :], in0=gv[:, :], scalar1=mv[:, 0:1], scalar2=vr[:, 0:1], op0=ALU.subtract, op1=ALU.mult),
                         reads=[gv, mv, vr], writes=[gv])
                    S.op("dve", lambda e: e.tensor_tensor(out=gv[:, :], in0=gv[:, :], in1=P[:, EP["alg"]:EP["alg"] + 512], op=ALU.mult), reads=[gv, P], writes=[gv])
                    S.op("dve", lambda e, b=b: e.tensor_tensor(out=v_b[:, b, :], in0=gv[:, :], in1=P[:, EP["alb"]:EP["alb"] + 512], op=ALU.add), reads=[gv, P], writes=[v_b])
                    pm = self.nps()
                    for g in range(4):
                        S.op("pe", lambda e, g=g, b=b, pm=pm: e.matmul(pm[:, g * 128:(g + 1) * 128], v_b[:, b, g * 128:(g + 1) * 128], wsT[:, g, :], start=True, stop=True),
                             reads=[v_b, wsT], writes=[pm])
                    pm3 = pm.t[:, :].rearrange("p (g t) -> p g t", g=4)
                    bs3 = P.t[:, EP["bs"]:EP["bs"] + 512].rearrange("p (g t) -> p g t", g=4)
                    S.op("dve", lambda e, pm3=pm3, bs3=bs3: e.tensor_tensor(out=tmp4[:], in0=pm3, in1=bs3, op=ALU.add), reads=[pm, P], writes=[tmp4])
                    S.op("dve", lambda e, b=b: e.tensor_tensor(out=actT[:, 0:4, b * 128:(b + 1) * 128], in0=tmp4[:], in1=uT[:, :, b * 128:(b + 1) * 128], op=ALU.mult),
                         reads=[tmp4, uT], writes=[actT])
                if i == 1:
                    self.dbg("e_actT", actT, actT[:], [128, 8, 512], BF16)
                    self.dbg("e_uT", uT, uT[:], [128, 4, 512])
                    self.dbg("e_vb", v_b, v_b[:], [128, 4, 512], BF16)
                for b in range(4):
                    for h in range(2):
                        po = self.nps()
                        self.proj_tm(po, actT, b * 128, w_out, h * 512)
                        S.op("dve", lambda e, po=po, b=b, h=h, x_t=x_t: e.tensor_tensor(out=x_t[:, b, h * 512:(h + 1) * 512], in0=x_t[:, b, h * 512:(h + 1) * 512],
                                                                                        in1=po[:, :], op=ALU.add), reads=[x_t, po], writes=[x_t])
                S.dma("sp", x_out.ap()[t0:t0 + 512, :].rearrange("(b p) d -> p b d", p=128), x_t[:], reads=[x_t], writes=[d_out])
            S.end_phase()

    def _rms(self, x_t, nb, P, goff, out, ss, rstd, junk, rows):
        S = self.S
        S.op("pool", lambda e: e.memset(ss[0:rows, 0:nb], 0.0), writes=[ss])
        for b in range(nb):
            S.op("act", lambda e, b=b: e.activation(out=junk[0:rows, :], in_=x_t[0:rows, b, :], func=AF.Square,
                                                    accum_out=ss[0:rows, b:b + 1]), reads=[x_t, ss], writes=[junk, ss])
        S.op("act", lambda e: e.activation(out=rstd[0:rows, 0:nb], in_=ss[0:rows, 0:nb], func=AF.Ln, bias=EPS, scale=1.0 / D),
             reads=[ss], writes=[rstd])
        S.op("act", lambda e: e.activation(out=rstd[0:rows, 0:nb], in_=rstd[0:rows, 0:nb], func=AF.Exp, scale=-0.5),
             reads=[rstd], writes=[rstd])
        if out is None:
            return
        for b in range(nb):
            S.op("dve", lambda e, b=b: e.scalar_tensor_tensor(out=out[0:rows, b, :], in0=x_t[0:rows, b, :], scalar=rstd[0:rows, b:b + 1],
                                                              in1=P[0:rows, goff:goff + D], op0=ALU.mult, op1=ALU.mult),
                 reads=[x_t, rstd, P], writes=[out])


def _mk_layout(items):
    off, d = 0, {}
    for k, w in items:
        d[k] = off
        off += w
    return d, off


EP, EP_N = _mk_layout([("gamma", D), ("alg", 512), ("alb", 512), ("bs", 512), ("cw", 124), ("cb", 4), ("blg", 4), ("blb", 4)])

XP, XP_N = _mk_layout([("gx", D), ("gf", D)])


def _xattn_router(self, x_in, x_out, aff_out, W, par, mem):
    cfg, S, nc = self.cfg, self.S, self.nc
    TOK, NT, NSEG, SEG = cfg.TOK, cfg.NT, cfg.NSEG, cfg.SEG
    TPS = SEG // 512
    with ExitStack() as ph:
        sb = lambda n, s, d=F32: self.sb(ph, n, s, d)
        wq = sb("wq", [128, 8, D], BF16)
        wkv = sb("wkv", [128, 8, 2 * D], BF16)
        wo = sb("wo", [128, 8, D], BF16)
        wr = sb("wr", [128, 8, NE])
        P = sb("xpar", [128, XP_N])
        self.load_w(wq, W["wq"].ap(), 8)
        self.load_w(wkv, W["wkv"].ap(), 8)
        self.load_w(wo, W["wo"].ap(), 8)
        S.dma("sp", wr[:], W["wr"].ap().rearrange("(k p) n -> p k n", p=128), writes=[wr])
        S.dma("sp", P[:], par.ap(), writes=[P])
        mem_b = sb("mem_b", [128, 2, D], BF16)
        memT = sb("memT", [128, 8, MEM], BF16)
        KT = sb("KT", [128, 8, MEM], BF16)
        V_b = sb("V_b", [128, 2, D], BF16)
        x_ts = [sb(f"xa_t{i}", [128, 4, D]) for i in range(1)]
        xn_b = sb("xa_nb", [128, 4, D], BF16)
        xnT = sb("xa_nT", [128, 8, 512], BF16)
        qT = sb("qT", [128, 8, 512], BF16)
        ET = sb("ET", [128, 2, 512], BF16)
        rec = sb("rec", [128, 512])
        oT = sb("oT", [128, 8, 512], BF16)
        junk = sb("xa_junk", [128, D], BF16)
        ss = sb("xa_ss", [128, 8]); rstd = sb("xa_rstd", [128, 8])
        xn2 = sb("xn2", [128, 1, D])
        xn2T = sb("xn2T", [128, 8, 128])
        mx = sb("mx", [128, 1]); sm = sb("sm", [128, 1]); ex = sb("ex", [128, NE])
        aff = sb("aff", [128, cfg.NB, NE])
        d_in = S.buf("d_xin"); d_out = S.buf("d_xout"); d_mem = S.buf("d_mem")
        for i in range(NT):
            seg = i // TPS
            t0 = i * 512
            x_t = x_ts[0]
            S.dma("sp", x_t[:], x_in.ap()[t0:t0 + 512, :].rearrange("(b p) d -> p b d", p=128), reads=[d_in], writes=[x_t])
            if i % TPS == 0:
                S.dma("pool", mem_b[:], mem.ap()[seg].rearrange("(b p) d -> p b d", p=128), reads=[d_mem], writes=[mem_b])
                self.transpose_blocks(mem_b, 2, memT)
                for n in range(8):
                    pk = self.nps()
                    self.proj_fm(pk, wkv, n * 128, memT, n=MEM)
                    if n % 2 == 0:
                        S.op("act", lambda e, pk=pk, n=n: e.copy(out=KT[:, n, :], in_=pk[:, 0:MEM]), reads=[pk], writes=[KT])
                    else:
                        S.op("dve", lambda e, pk=pk, n=n: e.tensor_copy(out=KT[:, n, :], in_=pk[:, 0:MEM]), reads=[pk], writes=[KT])
                for mb in range(2):
                    for h in range(2):
                        pv = self.nps()
                        self.proj_tm(pv, memT, mb * 128, wkv, D + h * 512)
                        if h == 0:
                            S.op("act", lambda e, pv=pv, mb=mb, h=h: e.copy(out=V_b[:, mb, h * 512:(h + 1) * 512], in_=pv[:, :]), reads=[pv], writes=[V_b])
                        else:
                            S.op("dve", lambda e, pv=pv, mb=mb, h=h: e.tensor_copy(out=V_b[:, mb, h * 512:(h + 1) * 512], in_=pv[:, :]), reads=[pv], writes=[V_b])
            self._rms(x_t, 4, P, XP["gx"], xn_b, ss, rstd, junk, 128)
            self.transpose_blocks(xn_b, 4, xnT)
            for n in range(8):
                pq = self.nps()
                self.proj_fm(pq, wq, n * 128, xnT)
                if n % 2 == 0:
                    S.op("act", lambda e, pq=pq, n=n: e.copy(out=qT[:, n, :], in_=pq[:, :]), reads=[pq], writes=[qT])
                else:
                    S.op("dve", lambda e, pq=pq, n=n: e.tensor_copy(out=qT[:, n, :], in_=pq[:, :]), reads=[pq], writes=[qT])
            for h in range(4):
                for mc in range(2):
                    psc = self.nps()
                    for dc in range(2):
                        S.op("pe", lambda e, psc=psc, h=h, mc=mc, dc=dc: e.matmul(psc[:, :], KT[:, 2 * h + dc, mc * 128:(mc + 1) * 128], qT[:, 2 * h + dc, :],
                                                                                  start=(dc == 0), stop=(dc == 1)), reads=[KT, qT], writes=[psc])
                    S.op("act", lambda e, psc=psc, mc=mc: e.activation(out=ET[:, mc, :], in_=psc[:, :], func=AF.Exp, scale=1.0 / 16.0), reads=[psc], writes=[ET])
                pd = self.nps()
                for mc in range(2):
                    S.op("pe", lambda e, pd=pd, mc=mc: e.matmul(pd[:, :], self.ones_b[:, :], ET[:, mc, :], start=(mc == 0), stop=(mc == 1)),
                         reads=[self.ones_b, ET], writes=[pd])
                S.op("dve", lambda e, pd=pd: e.reciprocal(out=rec[:, :], in_=pd[:, :]), reads=[pd], writes=[rec])
                for dc in range(2):
                    po = self.nps()
                    for mc in range(2):
                        S.op("pe", lambda e, po=po, h=h, mc=mc, dc=dc: e.matmul(po[:, :], V_b[:, mc, h * 256 + dc * 128:h * 256 + (dc + 1) * 128], ET[:, mc, :],
                                                                                start=(mc == 0), stop=(mc == 1)), reads=[V_b, ET], writes=[po])
                    S.op("dve", lambda e, po=po, h=h, dc=dc: e.tensor_tensor(out=oT[:, 2 * h + dc, :], in0=po[:, :], in1=rec[:, :], op=ALU.mult),
                         reads=[po, rec], writes=[oT])
            if i == 0:
                self.dbg("x_qT", qT, qT[:], [128, 8, 512], BF16)
                self.dbg("x_KT", KT, KT[:], [128, 8, MEM], BF16)
                self.dbg("x_V", V_b, V_b[:], [128, 2, D], BF16)
                self.dbg("x_oT", oT, oT[:], [128, 8, 512], BF16)
            for b in range(4):
                for hh in range(2):
                    pp = self.nps()
                    self.proj_tm(pp, oT, b * 128, wo, hh * 512)
                    S.op("dve", lambda e, pp=pp, b=b, hh=hh, x_t=x_t: e.tensor_tensor(out=x_t[:, b, hh * 512:(hh + 1) * 512], in0=x_t[:, b, hh * 512:(hh + 1) * 512],
                                                                                      in1=pp[:, :], op=ALU.add), reads=[x_t, pp], writes=[x_t])
            S.dma("sp", x_out.ap()[t0:t0 + 512, :].rearrange("(b p) d -> p b d", p=128), x_t[:], reads=[x_t], writes=[d_out])
            self._rms(x_t, 4, P, XP["gf"], None, ss, rstd, junk, 128)
            for b in range(4):
                S.op("dve", lambda e, b=b, x_t=x_t: e.scalar_tensor_tensor(out=xn2[:, 0, :], in0=x_t[:, b, :], scalar=rstd[:, b:b + 1],
                                                                          in1=P[:, XP["gf"]:XP["gf"] + D], op0=ALU.mult, op1=ALU.mult),
                     reads=[x_t, rstd, P], writes=[xn2])
                pa, pb = self.nps(), self.nps()
                for k in range(8):
                    pt = pa if k < 4 else pb
                    S.op("pe", lambda e, pt=pt, k=k, b=b: e.transpose(pt[:, (k % 4) * 128:(k % 4 + 1) * 128], xn2[:, 0, k * 128:(k + 1) * 128], self.id_f[:, :]),
                         reads=[xn2, self.id_f], writes=[pt])
                S.op("act", lambda e, pa=pa: e.copy(out=xn2T[:, 0:4, :], in_=pa[:, :].rearrange("p (k t) -> p k t", k=4)), reads=[pa], writes=[xn2T])
                S.op("dve", lambda e, pb=pb: e.tensor_copy(out=xn2T[:, 4:8, :], in_=pb[:, :].rearrange("p (k t) -> p k t", k=4)), reads=[pb], writes=[xn2T])
                if i == 0 and b == 0:
                    self.dbg("x_xn2T", xn2T, xn2T[:], [128, 8, 128])
                pr = self.nps()
                for k in range(8):
                    S.op("pe", lambda e, pr=pr, k=k: e.matmul(pr[:, 0:NE], xn2T[:, k, :], wr[:, k, :], start=(k == 0), stop=(k == 7)), reads=[xn2T, wr], writes=[pr])
                S.op("dve", lambda e, pr=pr: e.tensor_reduce(out=mx[:, :], in_=pr[:, 0:NE], axis=AX.X, op=ALU.max), reads=[pr], writes=[mx])
                S.op("dve", lambda e: e.tensor_scalar(out=mx[:, :], in0=mx[:, :], scalar1=-1.0, scalar2=None, op0=ALU.mult), reads=[mx], writes=[mx])
                S.op("pool", lambda e: e.memset(sm[:, :], 0.0), writes=[sm])
                S.op("act", lambda e, pr=pr: e.activation(out=ex[:, :], in_=pr[:, 0:NE], func=AF.Exp, bias=mx[:, 0:1], scale=1.0, accum_out=sm[:, 0:1]),
                     reads=[pr, mx, sm], writes=[ex, sm])
                S.op("dve", lambda e: e.reciprocal(out=sm[:, :], in_=sm[:, :]), reads=[sm], writes=[sm])
                S.op("dve", lambda e, i=i, b=b: e.tensor_scalar(out=aff[:, i * 4 + b, :], in0=ex[:, :], scalar1=sm[:, 0:1], scalar2=None, op0=ALU.mult),
                     reads=[ex, sm], writes=[aff])
        d_aff = S.buf("d_aff")
        S.dma("sp", aff_out.ap(), aff[:].rearrange("p b e -> p (b e)"), reads=[aff], writes=[d_aff])
        S.end_phase()


Prog.xattn_router = _xattn_router


def _final_norm(self, x_in, y_out, par):
    cfg, S = self.cfg, self.S
    with ExitStack() as ph:
        sb = lambda n, s, d=F32: self.sb(ph, n, s, d)
        P = sb("fpar", [128, D])
        S.dma("sp", P[:], par.ap(), writes=[P])
        x_ts = [sb(f"f_t{i}", [128, 4, D]) for i in range(2)]
        y_ts = [sb(f"f_y{i}", [128, 4, D]) for i in range(2)]
        junk = sb("f_junk", [128, D], BF16)
        ss = sb("f_ss", [128, 8]); rstd = sb("f_rstd", [128, 8])
        d_in = S.buf("d_xin"); d_out = S.buf("d_yout")
        for i in range(cfg.NT):
            t0 = i * 512
            x_t, y_t = x_ts[i % 2], y_ts[i % 2]
            S.dma("sp", x_t[:], x_in.ap()[t0:t0 + 512, :].rearrange("(b p) d -> p b d", p=128), reads=[d_in], writes=[x_t])
            self._rms(x_t, 4, P, 0, y_t, ss, rstd, junk, 128)
            S.dma("sp", y_out.ap()[t0:t0 + 512, :].rearrange("(b p) d -> p b d", p=128), y_t[:], reads=[y_t], writes=[d_out])
        S.end_phase()


Prog.final_norm = _final_norm


def core_segments():
    out = []
    for c in range(NCORES):
        if c < 4:
            out.append([("s", c, j) for j in range(4)] + [("p", 2 * c, 0), ("p", 2 * c + 1, 0)])
        else:
            out.append([("p", 8 + 6 * (c - 4) + j, 0) for j in range(6)])
    return out


def shard_tokens(cfg, xp, xs):
    SEG = cfg.SEG
    res = []
    for segs in core_segments():
        parts = []
        for g, q, j in segs:
            parts.append(xp[q] if g == "p" else xs[q, j * SEG:(j + 1) * SEG])
        res.append(np.ascontiguousarray(np.concatenate(parts, 0)))
    return res


def unshard_tokens(cfg, per_core, n_p=32, n_s=4):
    SEG = cfg.SEG
    yp = np.zeros((n_p, SEG, D), np.float32)
    ys = np.zeros((n_s, 4 * SEG, D), np.float32)
    for c, segs in enumerate(core_segments()):
        for si, (g, q, j) in enumerate(segs):
            blk = per_core[c][si * SEG:(si + 1) * SEG]
            if g == "p":
                yp[q] = blk
            else:
                ys[q, j * SEG:(j + 1) * SEG] = blk
    return yp, ys


def shard_mem(cfg, mp, ms):
    res = []
    for segs in core_segments():
        res.append(np.ascontiguousarray(np.stack([mp[q] if g == "p" else ms[q] for g, q, j in segs], 0)))
    return res


def tile_flags(cfg):
    SEG, NT, NSEG = cfg.SEG, cfg.NT, cfg.NSEG
    res = []
    for segs in core_segments():
        cont = [False] + [segs[s][0] == segs[s - 1][0] and segs[s][1] == segs[s - 1][1] and segs[s][2] == segs[s - 1][2] + 1
                          for s in range(1, NSEG)] + [False]
        f = np.zeros((2 * NT,), np.float32)
        for i in range(NT):
            t0 = i * 512
            s = t0 // SEG
            f[2 * i] = 1.0 if (t0 % SEG != 0 or cont[s]) else 0.0
            e = t0 + 512
            f[2 * i + 1] = 1.0 if (e % SEG != 0 or cont[s + 1]) else 0.0
        res.append(np.ascontiguousarray(np.broadcast_to(f[None, :], (128, 2 * NT))))
    return res


def bc(v, n=128):
    v = np.asarray(v, np.float32).reshape(1, -1)
    return np.broadcast_to(v, (n, v.shape[1]))


def pack_even_params(norm_mix_l, a_ln_g, a_ln_b, a_bs, conv_w, conv_b, b_ln_g, b_ln_b):
    P = np.zeros((128, EP_N), np.float32)
    P[:, EP["gamma"]:EP["gamma"] + D] = bc(norm_mix_l)
    P[:, EP["alg"]:EP["alg"] + 512] = bc(a_ln_g)
    P[:, EP["alb"]:EP["alb"] + 512] = bc(a_ln_b)
    P[:, EP["bs"]:EP["bs"] + 512] = bc(a_bs.reshape(-1))
    P[:, EP["cw"]:EP["cw"] + 124] = conv_w.T.reshape(4, 128, 31).transpose(1, 0, 2).reshape(128, 124)
    P[:, EP["cb"]:EP["cb"] + 4] = conv_b.reshape(4, 128).T
    P[:, EP["blg"]:EP["blg"] + 4] = b_ln_g.reshape(4, 128).T
    P[:, EP["blb"]:EP["blb"] + 4] = b_ln_b.reshape(4, 128).T
    return P


def pack_xattn_params(norm_x_l, norm_f_l):
    P = np.zeros((128, XP_N), np.float32)
    P[:, XP["gx"]:XP["gx"] + D] = bc(norm_x_l)
    P[:, XP["gf"]:XP["gf"] + D] = bc(norm_f_l)
    return P


def _moe(self, x_in, x_out, aff_all, aff_loc, gm_loc, W, par, cnt_out):
    cfg, S, nc = self.cfg, self.S, self.nc
    TOK, NT, NB, SEG, FF, NF, CMAX = cfg.TOK, cfg.NT, cfg.NB, cfg.SEG, cfg.FF, cfg.NF, cfg.CMAX
    NBS_C = 4 * SEG // 128
    NFS = 4 * NBS_C
    NFP = NCORES * NB - NFS
    xes = [self.dint(f"xe{e}_{self.cn}", [CMAX, D], BF16) for e in range(NE)]
    d_xe = [S.buf(f"d_xe{e}") for e in range(NE)]
    with ExitStack() as mo:
        posI = self.sb(mo, "posI", [128, NB, NE], I32)
        gate = self.sb(mo, "gate", [128, NB, NE])
        thr = self.sb(mo, "thr", [128, 2 * NE])
        with ExitStack() as ph:
            sb = lambda n, s, d=F32: self.sb(ph, n, s, d)
            A_P = sb("A_P", [128, NFP, NE]); A_S = sb("A_S", [128, NFS, NE])
            cmp_ = sb("cmp", [128, NFP, NE], BF16)
            lo = sb("lo", [128, 2 * NE]); hi = sb("hi", [128, 2 * NE]); mid = sb("mid", [128, 2 * NE])
            cnt = sb("cnt", [128, 2 * NE]); cond = sb("cond", [128, 2 * NE]); t1 = sb("t1", [128, 2 * NE])
            capm = sb("capm", [128, 2 * NE])
            src = aff_all.ap().rearrange("p (f e) -> p f e", e=NE)
            d_aff = S.buf("d_affall")
            po, so = 0, 0
            for c in range(NCORES):
                f0 = c * NB
                if c < 4:
                    S.dma("sp", A_S[:, so:so + NBS_C, :], src[:, f0:f0 + NBS_C, :], reads=[d_aff], writes=[A_S]); so += NBS_C
                    n = NB - NBS_C
                    S.dma("sp", A_P[:, po:po + n, :], src[:, f0 + NBS_C:f0 + NB, :], reads=[d_aff], writes=[A_P]); po += n
                else:
                    S.dma("sp", A_P[:, po:po + NB, :], src[:, f0:f0 + NB, :], reads=[d_aff], writes=[A_P]); po += NB
            S.op("pool", lambda e: e.memset(lo[:, :], 0.0), writes=[lo])
            S.op("pool", lambda e: e.memset(hi[:, :], 1.0), writes=[hi])
            S.op("pool", lambda e: e.memset(mid[:, :], 0.5), writes=[mid])
            S.op("pool", lambda e: e.memset(capm[:, 0:NE], cfg.CAP_P + 0.5), writes=[capm])
            S.op("pool", lambda e: e.memset(capm[:, NE:2 * NE], cfg.CAP_S + 0.5), writes=[capm])
            for it in range(32):
                for (A, nf, c0) in ((A_P, NFP, 0), (A_S, NFS, NE)):
                    mb = mid.t[:, c0:c0 + NE].rearrange("p (o e) -> p o e", o=1).to_broadcast([128, nf, NE])
                    S.op("dve", lambda e, A=A, nf=nf, mb=mb: e.tensor_tensor(out=cmp_[:, 0:nf, :], in0=A[:, :, :], in1=mb, op=ALU.is_gt),
                         reads=[A, mid], writes=[cmp_])
                    S.op("dve", lambda e, nf=nf, c0=c0: e.tensor_reduce(out=cnt[:, c0:c0 + NE], in_=cmp_[:, 0:nf, :].rearrange("p f e -> p e f"), axis=AX.X, op=ALU.add),
                         reads=[cmp_], writes=[cnt])
                pt = self.nps()
                S.op("pe", lambda e, pt=pt: e.matmul(pt[:, 0:2 * NE], self.ones_f[:, :], cnt[:, :], start=True, stop=True), reads=[self.ones_f, cnt], writes=[pt])
                S.op("dve", lambda e, pt=pt: e.tensor_tensor(out=cond[:, :], in0=pt[:, 0:2 * NE], in1=capm[:, :], op=ALU.is_gt), reads=[pt, capm], writes=[cond])
                S.op("dve", lambda e: e.tensor_tensor(out=t1[:, :], in0=cond[:, :], in1=mid[:, :], op=ALU.mult), reads=[cond, mid], writes=[t1])
                S.op("dve", lambda e: e.tensor_tensor(out=lo[:, :], in0=lo[:, :], in1=t1[:, :], op=ALU.max), reads=[lo, t1], writes=[lo])
                S.op("dve", lambda e: e.scalar_tensor_tensor(out=t1[:, :], in0=cond[:, :], scalar=4.0, in1=mid[:, :], op0=ALU.mult, op1=ALU.add), reads=[cond, mid], writes=[t1])
                S.op("dve", lambda e: e.tensor_tensor(out=hi[:, :], in0=hi[:, :], in1=t1[:, :], op=ALU.min), reads=[hi, t1], writes=[hi])
                S.op("dve", lambda e: e.tensor_tensor(out=t1[:, :], in0=lo[:, :], in1=hi[:, :], op=ALU.add), reads=[lo, hi], writes=[t1])
                S.op("dve", lambda e: e.tensor_scalar(out=mid[:, :], in0=t1[:, :], scalar1=0.5, scalar2=None, op0=ALU.mult), reads=[t1], writes=[mid])
            S.op("dve", lambda e: e.tensor_copy(out=thr[:, :], in_=hi[:, :]), reads=[hi], writes=[thr])
            S.end_phase()
        with ExitStack() as ph:
            sb = lambda n, s, d=F32: self.sb(ph, n, s, d)
            NC_ = NB * NE
            AL = sb("AL", [128, NB, NE]); gm = sb("gm", [128, NB])
            tt = sb("tt", [128, NB, NE]); sel = sb("sel", [128, NB, NE]); sel_b = sb("sel_b", [128, NB, NE], BF16)
            incl = sb("incl", [128, NC_]); ca = sb("ca", [128, NC_]); cb = sb("cb", [128, NC_]); cn0 = sb("cn0", [128, NC_])
            dthr = sb("dthr", [128, NE])
            d_al = S.buf("d_al")
            S.dma("sp", AL[:], aff_loc.ap().rearrange("p (f e) -> p f e", e=NE), reads=[d_al], writes=[AL])
            S.dma("sp", gm[:], gm_loc.ap(), reads=[d_al], writes=[gm])
            S.op("dve", lambda e: e.tensor_tensor(out=dthr[:, :], in0=thr[:, 0:NE], in1=thr[:, NE:2 * NE], op=ALU.subtract), reads=[thr], writes=[dthr])
            gmb = gm.t[:, :].rearrange("p (f o) -> p f o", o=1).to_broadcast([128, NB, NE])
            dtb = dthr.t[:, :].rearrange("p (o e) -> p o e", o=1).to_broadcast([128, NB, NE])
            tsb = thr.t[:, NE:2 * NE].rearrange("p (o e) -> p o e", o=1).to_broadcast([128, NB, NE])
            S.op("dve", lambda e: e.tensor_tensor(out=tt[:], in0=gmb, in1=dtb, op=ALU.mult), reads=[gm, dthr], writes=[tt])
            S.op("dve", lambda e: e.tensor_tensor(out=tt[:], in0=tt[:], in1=tsb, op=ALU.add), reads=[tt, thr], writes=[tt])
            S.op("dve", lambda e: e.tensor_tensor(out=sel[:], in0=AL[:], in1=tt[:], op=ALU.is_gt), reads=[AL, tt], writes=[sel])
            S.op("dve", lambda e: e.tensor_copy(out=sel_b[:], in_=sel[:]), reads=[sel], writes=[sel_b])
            S.op("dve", lambda e: e.tensor_tensor(out=gate[:], in0=AL[:], in1=sel[:], op=ALU.mult), reads=[AL, sel], writes=[gate])
            selb2 = sel_b.t[:].rearrange("p f e -> p (f e)")
            for c0 in range(0, NC_, 512):
                n = min(512, NC_ - c0)
                pi = self.nps(); pc = self.nps()
                S.op("pe", lambda e, pi=pi, c0=c0, n=n: e.matmul(pi[:, 0:n], self.tri_b[:, :], selb2[:, c0:c0 + n], start=True, stop=True), reads=[self.tri_b, sel_b], writes=[pi])
                S.op("pe", lambda e, pc=pc, c0=c0, n=n: e.matmul(pc[:, 0:n], self.ones_b[:, :], selb2[:, c0:c0 + n], start=True, stop=True), reads=[self.ones_b, sel_b], writes=[pc])
                S.op("act", lambda e, pi=pi, c0=c0, n=n: e.copy(out=incl[:, c0:c0 + n], in_=pi[:, 0:n]), reads=[pi], writes=[incl])
                S.op("dve", lambda e, pc=pc, c0=c0, n=n: e.tensor_copy(out=cn0[:, c0:c0 + n], in_=pc[:, 0:n]), reads=[pc], writes=[cn0])
            cur = cn0
            sft = 1
            k = 0
            while sft < NB:
                nxt = ca if k % 2 == 0 else cb
                w = sft * NE
                S.op("dve", lambda e, cur=cur, nxt=nxt, w=w: e.tensor_copy(out=nxt[:, 0:w], in_=cur[:, 0:w]), reads=[cur], writes=[nxt])
                S.op("dve", lambda e, cur=cur, nxt=nxt, w=w: e.tensor_tensor(out=nxt[:, w:NC_], in0=cur[:, w:NC_], in1=cur[:, 0:NC_ - w], op=ALU.add), reads=[cur], writes=[nxt])
                cur = nxt
                sft *= 2
                k += 1
            d_cnt = S.buf("d_cnt")
            S.dma("sp", cnt_out.ap(), cur[:, NC_ - NE:NC_], reads=[cur], writes=[d_cnt])
            S.op("dve", lambda e, cur=cur: e.tensor_tensor(out=incl[:, :], in0=incl[:, :], in1=cur[:, :], op=ALU.add), reads=[incl, cur], writes=[incl])
            S.op("dve", lambda e: e.tensor_tensor(out=incl[:, :], in0=incl[:, :], in1=cn0[:, :], op=ALU.subtract), reads=[incl, cn0], writes=[incl])
            sel2 = sel.t[:].rearrange("p f e -> p (f e)")
            S.op("dve", lambda e: e.scalar_tensor_tensor(out=incl[:, :], in0=sel2, scalar=-1.0e6, in1=incl[:, :], op0=ALU.mult, op1=ALU.add), reads=[sel, incl], writes=[incl])
            S.op("dve", lambda e: e.tensor_scalar(out=incl[:, :], in0=incl[:, :], scalar1=1.0e6 - 1.0, scalar2=None, op0=ALU.add), reads=[incl], writes=[incl])
            S.op("dve", lambda e: e.tensor_copy(out=posI[:].rearrange("p f e -> p (f e)"), in_=incl[:, :]), reads=[incl], writes=[posI])
            S.end_phase()
        xn_d = self.dint(f"xn_d_{self.cn}", [TOK, D], BF16)
        meta_ds = [self.dint(f"meta{e}_{self.cn}", [CMAX, 2], F32) for e in range(NE)]
        d_xn = S.buf("d_xn"); d_xacc = S.buf("d_xacc"); d_meta = [S.buf(f"d_meta{e}") for e in range(NE)]
        with ExitStack() as ph:
            sb = lambda n, s, d=F32: self.sb(ph, n, s, d)
            P = sb("mpar", [128, D])
            S.dma("sp", P[:], par.ap(), writes=[P])
            x_ts = [sb(f"m_xt{i}", [128, 4, D]) for i in range(2)]
            xn_bs = [sb(f"m_xn{i}", [128, 4, D], BF16) for i in range(2)]
            junk = sb("m_junk", [128, D], BF16); ss = sb("m_ss", [128, 8]); rstd = sb("m_rstd", [128, 8])
            onesm = sb("m_ones", [128, 2 * (CMAX // 128)])
            d_in = S.buf("d_xin")
            S.op("pool", lambda e: e.memset(onesm[:, :], 1.0), writes=[onesm])
            for ex in range(NE):
                S.dma("sp", meta_ds[ex].ap().rearrange("(p r) c -> p (r c)", p=128), onesm[:, :], reads=[onesm], writes=[d_meta[ex]])
            for i in range(NT):
                t0 = i * 512
                x_t, xn_b = x_ts[i % 2], xn_bs[i % 2]
                S.dma("sp", x_t[:], x_in.ap()[t0:t0 + 512, :].rearrange("(b p) d -> p b d", p=128), reads=[d_in], writes=[x_t])
                S.dma("act", x_out.ap()[t0:t0 + 512, :].rearrange("(b p) d -> p b d", p=128), x_t[:], reads=[x_t], writes=[d_xacc])
                self._rms(x_t, 4, P, 0, xn_b, ss, rstd, junk, 128)
                S.dma("sp", xn_d.ap()[t0:t0 + 512, :].rearrange("(b p) d -> p b d", p=128), xn_b[:], reads=[xn_b], writes=[d_xn])
            S.end_phase()
        with ExitStack() as ph:
            sb = lambda n, s, d=F32: self.sb(ph, n, s, d)
            w1 = sb("w1", [128, 8, FF], BF16); w3 = sb("w3", [128, 8, FF], BF16); w2 = sb("w2", [128, NF, D], BF16)
            xe_t = sb("xe_t", [128, 4, D], BF16)
            xeT = sb("xeT", [128, 8, 512], BF16)
            hidT = sb("hidT", [128, NF, 512], BF16)
            s1s = [sb(f"s1_{i}", [128, 512]) for i in range(2)]
            y_ts = [sb(f"y_t{i}", [128, D]) for i in range(2)]
            scs = [sb(f"sc{i}", [128, 2, D], BF16) for i in range(2)]
            metaes = [sb(f"metae{i}", [128, NB, 2]) for i in range(2)]
            metats = [sb(f"metat{i}", [128, 2]) for i in range(4)]
            tokidx = sb("tokidx", [128, NB], I32)
            S.op("pool", lambda e: e.iota(tokidx[:, :], pattern=[[128, NB]], base=0, channel_multiplier=1), writes=[tokidx])
            yn = 0; sn = 0; mn = 0
            tiles_ = [(s0, min(512, CMAX - s0)) for s0 in range(0, CMAX, 512)]
            def scatter_expert(ex):
                nonlocal sn
                me = metaes[ex % 2]
                me_i = me.t.bitcast(I32)
                S.op("dve", lambda e, me=me, ex=ex: e.tensor_copy(out=me[:, :, 0:1], in_=gate[:, :, ex:ex + 1]), reads=[gate], writes=[me])
                S.op("dve", lambda e, me=me, me_i=me_i: e.tensor_copy(out=me_i[:, :, 1:2], in_=tokidx[:, :].rearrange("p (f o) -> p f o", o=1)), reads=[tokidx], writes=[me])
                for g2 in range(NB // 2):
                    sc = scs[sn % 2]; sn += 1
                    S.dma("sp", sc[:], xn_d.ap()[g2 * 256:(g2 + 1) * 256, :].rearrange("(b p) d -> p b d", p=128), reads=[d_xn], writes=[sc])
                    for b in range(2):
                        f = g2 * 2 + b
                        S.op("pool", lambda e, ex=ex, f=f, b=b, sc=sc: e.indirect_dma_start(
                            out=xes[ex].ap(), out_offset=bass.IndirectOffsetOnAxis(ap=posI[:, f, ex:ex + 1], axis=0),
                            in_=sc[:, b, :], in_offset=None, bounds_check=self.breg(e), oob_is_err=False),
                            reads=[sc, posI], writes=[d_xe[ex]], dma_key=d_xe[ex])
                        S.op("pool", lambda e, ex=ex, f=f, me=me: e.indirect_dma_start(
                            out=meta_ds[ex].ap(), out_offset=bass.IndirectOffsetOnAxis(ap=posI[:, f, ex:ex + 1], axis=0),
                            in_=me[:, f, :], in_offset=None, bounds_check=self.breg(e), oob_is_err=False),
                            reads=[me, posI, d_meta[ex]], writes=[d_meta[ex]], dma_key=d_meta[ex])

            wbs = [(self.dint(f"wb1_{q}_{self.cn}", [D, FF], BF16), self.dint(f"wb3_{q}_{self.cn}", [D, FF], BF16),
                    self.dint(f"wb2_{q}_{self.cn}", [FF, D], BF16)) for q in range(2)]
            d_wb = [[[S.buf(f"d_wb{q}_{m}_{hf}") for hf in range(2)] for m in range(3)] for q in range(2)]

            def cast_expert(ex):
                q = ex % 2
                for m, (dst, srcw, rows) in enumerate(((wbs[q][0], W["w1"].ap()[ex], D), (wbs[q][1], W["w3"].ap()[ex], D), (wbs[q][2], W["w2"].ap()[ex], FF))):
                    step = rows // 2
                    for hf in range(2):
                        r0 = hf * step
                        S.dma("pool", dst.ap()[r0:r0 + step, :], srcw[r0:r0 + step, :], writes=[d_wb[q][m][hf]])

            scatter_expert(0)
            cast_expert(0)
            for ex in range(NE):
                wb = wbs[ex % 2]
                v1 = wb[0].ap().rearrange("(k p) n -> p k n", p=128)
                v3 = wb[1].ap().rearrange("(k p) n -> p k n", p=128)
                v2 = wb[2].ap().rearrange("(k p) n -> p k n", p=128)
                dq = d_wb[ex % 2]
                for hf in range(2):
                    k0 = hf * 4
                    S.dma("sp", w1[:, k0:k0 + 4, :], v1[:, k0:k0 + 4, :], reads=[dq[0][hf]], writes=[w1])
                    S.dma("act", w3[:, k0:k0 + 4, :], v3[:, k0:k0 + 4, :], reads=[dq[1][hf]], writes=[w3])
                hk = NF // 2
                S.dma("sp", w2[:, 0:hk, :], v2[:, 0:hk, :], reads=[dq[2][0]], writes=[w2])
                S.dma("act", w2[:, hk:NF, :], v2[:, hk:NF, :], reads=[dq[2][1]], writes=[w2])
                if ex + 1 < NE:
                    cast_expert(ex + 1)
                    scatter_expert(ex + 1)
                for (s0, tw) in tiles_:
                    nbk = tw // 128
                    S.dma("sp", xe_t[:, 0:nbk, :], xes[ex].ap()[s0:s0 + tw, :].rearrange("(b p) d -> p b d", p=128), reads=[d_xe[ex]], writes=[xe_t])
                    self.transpose_blocks(xe_t, nbk, xeT)
                    for fc in range(NF):
                        p1 = self.nps(); p3 = self.nps()
                        self.proj_fm(p1, w1, fc * 128, xeT, n=tw)
                        self.proj_fm(p3, w3, fc * 128, xeT, n=tw)
                        s1 = s1s[fc % 2]
                        S.op("act", lambda e, p1=p1, s1=s1, tw=tw: e.activation(out=s1[:, 0:tw], in_=p1[:, 0:tw], func=AF.Silu), reads=[p1], writes=[s1])
                        S.op("dve", lambda e, p3=p3, s1=s1, fc=fc, tw=tw: e.tensor_tensor(out=hidT[:, fc, 0:tw], in0=p3[:, 0:tw], in1=s1[:, 0:tw], op=ALU.mult), reads=[p3, s1], writes=[hidT])
                    for b in range(nbk):
                        y_t = y_ts[yn % 2]; yn += 1
                        mt = metats[mn % 4]; mn += 1
                        mt_i = mt.t.bitcast(I32)
                        S.dma("sp", mt[:, :], meta_ds[ex].ap()[s0 + b * 128:s0 + (b + 1) * 128, :], reads=[d_meta[ex]], writes=[mt])
                        for hh in range(2):
                            py = self.nps()
                            for fc in range(NF):
                                S.op("pe", lambda e, py=py, fc=fc, b=b, hh=hh: e.matmul(py[:, :], hidT[:, fc, b * 128:(b + 1) * 128], w2[:, fc, hh * 512:(hh + 1) * 512],
                                                                                        start=(fc == 0), stop=(fc == NF - 1)), reads=[hidT, w2], writes=[py])
                            if hh == 0:
                                S.op("act", lambda e, py=py, y_t=y_t, mt=mt: e.activation(out=y_t[:, 0:512], in_=py[:, :], func=AF.Copy, scale=mt[:, 0:1]), reads=[py, mt], writes=[y_t])
                            else:
                                S.op("dve", lambda e, py=py, y_t=y_t, mt=mt: e.tensor_scalar(out=y_t[:, 512:1024], in0=py[:, :], scalar1=mt[:, 0:1], scalar2=None, op0=ALU.mult), reads=[py, mt], writes=[y_t])
                        S.op("pool", lambda e, y_t=y_t, mt_i=mt_i: e.indirect_dma_start(
                            out=x_out.ap(), out_offset=bass.IndirectOffsetOnAxis(ap=mt_i[:, 1:2], axis=0),
                            in_=y_t[:, :], in_offset=None, bounds_check=self.breg2(e), oob_is_err=False, compute_op=ALU.add),
                            reads=[y_t, mt, d_xacc], writes=[d_xacc], dma_key=d_xacc)
            S.end_phase()


Prog.moe = _moe


def group_mask(cfg):
    res = []
    for segs in core_segments():
        m = np.zeros((cfg.NB,), np.float32)
        bps = cfg.SEG // 128
        for si, (g, q, j) in enumerate(segs):
            m[si * bps:(si + 1) * bps] = 1.0 if g == "p" else 0.0
        res.append(np.ascontiguousarray(np.broadcast_to(m[None, :], (128, cfg.NB))))
    return res


OP, OP_N = _mk_layout([("gamma", D), ("hg", D), ("bg", 16)])


def _odd_mixer(self, x_in, x_out, W, par, flags):
    cfg, S, nc = self.cfg, self.S, self.nc
    TOK, NT = cfg.TOK, cfg.NT
    HD = 256
    hf_d = self.dint(f"hf_{self.cn}", [TOK, HD], F32)
    d_hf = S.buf("d_hf")
    d_xo = S.buf("d_xo_odd")
    for h in range(4):
        for dirn in (0, 1):
            with ExitStack() as ph:
                sb = lambda n, s_, d=F32: self.sb(ph, n, s_, d)
                bwd = dirn == 1
                wq = sb("o_wq", [128, 8, HD], BF16); wk = sb("o_wk", [128, 8, HD], BF16); wv = sb("o_wv", [128, 8, HD], BF16)
                wgt = sb("o_wg", [128, 8, 256], BF16)
                P = sb("o_par", [128, OP_N]); fl = sb("o_fl", [128, 2 * NT])
                win = W["w_in"].ap()
                self.load_w(wq, win[:, h * HD:(h + 1) * HD], 8, ndma=2)
                self.load_w(wk, win[:, D + h * HD:D + (h + 1) * HD], 8, ndma=2)
                self.load_w(wv, win[:, 2 * D + h * HD:2 * D + (h + 1) * HD], 8, ndma=2)
                ci = (8 if bwd else 0) + h
                cf = (12 if bwd else 4) + h
                wgr = W["wg_rep"].ap().rearrange("(k p) n -> p k n", p=128)
                S.dma("pool", wgt[:, :, 0:128], wgr[:, :, ci * 128:(ci + 1) * 128], writes=[wgt])
                S.dma("pool", wgt[:, :, 128:256], wgr[:, :, cf * 128:(cf + 1) * 128], writes=[wgt])
                S.dma("sp", P[:], par.ap(), writes=[P])
                S.dma("sp", fl[:], flags.ap(), writes=[fl])
                if bwd:
                    wo = sb("o_wo", [128, 8, HD], BF16); wout = sb("o_wout", [128, 2, D], BF16)
                    self.load_w(wo, win[:, 3 * D + h * HD:3 * D + (h + 1) * HD], 8, ndma=2)
                    self.load_w(wout, W["w_out"].ap()[h * HD:(h + 1) * HD, :], 2, ndma=1)
                    o_sig = sb("o_sig", [128, 4, HD], BF16)
                    hf_t = sb("hf_t", [128, 4, HD]); hsq = sb("hsq", [128, HD], BF16)
                    hss = sb("hss", [128, 4]); hrs = sb("hrs", [128, 4])
                    hs_b = sb("hs_b", [128, 4, HD], BF16); outT = sb("outT", [128, 2, 512], BF16)
                    xacc = sb("xacc", [128, 4, D])
                x_t = sb("o_xt", [128, 4, D]); xn_b = sb("o_xn", [128, 4, D], BF16); xnT = sb("o_xnT", [128, 8, 512], BF16)
                junk = sb("o_junk", [128, D], BF16); ss = sb("o_ss", [128, 8]); rstd = sb("o_rstd", [128, 8])
                qT = sb("o_qT", [128, 2, 512], BF16); kT = sb("o_kT", [128, 2, 512], BF16)
                ktok = sb("o_ktok", [128, 4, HD], BF16); v_aug = sb("o_vaug", [128, 4, HD + 1], BF16)
                ib = sb("o_ib", [128, 512]); Lb = sb("o_Lb", [128, 512]); Bnb = sb("o_Bnb", [128, 512]); betab = sb("o_beta", [128, 512])
                Rb = sb("o_Rb", [128, 512]); nRb = sb("o_nRb", [128, 512]); BmRb = sb("o_BmR", [128, 512])
                cB = sb("o_cB", [128, 1]); cR = sb("o_cR", [128, 1]); nbf = sb("o_nbf", [128, 1])
                cols = [sb(f"o_cols{i}", [128, 8]) for i in range(2)]
                dj = sb("o_dj", [128, 128])
                Es = [sb(f"o_E{i}", [128, 128]) for i in range(2)]
                smTs = [sb(f"o_smT{i}", [128, 128], BF16) for i in range(2)]
                Bss = [sb(f"o_Bs{i}", [128, HD + 1]) for i in range(2)]
                numas = [sb(f"o_num{i}", [128, HD + 1]) for i in range(2)]
                wgvs = [sb(f"o_wgv{i}", [128, HD + 1], BF16) for i in range(2)]
                C = sb("o_C", [128, 2, HD + 1]); C_bf = sb("o_Cbf", [128, 2, HD + 1], BF16)
                h_t = sb("o_ht", [128, 4, HD])
                d_in = S.buf("d_xin_o")
                S.op("pool", lambda e: e.memset(C[:], 0.0), writes=[C])
                S.op("pool", lambda e: e.memset(C_bf[:], 0.0), writes=[C_bf])
                S.op("pool", lambda e: e.memset(cB[:], 0.0), writes=[cB])
                S.op("pool", lambda e: e.memset(cR[:], 0.0), writes=[cR])
                S.op("pool", lambda e: e.memset(v_aug[:, :, HD:HD + 1], 1.0), writes=[v_aug])
                bfc = OP["bg"] + cf; bic = OP["bg"] + ci
                S.op("dve", lambda e: e.tensor_scalar(out=nbf[:, :], in0=P[:, bfc:bfc + 1], scalar1=-1.0, scalar2=None, op0=ALU.mult), reads=[P], writes=[nbf])
                mask = self.tril_f if bwd else self.tri_f
                tiles = range(NT - 1, -1, -1) if bwd else range(NT)
                cn = 0
                for i in tiles:
                    t0 = i * 512
                    S.dma("sp", x_t[:], x_in.ap()[t0:t0 + 512, :].rearrange("(b p) d -> p b d", p=128), reads=[d_in], writes=[x_t])
                    if bwd:
                        S.dma("sp", hf_t[:], hf_d.ap()[t0:t0 + 512, :].rearrange("(b p) d -> p b d", p=128), reads=[d_hf], writes=[hf_t])
                        src = x_in if h == 0 else x_out
                        S.dma("sp", xacc[:], src.ap()[t0:t0 + 512, :].rearrange("(b p) d -> p b d", p=128), reads=[d_in, d_xo], writes=[xacc])
                    self._rms(x_t, 4, P, OP["gamma"], xn_b, ss, rstd, junk, 128)
                    self.transpose_blocks(xn_b, 4, xnT)
                    for ec in range(2):
                        pq = self.nps(); self.proj_fm(pq, wq, ec * 128, xnT)
                        S.op("act", lambda e, pq=pq, ec=ec: e.copy(out=qT[:, ec, :], in_=pq[:, :]), reads=[pq], writes=[qT])
                        pk = self.nps(); self.proj_fm(pk, wk, ec * 128, xnT)
                        S.op("dve", lambda e, pk=pk, ec=ec: e.tensor_scalar(out=kT[:, ec, :], in0=pk[:, :], scalar1=1.0 / 16.0, scalar2=None, op0=ALU.mult), reads=[pk], writes=[kT])
                    for b in range(4):
                        pk2 = self.nps(); self.proj_tm(pk2, xnT, b * 128, wk, 0, n=HD)
                        S.op("act", lambda e, pk2=pk2, b=b: e.activation(out=ktok[:, b, :], in_=pk2[:, 0:HD], func=AF.Copy, scale=1.0 / 16.0), reads=[pk2], writes=[ktok])
                        pv = self.nps(); self.proj_tm(pv, xnT, b * 128, wv, 0, n=HD)
                        S.op("dve", lambda e, pv=pv, b=b: e.tensor_copy(out=v_aug[:, b, 0:HD], in_=pv[:, 0:HD]), reads=[pv], writes=[v_aug])
                        if bwd:
                            po_ = self.nps(); self.proj_tm(po_, xnT, b * 128, wo, 0, n=HD)
                            S.op("act", lambda e, po_=po_, b=b: e.activation(out=o_sig[:, b, :], in_=po_[:, 0:HD], func=AF.Sigmoid), reads=[po_], writes=[o_sig])
                    pgi = self.nps(); self.proj_fm(pgi, wgt, 0, xnT)
                    pgf = self.nps(); self.proj_fm(pgf, wgt, 128, xnT)
                    S.op("act", lambda e, pgi=pgi: e.activation(out=ib[:, :], in_=pgi[:, :], func=AF.Identity, bias=P[:, bic:bic + 1], scale=1.0), reads=[pgi, P], writes=[ib])
                    S.op("act", lambda e, pgf=pgf: e.activation(out=Lb[:, :], in_=pgf[:, :], func=AF.Exp, bias=nbf[:, 0:1], scale=-1.0), reads=[pgf, nbf], writes=[Lb])
                    S.op("act", lambda e: e.activation(out=Lb[:, :], in_=Lb[:, :], func=AF.Ln, bias=1.0, scale=1.0), reads=[Lb], writes=[Lb])
                    fcol = 2 * i + (1 if bwd else 0)
                    S.op("dve", lambda e, fcol=fcol: e.tensor_scalar(out=cB[:, :], in0=cB[:, :], scalar1=fl[:, fcol:fcol + 1], scalar2=None, op0=ALU.mult), reads=[cB, fl], writes=[cB])
                    S.op("dve", lambda e, fcol=fcol: e.tensor_scalar(out=cR[:, :], in0=cR[:, :], scalar1=fl[:, fcol:fcol + 1], scalar2=None, op0=ALU.mult), reads=[cR, fl], writes=[cR])
                    S.op("dve", lambda e, fcol=fcol: e.tensor_scalar(out=C[:].rearrange("p a b -> p (a b)"), in0=C[:].rearrange("p a b -> p (a b)"),
                                                                     scalar1=fl[:, fcol:fcol + 1], scalar2=None, op0=ALU.mult), reads=[C, fl], writes=[C])
                    S.op("act", lambda e: e.copy(out=C_bf[:].rearrange("p a b -> p (a b)"), in_=C[:].rearrange("p a b -> p (a b)")), reads=[C], writes=[C_bf])
                    rv = (lambda t: t.t[:, ::-1]) if bwd else (lambda t: t.t[:, :])
                    S.op("dve", lambda e: e.tensor_tensor_scan(out=rv(Bnb), data0=self.ones512[:, :], data1=rv(Lb), initial=cB[:, 0:1], op0=ALU.mult, op1=ALU.add),
                         reads=[Lb, cB, self.ones512], writes=[Bnb])
                    S.op("dve", lambda e: e.tensor_tensor(out=betab[:, :], in0=ib[:, :], in1=Bnb[:, :], op=ALU.add), reads=[ib, Bnb], writes=[betab])
                    S.op("dve", lambda e: e.tensor_tensor_scan(out=rv(Rb), data0=self.ones512[:, :], data1=rv(betab), initial=cR[:, 0:1], op0=ALU.mult, op1=ALU.max),
                         reads=[betab, cR, self.ones512], writes=[Rb])
                    S.op("dve", lambda e: e.tensor_scalar(out=nRb[:, :], in0=Rb[:, :], scalar1=-1.0, scalar2=None, op0=ALU.mult), reads=[Rb], writes=[nRb])
                    S.op("dve", lambda e: e.tensor_tensor(out=BmRb[:, :], in0=Bnb[:, :], in1=Rb[:, :], op=ALU.subtract), reads=[Bnb, Rb], writes=[BmRb])
                    chunks = range(3, -1, -1) if bwd else range(4)
                    first = True
                    for c in chunks:
                        c0 = c * 128
                        endc = c0 if bwd else c0 + 127
                        if first:
                            mu_buf, mu_ap = cR, cR.t[:, 0:1]
                        else:
                            pc_ = (c + 1) * 128 if bwd else c0 - 1
                            mu_buf, mu_ap = Rb, Rb.t[:, pc_:pc_ + 1]
                        first = False
                        col = cols[cn % 2]; E = Es[cn % 2]; smT = smTs[cn % 2]; Bs = Bss[cn % 2]; numa = numas[cn % 2]; wgv = wgvs[cn % 2]
                        cn += 1
                        for (src_b, ci_) in ((betab, 0), (Rb, 1), (BmRb, 2)):
                            S.op("dve", lambda e, src_b=src_b, ci_=ci_, col=col, c0=c0: e.scalar_tensor_tensor(out=dj[:, :], in0=src_b[:, c0:c0 + 128], scalar=1.0, in1=self.id_f[:, :],
                                                                                                       op0=ALU.mult, op1=ALU.mult, accum_out=col[:, ci_:ci_ + 1]),
                                 reads=[src_b, self.id_f], writes=[dj, col])
                        S.op("act", lambda e, col=col, mu_ap=mu_ap: e.activation(out=col[:, 3:4], in_=col[:, 1:2], func=AF.Exp, bias=mu_ap, scale=-1.0), reads=[col, mu_buf], writes=[col])
                        S.op("act", lambda e, col=col: e.activation(out=col[:, 4:5], in_=col[:, 2:3], func=AF.Exp), reads=[col], writes=[col])
                        S.op("act", lambda e, col=col, endc=endc: e.activation(out=col[:, 5:6], in_=col[:, 0:1], func=AF.Exp, bias=nRb[:, endc:endc + 1], scale=1.0), reads=[col, nRb], writes=[col])
                        S.op("act", lambda e, col=col, endc=endc, mu_ap=mu_ap: e.activation(out=col[:, 6:7], in_=mu_ap, func=AF.Exp, bias=nRb[:, endc:endc + 1], scale=1.0),
                             reads=[col, nRb, mu_buf], writes=[col])
                        psS = self.nps()
                        for ec in range(2):
                            S.op("pe", lambda e, psS=psS, ec=ec, c0=c0: e.matmul(psS[:, 0:128], kT[:, ec, c0:c0 + 128], qT[:, ec, c0:c0 + 128], start=(ec == 0), stop=(ec == 1)),
                                 reads=[kT, qT], writes=[psS])
                        S.op("act", lambda e, E=E, col=col, c0=c0: e.activation(out=E[:, :], in_=Rb[:, c0:c0 + 128], func=AF.Exp, bias=col[:, 0:1], scale=-1.0), reads=[Rb, col], writes=[E])
                        S.op("dve", lambda e, E=E: e.tensor_tensor(out=E[:, :], in0=E[:, :], in1=mask[:, :], op=ALU.mult), reads=[E, mask], writes=[E])
                        S.op("dve", lambda e, E=E, smT=smT, psS=psS: e.tensor_tensor(out=smT[:, :], in0=psS[:, 0:128], in1=E[:, :], op=ALU.mult), reads=[psS, E], writes=[smT])
                        psA = self.nps(); psB = self.nps()
                        S.op("pe", lambda e, psA=psA, smT=smT, c=c: e.matmul(psA[:, 0:HD + 1], smT[:, :], v_aug[:, c, :], start=True, stop=True), reads=[smT, v_aug], writes=[psA])
                        for ec in range(2):
                            S.op("pe", lambda e, psB=psB, ec=ec, c0=c0: e.matmul(psB[:, 0:HD + 1], qT[:, ec, c0:c0 + 128], C_bf[:, ec, :], start=(ec == 0), stop=(ec == 1)),
                                 reads=[qT, C_bf], writes=[psB])
                        S.op("act", lambda e, psB=psB, Bs=Bs, col=col: e.activation(out=Bs[:, :], in_=psB[:, 0:HD + 1], func=AF.Copy, scale=col[:, 3:4]), reads=[psB, col], writes=[Bs])
                        S.op("dve", lambda e, psA=psA, Bs=Bs, numa=numa: e.tensor_tensor(out=numa[:, :], in0=psA[:, 0:HD + 1], in1=Bs[:, :], op=ALU.add), reads=[psA, Bs], writes=[numa])
                        S.op("act", lambda e, numa=numa, col=col: e.activation(out=col[:, 7:8], in_=numa[:, HD:HD + 1], func=AF.Abs), reads=[numa], writes=[col])
                        S.op("dve", lambda e, col=col: e.tensor_tensor(out=col[:, 7:8], in0=col[:, 7:8], in1=col[:, 4:5], op=ALU.max), reads=[col], writes=[col])
                        S.op("dve", lambda e, col=col: e.reciprocal(out=col[:, 7:8], in_=col[:, 7:8]), reads=[col], writes=[col])
                        S.op("act", lambda e, numa=numa, col=col, c=c: e.activation(out=h_t[:, c, :], in_=numa[:, 0:HD], func=AF.Copy, scale=col[:, 7:8]), reads=[numa, col], writes=[h_t])
                        S.op("dve", lambda e, wgv=wgv, col=col, c=c: e.tensor_scalar(out=wgv[:, :], in0=v_aug[:, c, :], scalar1=col[:, 5:6], scalar2=None, op0=ALU.mult), reads=[v_aug, col], writes=[wgv])
                        for ec in range(2):
                            psU = self.nps()
                            S.op("pe", lambda e, psU=psU, ec=ec, wgv=wgv, c=c: e.matmul(psU[:, 0:HD + 1], ktok[:, c, ec * 128:(ec + 1) * 128], wgv[:, :], start=True, stop=True),
                                 reads=[ktok, wgv], writes=[psU])
                            S.op("dve", lambda e, psU=psU, ec=ec, col=col: e.scalar_tensor_tensor(out=C[:, ec, :], in0=C[:, ec, :], scalar=col[:, 6:7], in1=psU[:, 0:HD + 1],
                                                                                               op0=ALU.mult, op1=ALU.add), reads=[C, col, psU], writes=[C])
                        S.op("act", lambda e: e.copy(out=C_bf[:].rearrange("p a b -> p (a b)"), in_=C[:].rearrange("p a b -> p (a b)")), reads=[C], writes=[C_bf])
                    endt = 0 if bwd else 511
                    S.op("dve", lambda e, endt=endt: e.tensor_copy(out=cB[:, :], in_=Bnb[:, endt:endt + 1]), reads=[Bnb], writes=[cB])
                    S.op("dve", lambda e, endt=endt: e.tensor_copy(out=cR[:, :], in_=Rb[:, endt:endt + 1]), reads=[Rb], writes=[cR])
                    if not bwd:
                        S.dma("sp", hf_d.ap()[t0:t0 + 512, :].rearrange("(b p) d -> p b d", p=128), h_t[:], reads=[h_t], writes=[d_hf])
                    else:
                        S.op("dve", lambda e: e.tensor_tensor(out=h_t[:], in0=h_t[:], in1=hf_t[:], op=ALU.add), reads=[h_t, hf_t], writes=[h_t])
                        for b in range(4):
                            S.op("act", lambda e, b=b: e.activation(out=hsq[:, :], in_=h_t[:, b, :], func=AF.Square, accum_out=hss[:, b:b + 1]), reads=[h_t], writes=[hsq, hss])
                        S.op("act", lambda e: e.activation(out=hrs[:, :], in_=hss[:, :], func=AF.Ln, bias=EPS, scale=1.0 / HD), reads=[hss], writes=[hrs])
                        S.op("act", lambda e: e.activation(out=hrs[:, :], in_=hrs[:, :], func=AF.Exp, scale=-0.5), reads=[hrs], writes=[hrs])
                        hg0 = OP["hg"] + h * HD
                        for b in range(4):
                            S.op("dve", lambda e, b=b: e.scalar_tensor_tensor(out=h_t[:, b, :], in0=h_t[:, b, :], scalar=hrs[:, b:b + 1], in1=P[:, hg0:hg0 + HD], op0=ALU.mult, op1=ALU.mult),
                                 reads=[h_t, hrs, P], writes=[h_t])
                        S.op("dve", lambda e: e.tensor_tensor(out=hs_b[:], in0=h_t[:], in1=o_sig[:], op=ALU.mult), reads=[h_t, o_sig], writes=[hs_b])
                        for b in range(4):
                            pt = self.nps(); ptb = pt.t.bitcast(BF16)
                            for dc in range(2):
                                S.op("pe", lambda e, ptb=ptb, pt=pt, b=b, dc=dc: e.transpose(ptb[:, dc * 128:(dc + 1) * 128], hs_b[:, b, dc * 128:(dc + 1) * 128], self.id_b[:, :]),
                                     reads=[hs_b, self.id_b], writes=[pt])
                            S.op("act", lambda e, ptb=ptb, pt=pt, b=b: e.copy(out=outT[:, :, b * 128:(b + 1) * 128], in_=ptb[:, 0:256].rearrange("p (k t) -> p k t", k=2)), reads=[pt], writes=[outT])
                        for b in range(4):
                            for hh in range(2):
                                pp = self.nps()
                                for dc in range(2):
                                    S.op("pe", lambda e, pp=pp, b=b, hh=hh, dc=dc: e.matmul(pp[:, :], outT[:, dc, b * 128:(b + 1) * 128], wout[:, dc, hh * 512:(hh + 1) * 512],
                                                                                           start=(dc == 0), stop=(dc == 1)), reads=[outT, wout], writes=[pp])
                                S.op("dve", lambda e, pp=pp, b=b, hh=hh: e.tensor_tensor(out=xacc[:, b, hh * 512:(hh + 1) * 512], in0=xacc[:, b, hh * 512:(hh + 1) * 512], in1=pp[:, :], op=ALU.add),
                                     reads=[xacc, pp], writes=[xacc])
                        S.dma("sp", x_out.ap()[t0:t0 + 512, :].rearrange("(b p) d -> p b d", p=128), xacc[:], reads=[xacc], writes=[d_xo])
                S.end_phase()


Prog.odd_mixer = _odd_mixer


def pack_odd_params(norm_mix_l, hnorm_g, b_gate):
    P = np.zeros((128, OP_N), np.float32)
    P[:, OP["gamma"]:OP["gamma"] + D] = bc(norm_mix_l)
    P[:, OP["hg"]:OP["hg"] + D] = bc(hnorm_g.reshape(-1))
    P[:, OP["bg"]:OP["bg"] + 16] = bc(b_gate.reshape(-1))
    return P


def gate_rep(w_in_odd):
    g = w_in_odd[:, 4 * D:4 * D + 16]
    return np.ascontiguousarray(np.repeat(g, 128, axis=1))


def _xattn_inputs(p):
    return ({"wq": p.din("wq", [D, D]), "wkv": p.din("wkv", [D, 2 * D]), "wo": p.din("wo", [D, D]), "wr": p.din("wr", [D, NE])},
            p.din("x_par", [128, XP_N]), p.din("mem", [p.cfg.NSEG, MEM, D]))


def _moe_inputs(p):
    cfg = p.cfg
    return (p.din("aff_all", [128, NCORES * cfg.NB * NE]), p.din("aff_loc", [128, cfg.NB * NE]), p.din("gm", [128, cfg.NB]),
            {"w1": p.din("w1", [NE, D, cfg.FF]), "w3": p.din("w3", [NE, D, cfg.FF]), "w2": p.din("w2", [NE, cfg.FF, D])},
            p.din("m_par", [128, D]), p.dout("cnt", [128, NE]))


def build_L1(cfg):
    p = Prog(cfg)
    TOK, NT = cfg.TOK, cfg.NT
    x = p.din("x", [TOK, D]); flags = p.din("flags", [128, 2 * NT])
    ew = {"w_in": p.din("e_w_in", [D, 2048]), "w_out": p.din("e_w_out", [D, D]), "wsT": p.din("e_wsT", [128, 4, 128])}
    epar = p.din("e_par", [128, EP_N])
    xw, xpar, mem = _xattn_inputs(p)
    x1 = p.dint("x1", [TOK, D]); xo = p.dout("xo", [TOK, D]); aff = p.dout("aff", [128, cfg.NB * NE])
    p.consts()
    p.even_mixer(x, x1, ew, epar, flags)
    p.xattn_router(x1, xo, aff, xw, xpar, mem)
    p.st.close()
    return p


def build_L2(cfg):
    p = Prog(cfg)
    TOK, NT = cfg.TOK, cfg.NT
    x = p.din("x", [TOK, D]); flags = p.din("flags", [128, 2 * NT])
    aff_all, aff_loc, gm, mw, mpar, cnt = _moe_inputs(p)
    ow = {"w_in": p.din("o_w_in", [D, 4 * D + 16]), "wg_rep": p.din("o_wg_rep", [D, 16 * 128]), "w_out": p.din("o_w_out", [D, D])}
    opar = p.din("o_par", [128, OP_N])
    xw, xpar, mem = _xattn_inputs(p)
    x3 = p.dint("x3", [TOK, D]); x4 = p.dint("x4", [TOK, D]); xo = p.dout("xo", [TOK, D]); aff = p.dout("aff", [128, cfg.NB * NE])
    p.consts()
    p.moe(x, x3, aff_all, aff_loc, gm, mw, mpar, cnt)
    p.odd_mixer(x3, x4, ow, opar, flags)
    p.xattn_router(x4, xo, aff, xw, xpar, mem)
    p.st.close()
    return p


def build_L3(cfg):
    p = Prog(cfg)
    TOK = cfg.TOK
    x = p.din("x", [TOK, D])
    aff_all, aff_loc, gm, mw, mpar, cnt = _moe_inputs(p)
    fpar = p.din("f_par", [128, D])
    x6 = p.dint("x6", [TOK, D]); y = p.dout("y", [TOK, D])
    p.consts()
    p.moe(x, x6, aff_all, aff_loc, gm, mw, mpar, cnt)
    p.final_norm(x6, y, fpar)
    p.st.close()
    return p


def _launch(p, in_maps):
    res = run_bass_kernel_spmd(p.nc, in_maps, core_ids=list(range(NCORES)))
    return res.results


def _check_capacity(cfg, results):
    import sys
    allc = np.stack([r["cnt"][0] for r in results], 0)
    print(f"[moe] per-(core,expert) counts: mean={allc.mean():.1f} min={allc.min():.0f} max={allc.max():.0f} CMAX={cfg.CMAX}", file=sys.stderr)
    for c, r in enumerate(results):
        mx = float(np.max(r["cnt"][0]))
        if mx > cfg.CMAX:
            raise RuntimeError(f"MoE per-core expert capacity exceeded on core {c}: {mx} > {cfg.CMAX} "
                               "(static capacity assumption violated; refusing to return a silently wrong result)")


def run_model(cfg, I):
    f32 = lambda a: np.ascontiguousarray(np.asarray(a, dtype=np.float32))
    I = {k: f32(v) for k, v in I.items()}
    xsh = shard_tokens(cfg, I["x_prompt"], I["x_sample"])
    msh = shard_mem(cfg, I["mem_prompt"], I["mem_sample"])
    fl = tile_flags(cfg)
    gms = group_mask(cfg)

    def xattn_common(l):
        return {"wq": I["xa_wq"][l], "wkv": I["xa_wkv"][l], "wo": I["xa_wo"][l], "wr": I["moe_router"][l],
                "x_par": pack_xattn_params(I["norm_xattn"][l], I["norm_ffn"][l])}

    def moe_common(l, aff_locs):
        aff_all = np.ascontiguousarray(np.concatenate(aff_locs, axis=1))
        return {"aff_all": aff_all, "w1": I["moe_w1"][l], "w3": I["moe_w3"][l], "w2": I["moe_w2"][l],
                "m_par": np.ascontiguousarray(bc(I["norm_ffn"][l]))}

    p1 = build_L1(cfg)
    c1 = dict(xattn_common(0))
    c1.update({"e_w_in": I["even_w_in"][0], "e_w_out": I["even_w_out"][0],
               "e_wsT": np.ascontiguousarray(I["a_ws"][0].transpose(2, 0, 1)),
               "e_par": pack_even_params(I["norm_mix"][0], I["a_ln_g"][0], I["a_ln_b"][0], I["a_bs"][0], I["b_conv_w"][0],
                                         I["b_conv_b"][0], I["b_ln_g"][0], I["b_ln_b"][0])})
    r1 = _launch(p1, [dict(c1, x=xsh[c], mem=msh[c], flags=fl[c]) for c in range(NCORES)])
    del p1
    p2 = build_L2(cfg)
    c2 = dict(xattn_common(1))
    c2.update(moe_common(0, [r["aff"] for r in r1]))
    c2.update({"o_w_in": I["odd_w_in"][0], "o_wg_rep": gate_rep(I["odd_w_in"][0]), "o_w_out": I["odd_w_out"][0],
               "o_par": pack_odd_params(I["norm_mix"][1], I["odd_hnorm_g"][0], I["odd_b_gate"][0])})
    r2 = _launch(p2, [dict(c2, x=r1[c]["xo"], aff_loc=r1[c]["aff"], gm=gms[c], mem=msh[c], flags=fl[c]) for c in range(NCORES)])
    _check_capacity(cfg, r2)
    del p2, r1
    p3 = build_L3(cfg)
    c3 = moe_common(1, [r["aff"] for r in r2])
    c3["f_par"] = np.ascontiguousarray(bc(I["norm_final"]))
    r3 = _launch(p3, [dict(c3, x=r2[c]["xo"], aff_loc=r2[c]["aff"], gm=gms[c]) for c in range(NCORES)])
    _check_capacity(cfg, r3)
    yp, ys = unshard_tokens(cfg, [r["y"] for r in r3])
    return yp, ys


def kernel(**inputs):
    cfg = Cfg(seg=2048, ff=2048)
    yp, ys = run_model(cfg, inputs)
    return (yp.astype(np.float32), ys.astype(np.float32))


def _odd_mixer2(self, x_in, x_out, W, par, flags):
    cfg, S, nc = self.cfg, self.S, self.nc
    TOK, NT = cfg.TOK, cfg.NT
    HD = 256
    hf_ds = [self.dint(f"hf{h}_{self.cn}", [TOK, HD], F32) for h in range(4)]
    d_hf = [S.buf(f"d_hf{h}") for h in range(4)]
    d_xo = S.buf("d_xo_odd")
    win = W["w_in"].ap()
    wgr = W["wg_rep"].ap().rearrange("(k p) n -> p k n", p=128)
    for pair in ((0, 1), (2, 3)):
        for dirn in (0, 1):
            with ExitStack() as ph:
                sb = lambda n, s_, d=F32: self.sb(ph, n, s_, d)
                bwd = dirn == 1
                P = sb("o_par", [128, OP_N]); fl = sb("o_fl", [128, 2 * NT])
                S.dma("sp", P[:], par.ap(), writes=[P])
                S.dma("sp", fl[:], flags.ap(), writes=[fl])
                x_t = sb("o_xt", [128, 4, D]); xn_b = sb("o_xn", [128, 4, D], BF16); xnT = sb("o_xnT", [128, 8, 512], BF16)
                junk = sb("o_junk", [128, D], BF16); ss = sb("o_ss", [128, 8]); rstd = sb("o_rstd", [128, 8])
                d_in = S.buf("d_xin_o")
                mask = self.tril_f if bwd else self.tri_f
                HB = {}
                for h in pair:
                    B = {}
                    B["wq"] = sb(f"wq{h}", [128, 8, HD], BF16); B["wk"] = sb(f"wk{h}", [128, 8, HD], BF16); B["wv"] = sb(f"wv{h}", [128, 8, HD], BF16)
                    B["wgt"] = sb(f"wg{h}", [128, 8, 256], BF16)
                    self.load_w(B["wq"], win[:, h * HD:(h + 1) * HD], 8, ndma=2)
                    self.load_w(B["wk"], win[:, D + h * HD:D + (h + 1) * HD], 8, ndma=2)
                    self.load_w(B["wv"], win[:, 2 * D + h * HD:2 * D + (h + 1) * HD], 8, ndma=2)
                    ci = (8 if bwd else 0) + h
                    cf = (12 if bwd else 4) + h
                    B["ci"], B["cf"] = ci, cf
                    S.dma("pool", B["wgt"][:, :, 0:128], wgr[:, :, ci * 128:(ci + 1) * 128], writes=[B["wgt"]])
                    S.dma("pool", B["wgt"][:, :, 128:256], wgr[:, :, cf * 128:(cf + 1) * 128], writes=[B["wgt"]])
                    if bwd:
                        B["wo"] = sb(f"wo{h}", [128, 8, HD], BF16); B["wout"] = sb(f"wout{h}", [128, 2, D], BF16)
                        self.load_w(B["wo"], win[:, 3 * D + h * HD:3 * D + (h + 1) * HD], 8, ndma=2)
                        self.load_w(B["wout"], W["w_out"].ap()[h * HD:(h + 1) * HD, :], 2, ndma=1)
                        B["o_sig"] = sb(f"osig{h}", [128, 4, HD], BF16)
                        B["hf_t"] = sb(f"hft{h}", [128, 4, HD])
                        B["hss"] = sb(f"hss{h}", [128, 4]); B["hrs"] = sb(f"hrs{h}", [128, 4])
                        B["hs_b"] = sb(f"hsb{h}", [128, 4, HD], BF16); B["outT"] = sb(f"outT{h}", [128, 2, 512], BF16)
                    B["qT"] = sb(f"qT{h}", [128, 2, 512], BF16); B["kT"] = sb(f"kT{h}", [128, 2, 512], BF16)
                    B["ktok"] = sb(f"ktok{h}", [128, 4, HD], BF16); B["v_aug"] = sb(f"vaug{h}", [128, 4, HD + 1], BF16)
                    B["ib"] = sb(f"ib{h}", [128, 512]); B["Lb"] = sb(f"Lb{h}", [128, 512]); B["Bnb"] = sb(f"Bnb{h}", [128, 512])
                    B["betab"] = sb(f"beta{h}", [128, 512]); B["Rb"] = sb(f"Rb{h}", [128, 512])
                    B["cB"] = sb(f"cB{h}", [128, 1]); B["cR"] = sb(f"cR{h}", [128, 1]); B["nbf"] = sb(f"nbf{h}", [128, 1])
                    B["cols"] = [sb(f"cols{h}_{i}", [128, 12]) for i in range(2)]
                    B["dj"] = sb(f"dj{h}", [128, 128])
                    B["E"] = sb(f"E{h}", [128, 128]); B["smT"] = sb(f"smT{h}", [128, 128], BF16)
                    B["Bs"] = sb(f"Bs{h}", [128, HD + 1]); B["numa"] = sb(f"num{h}", [128, HD + 1]); B["wgv"] = sb(f"wgv{h}", [128, HD + 1], BF16)
                    B["C"] = sb(f"C{h}", [128, 2, HD + 1]); B["C_bf"] = sb(f"Cbf{h}", [128, 2, HD + 1], BF16)
                    B["h_t"] = sb(f"ht{h}", [128, 4, HD])
                    B["cn"] = 0
                    C, C_bf, cB, cR, v_aug, nbf = B["C"], B["C_bf"], B["cB"], B["cR"], B["v_aug"], B["nbf"]
                    S.op("pool", lambda e, C=C: e.memset(C[:], 0.0), writes=[C])
                    S.op("pool", lambda e, C_bf=C_bf: e.memset(C_bf[:], 0.0), writes=[C_bf])
                    S.op("pool", lambda e, cB=cB: e.memset(cB[:], 0.0), writes=[cB])
                    S.op("pool", lambda e, cR=cR: e.memset(cR[:], 0.0), writes=[cR])
                    S.op("pool", lambda e, v_aug=v_aug: e.memset(v_aug[:, :, HD:HD + 1], 1.0), writes=[v_aug])
                    bfc = OP["bg"] + cf
                    S.op("dve", lambda e, nbf=nbf, bfc=bfc: e.tensor_scalar(out=nbf[:, :], in0=P[:, bfc:bfc + 1], scalar1=-1.0, scalar2=None, op0=ALU.mult), reads=[P], writes=[nbf])
                    HB[h] = B

                def gen_head(h, i):
                    B = HB[h]
                    wq, wk, wv, wgt = B["wq"], B["wk"], B["wv"], B["wgt"]
                    qT, kT, ktok, v_aug = B["qT"], B["kT"], B["ktok"], B["v_aug"]
                    ib, Lb, Bnb, betab, Rb = B["ib"], B["Lb"], B["Bnb"], B["betab"], B["Rb"]
                    cB, cR, nbf, dj = B["cB"], B["cR"], B["nbf"], B["dj"]
                    E, smT, Bs, numa, wgv, C, C_bf, h_t = B["E"], B["smT"], B["Bs"], B["numa"], B["wgv"], B["C"], B["C_bf"], B["h_t"]
                    bic = OP["bg"] + B["ci"]
                    t0 = i * 512
                    for ec in range(2):
                        pq = self.nps(); self.proj_fm(pq, wq, ec * 128, xnT)
                        S.op("act", lambda e, pq=pq, ec=ec: e.copy(out=qT[:, ec, :], in_=pq[:, :]), reads=[pq], writes=[qT])
                        pk = self.nps(); self.proj_fm(pk, wk, ec * 128, xnT)
                        S.op("dve", lambda e, pk=pk, ec=ec: e.tensor_scalar(out=kT[:, ec, :], in0=pk[:, :], scalar1=1.0 / 16.0, scalar2=None, op0=ALU.mult), reads=[pk], writes=[kT])
                        yield
                    for b in range(4):
                        pk2 = self.nps(); self.proj_tm(pk2, xnT, b * 128, wk, 0, n=HD)
                        S.op("act", lambda e, pk2=pk2, b=b: e.activation(out=ktok[:, b, :], in_=pk2[:, 0:HD], func=AF.Copy, scale=1.0 / 16.0), reads=[pk2], writes=[ktok])
                        pv = self.nps(); self.proj_tm(pv, xnT, b * 128, wv, 0, n=HD)
                        S.op("dve", lambda e, pv=pv, b=b: e.tensor_copy(out=v_aug[:, b, 0:HD], in_=pv[:, 0:HD]), reads=[pv], writes=[v_aug])
                        if bwd:
                            po_ = self.nps(); self.proj_tm(po_, xnT, b * 128, B["wo"], 0, n=HD)
                            S.op("act", lambda e, po_=po_, b=b: e.activation(out=B["o_sig"][:, b, :], in_=po_[:, 0:HD], func=AF.Sigmoid), reads=[po_], writes=[B["o_sig"]])
                        yield
                    pgi = self.nps(); self.proj_fm(pgi, wgt, 0, xnT)
                    pgf = self.nps(); self.proj_fm(pgf, wgt, 128, xnT)
                    S.op("act", lambda e, pgi=pgi: e.activation(out=ib[:, :], in_=pgi[:, :], func=AF.Identity, bias=P[:, bic:bic + 1], scale=1.0), reads=[pgi, P], writes=[ib])
                    S.op("act", lambda e, pgf=pgf: e.activation(out=Lb[:, :], in_=pgf[:, :], func=AF.Exp, bias=nbf[:, 0:1], scale=-1.0), reads=[pgf, nbf], writes=[Lb])
                    S.op("act", lambda e: e.activation(out=Lb[:, :], in_=Lb[:, :], func=AF.Ln, bias=1.0, scale=1.0), reads=[Lb], writes=[Lb])
                    fcol = 2 * i + (1 if bwd else 0)
                    S.op("dve", lambda e: e.tensor_scalar(out=cB[:, :], in0=cB[:, :], scalar1=fl[:, fcol:fcol + 1], scalar2=None, op0=ALU.mult), reads=[cB, fl], writes=[cB])
                    S.op("dve", lambda e: e.tensor_scalar(out=cR[:, :], in0=cR[:, :], scalar1=fl[:, fcol:fcol + 1], scalar2=None, op0=ALU.mult), reads=[cR, fl], writes=[cR])
                    S.op("dve", lambda e: e.tensor_scalar(out=C[:].rearrange("p a b -> p (a b)"), in0=C[:].rearrange("p a b -> p (a b)"),
                                                          scalar1=fl[:, fcol:fcol + 1], scalar2=None, op0=ALU.mult), reads=[C, fl], writes=[C])
                    S.op("act", lambda e: e.copy(out=C_bf[:].rearrange("p a b -> p (a b)"), in_=C[:].rearrange("p a b -> p (a b)")), reads=[C], writes=[C_bf])
                    yield
                    rv = (lambda t: t.t[:, ::-1]) if bwd else (lambda t: t.t[:, :])
                    S.op("dve", lambda e: e.tensor_tensor_scan(out=rv(Bnb), data0=self.ones512[:, :], data1=rv(Lb), initial=cB[:, 0:1], op0=ALU.mult, op1=ALU.add),
                         reads=[Lb, cB, self.ones512], writes=[Bnb])
                    S.op("dve", lambda e: e.tensor_tensor(out=betab[:, :], in0=ib[:, :], in1=Bnb[:, :], op=ALU.add), reads=[ib, Bnb], writes=[betab])
                    S.op("dve", lambda e: e.tensor_tensor_scan(out=rv(Rb), data0=self.ones512[:, :], data1=rv(betab), initial=cR[:, 0:1], op0=ALU.mult, op1=ALU.max),
                         reads=[betab, cR, self.ones512], writes=[Rb])
                    yield
                    chunks = range(3, -1, -1) if bwd else range(4)
                    first = True
                    for c in chunks:
                        c0 = c * 128
                        endc = c0 if bwd else c0 + 127
                        if first:
                            mu_buf, mu_ap = cR, cR.t[:, 0:1]
                        else:
                            pc_ = (c + 1) * 128 if bwd else c0 - 1
                            mu_buf, mu_ap = Rb, Rb.t[:, pc_:pc_ + 1]
                        first = False
                        col = B["cols"][B["cn"] % 2]; B["cn"] += 1
                        for (src_b, ci_) in ((betab, 0), (Rb, 1), (Bnb, 2)):
                            S.op("dve", lambda e, src_b=src_b, ci_=ci_, col=col, c0=c0: e.scalar_tensor_tensor(out=dj[:, :], in0=src_b[:, c0:c0 + 128], scalar=1.0, in1=self.id_f[:, :],
                                                                                                              op0=ALU.mult, op1=ALU.mult, accum_out=col[:, ci_:ci_ + 1]),
                                 reads=[src_b, self.id_f], writes=[dj, col])
                        S.op("dve", lambda e, col=col, endc=endc: e.tensor_scalar(out=col[:, 8:9], in0=Rb[:, endc:endc + 1], scalar1=-1.0, scalar2=None, op0=ALU.mult), reads=[Rb], writes=[col])
                        S.op("dve", lambda e, col=col: e.tensor_tensor(out=col[:, 9:10], in0=col[:, 2:3], in1=col[:, 1:2], op=ALU.subtract), reads=[col], writes=[col])
                        yield
                        S.op("act", lambda e, col=col, mu_ap=mu_ap, mu_buf=mu_buf: e.activation(out=col[:, 3:4], in_=col[:, 1:2], func=AF.Exp, bias=mu_ap, scale=-1.0), reads=[col, mu_buf], writes=[col])
                        S.op("act", lambda e, col=col: e.activation(out=col[:, 4:5], in_=col[:, 9:10], func=AF.Exp), reads=[col], writes=[col])
                        S.op("act", lambda e, col=col: e.activation(out=col[:, 5:6], in_=col[:, 0:1], func=AF.Exp, bias=col[:, 8:9], scale=1.0), reads=[col], writes=[col])
                        S.op("act", lambda e, col=col, mu_ap=mu_ap, mu_buf=mu_buf: e.activation(out=col[:, 6:7], in_=mu_ap, func=AF.Exp, bias=col[:, 8:9], scale=1.0),
                             reads=[col, mu_buf], writes=[col])
                        psS = self.nps()
                        for ec in range(2):
                            S.op("pe", lambda e, psS=psS, ec=ec, c0=c0: e.matmul(psS[:, 0:128], kT[:, ec, c0:c0 + 128], qT[:, ec, c0:c0 + 128], start=(ec == 0), stop=(ec == 1)),
                                 reads=[kT, qT], writes=[psS])
                        S.op("act", lambda e, col=col, c0=c0: e.activation(out=E[:, :], in_=Rb[:, c0:c0 + 128], func=AF.Exp, bias=col[:, 0:1], scale=-1.0), reads=[Rb, col], writes=[E])
                        yield
                        S.op("dve", lambda e: e.tensor_tensor(out=E[:, :], in0=E[:, :], in1=mask[:, :], op=ALU.mult), reads=[E, mask], writes=[E])
                        S.op("dve", lambda e, psS=psS: e.tensor_tensor(out=smT[:, :], in0=psS[:, 0:128], in1=E[:, :], op=ALU.mult), reads=[psS, E], writes=[smT])
                        psA = self.nps(); psB = self.nps()
                        S.op("pe", lambda e, psA=psA, c=c: e.matmul(psA[:, 0:HD + 1], smT[:, :], v_aug[:, c, :], start=True, stop=True), reads=[smT, v_aug], writes=[psA])
                        for ec in range(2):
                            S.op("pe", lambda e, psB=psB, ec=ec, c0=c0: e.matmul(psB[:, 0:HD + 1], qT[:, ec, c0:c0 + 128], C_bf[:, ec, :], start=(ec == 0), stop=(ec == 1)),
                                 reads=[qT, C_bf], writes=[psB])
                        S.op("act", lambda e, psB=psB, col=col: e.activation(out=Bs[:, :], in_=psB[:, 0:HD + 1], func=AF.Copy, scale=col[:, 3:4]), reads=[psB, col], writes=[Bs])
                        yield
                        S.op("dve", lambda e, psA=psA: e.tensor_tensor(out=numa[:, :], in0=psA[:, 0:HD + 1], in1=Bs[:, :], op=ALU.add), reads=[psA, Bs], writes=[numa])
                        S.op("act", lambda e, col=col: e.activation(out=col[:, 7:8], in_=numa[:, HD:HD + 1], func=AF.Abs), reads=[numa], writes=[col])
                        S.op("dve", lambda e, col=col: e.tensor_tensor(out=col[:, 7:8], in0=col[:, 7:8], in1=col[:, 4:5], op=ALU.max), reads=[col], writes=[col])
                        S.op("dve", lambda e, col=col: e.reciprocal(out=col[:, 7:8], in_=col[:, 7:8]), reads=[col], writes=[col])
                        S.op("act", lambda e, col=col, c=c: e.activation(out=h_t[:, c, :], in_=numa[:, 0:HD], func=AF.Copy, scale=col[:, 7:8]), reads=[numa, col], writes=[h_t])
                        yield
                        S.op("dve", lambda e, col=col, c=c: e.tensor_scalar(out=wgv[:, :], in0=v_aug[:, c, :], scalar1=col[:, 5:6], scalar2=None, op0=ALU.mult), reads=[v_aug, col], writes=[wgv])
                        for ec in range(2):
                            psU = self.nps()
                            S.op("pe", lambda e, psU=psU, ec=ec, c=c: e.matmul(psU[:, 0:HD + 1], ktok[:, c, ec * 128:(ec + 1) * 128], wgv[:, :], start=True, stop=True),
                                 reads=[ktok, wgv], writes=[psU])
                            S.op("dve", lambda e, psU=psU, ec=ec, col=col: e.scalar_tensor_tensor(out=C[:, ec, :], in0=C[:, ec, :], scalar=col[:, 6:7], in1=psU[:, 0:HD + 1],
                                                                                               op0=ALU.mult, op1=ALU.add), reads=[C, col, psU], writes=[C])
                        S.op("act", lambda e: e.copy(out=C_bf[:].rearrange("p a b -> p (a b)"), in_=C[:].rearrange("p a b -> p (a b)")), reads=[C], writes=[C_bf])
                        yield
                    endt = 0 if bwd else 511
                    S.op("dve", lambda e: e.tensor_copy(out=cB[:, :], in_=Bnb[:, endt:endt + 1]), reads=[Bnb], writes=[cB])
                    S.op("dve", lambda e: e.tensor_copy(out=cR[:, :], in_=Rb[:, endt:endt + 1]), reads=[Rb], writes=[cR])
                    if not bwd:
                        S.dma("sp", hf_ds[h].ap()[t0:t0 + 512, :].rearrange("(b p) d -> p b d", p=128), h_t[:], reads=[h_t], writes=[d_hf[h]])
                    else:
                        hf_t, hss, hrs, hs_b, outT, o_sig, wout = B["hf_t"], B["hss"], B["hrs"], B["hs_b"], B["outT"], B["o_sig"], B["wout"]
                        S.op("dve", lambda e: e.tensor_tensor(out=h_t[:], in0=h_t[:], in1=hf_t[:], op=ALU.add), reads=[h_t, hf_t], writes=[h_t])
                        for b in range(4):
                            S.op("act", lambda e, b=b: e.activation(out=dj[:, :].bitcast(BF16), in_=h_t[:, b, :], func=AF.Square, accum_out=hss[:, b:b + 1]), reads=[h_t], writes=[dj, hss])
                        S.op("act", lambda e: e.activation(out=hrs[:, :], in_=hss[:, :], func=AF.Ln, bias=EPS, scale=1.0 / HD), reads=[hss], writes=[hrs])
                        S.op("act", lambda e: e.activation(out=hrs[:, :], in_=hrs[:, :], func=AF.Exp, scale=-0.5), reads=[hrs], writes=[hrs])
                        yield
                        hg0 = OP["hg"] + h * HD
                        for b in range(4):
                            S.op("dve", lambda e, b=b: e.scalar_tensor_tensor(out=h_t[:, b, :], in0=h_t[:, b, :], scalar=hrs[:, b:b + 1], in1=P[:, hg0:hg0 + HD], op0=ALU.mult, op1=ALU.mult),
                                 reads=[h_t, hrs, P], writes=[h_t])
                        S.op("dve", lambda e: e.tensor_tensor(out=hs_b[:], in0=h_t[:], in1=o_sig[:], op=ALU.mult), reads=[h_t, o_sig], writes=[hs_b])
                        for b in range(4):
                            pt = self.nps(); ptb = pt.t.bitcast(BF16)
                            for dc in range(2):
                                S.op("pe", lambda e, ptb=ptb, b=b, dc=dc: e.transpose(ptb[:, dc * 128:(dc + 1) * 128], hs_b[:, b, dc * 128:(dc + 1) * 128], self.id_b[:, :]),
                                     reads=[hs_b, self.id_b], writes=[pt])
                            S.op("act", lambda e, ptb=ptb, b=b: e.copy(out=outT[:, :, b * 128:(b + 1) * 128], in_=ptb[:, 0:256].rearrange("p (k t) -> p k t", k=2)), reads=[pt], writes=[outT])
                        yield
                        for b in range(4):
                            for hh in range(2):
                                pp = self.nps()
                                for dc in range(2):
                                    S.op("pe", lambda e, pp=pp, b=b, hh=hh, dc=dc: e.matmul(pp[:, :], outT[:, dc, b * 128:(b + 1) * 128], wout[:, dc, hh * 512:(hh + 1) * 512],
                                                                                           start=(dc == 0), stop=(dc == 1)), reads=[outT, wout], writes=[pp])
                                S.op("dve", lambda e, pp=pp, b=b, hh=hh: e.tensor_tensor(out=x_t[:, b, hh * 512:(hh + 1) * 512], in0=x_t[:, b, hh * 512:(hh + 1) * 512], in1=pp[:, :], op=ALU.add),
                                     reads=[x_t, pp], writes=[x_t])
                            yield

                tiles = range(NT - 1, -1, -1) if bwd else range(NT)
                for i in tiles:
                    t0 = i * 512
                    S.dma("sp", x_t[:], x_in.ap()[t0:t0 + 512, :].rearrange("(b p) d -> p b d", p=128), reads=[d_in, d_xo], writes=[x_t])
                    if bwd:
                        for h in pair:
                            S.dma("sp", HB[h]["hf_t"][:], hf_ds[h].ap()[t0:t0 + 512, :].rearrange("(b p) d -> p b d", p=128), reads=[d_hf[h]], writes=[HB[h]["hf_t"]])
                    self._rms(x_t, 4, P, OP["gamma"], xn_b, ss, rstd, junk, 128)
                    self.transpose_blocks(xn_b, 4, xnT)
                    if bwd and pair[0] != 0:
                        S.dma("sp", x_t[:], x_out.ap()[t0:t0 + 512, :].rearrange("(b p) d -> p b d", p=128), reads=[d_xo, xn_b], writes=[x_t])
                    gens = [gen_head(h, i) for h in pair]
                    live = list(gens)
                    while live:
                        for g in list(live):
                            try:
                                next(g)
                            except StopIteration:
                                live.remove(g)
                    if bwd:
                        S.dma("sp", x_out.ap()[t0:t0 + 512, :].rearrange("(b p) d -> p b d", p=128), x_t[:], reads=[x_t], writes=[d_xo])
                S.end_phase()


Prog.odd_mixer = _odd_mixer2
```
